# Optimizing a Trainium2 kernel written in Bass

```python
import math
import jax
import jax.numpy as jnp
from jax import lax
import numpy as np

D_MODEL = 1024
BATCH = 16
SEQ = 2048
DEPTH = 2

HEAD_DIM = 64
GRID_W = 64
Q_BLOCK = 128
ROPE_THETA = 10000.0
NORM_EPS = 1e-6
NEG_INF = -1e30

NUM_BUCKETS = 32
REL_MAX_DISTANCE = 1024

MLA_HEADS = 4
MLA_NOPE_DIM = 64
MLA_ROPE_DIM = 32
MLA_QK_DIM = MLA_NOPE_DIM + MLA_ROPE_DIM
MLA_V_DIM = 64
MLA_Q_RANK = 256
MLA_KV_RANK = 128

DIL_HEADS = 4
DIL_PATTERNS = ((128, 1), (512, 4), (2048, 16))
DIL_BLOCK = max(w // (2 * d) for w, d in DIL_PATTERNS)

GQA_HEADS = 4
GQA_KV_HEADS = 2
GQA_GROUP = GQA_HEADS // GQA_KV_HEADS
AXIAL_DIM = HEAD_DIM // 2

DIFF_HEADS = 4
DIFF_QK_DIM = HEAD_DIM // 2
DIFF_V_DIM = HEAD_DIM

NUM_BIAS_HEADS = DIL_HEADS + DIFF_HEADS

D_MIX = MLA_HEADS * MLA_V_DIM + DIL_HEADS * HEAD_DIM + GQA_HEADS * HEAD_DIM + DIFF_HEADS * DIFF_V_DIM

IN_SIZES = (
    MLA_Q_RANK, MLA_KV_RANK, MLA_ROPE_DIM,
    DIL_HEADS * HEAD_DIM, DIL_HEADS * HEAD_DIM, DIL_HEADS * HEAD_DIM,
    GQA_HEADS * HEAD_DIM, GQA_KV_HEADS * HEAD_DIM, GQA_KV_HEADS * HEAD_DIM,
    DIFF_HEADS * 2 * DIFF_QK_DIM, DIFF_HEADS * 2 * DIFF_QK_DIM, DIFF_HEADS * DIFF_V_DIM,
)
IN_COLS = sum(IN_SIZES)

N_GROUPS = 4
EXPERTS_PER_GROUP = 4
N_EXPERTS = N_GROUPS * EXPERTS_PER_GROUP
TOP_K = 2
D_FF_EXPERT = 512

kernel_name = 'hybrid_parallel_head_encoder'


def rms_norm(x, gain=None):
    xf = x.astype(jnp.float32)
    y = xf * lax.rsqrt(jnp.mean(xf * xf, axis=-1, keepdims=True) + NORM_EPS)
    if gain is not None:
        y = y * gain.astype(jnp.float32)
    return y.astype(x.dtype)


def rope_cos_sin(pos, dim):
    inv = 1.0 / (ROPE_THETA ** (jnp.arange(0, dim, 2, dtype=jnp.float32) / dim))
    ang = pos.astype(jnp.float32)[:, None] * inv[None, :]
    ang = jnp.concatenate([ang, ang], axis=-1)
    return jnp.cos(ang), jnp.sin(ang)


def apply_rope(x, cos, sin):
    half = x.shape[-1] // 2
    xf = x.astype(jnp.float32)
    rot = jnp.concatenate([-xf[..., half:], xf[..., :half]], axis=-1)
    return (xf * cos[None, :, None, :] + rot * sin[None, :, None, :]).astype(x.dtype)


def axial_rope(x, row_cos, row_sin, col_cos, col_sin):
    return jnp.concatenate([apply_rope(x[..., :AXIAL_DIM], row_cos, row_sin),
                            apply_rope(x[..., AXIAL_DIM:], col_cos, col_sin)], axis=-1)


def rel_bucket(rel):
    half = NUM_BUCKETS // 2
    max_exact = half // 2
    n = jnp.abs(rel)
    nf = jnp.maximum(n, 1).astype(jnp.float32)
    log_ratio = jnp.log(nf / max_exact) / math.log(REL_MAX_DISTANCE / max_exact)
    large = jnp.minimum(max_exact + (log_ratio * (half - max_exact)).astype(jnp.int32), half - 1)
    return jnp.where(rel > 0, half, 0) + jnp.where(n < max_exact, n, large)


def sweep_query_blocks(block_fn, q):
    b, s = q.shape[:2]
    nb = s // Q_BLOCK
    qb = jnp.moveaxis(q.reshape((b, nb, Q_BLOCK) + q.shape[2:]), 1, 0)
    starts = jnp.arange(nb, dtype=jnp.int32) * Q_BLOCK
    out = lax.map(lambda a: block_fn(a[0], a[1]), (starts, qb))
    out = jnp.moveaxis(out, 0, 1)
    return out.reshape((b, s) + out.shape[3:])


def mla_mixer(c_q, c_kv, k_rope, q_norm_g, kv_norm_g, w_uq, w_ukv, qk_g, cos, sin):
    b, s, _ = c_q.shape
    q = jnp.einsum('bsr,rc->bsc', rms_norm(c_q, q_norm_g), w_uq).reshape(b, s, MLA_HEADS, MLA_QK_DIM)
    kv = jnp.einsum('bsr,rc->bsc', rms_norm(c_kv, kv_norm_g), w_ukv)
    kv = kv.reshape(b, s, MLA_HEADS, MLA_NOPE_DIM + MLA_V_DIM)
    k_nope, v = kv[..., :MLA_NOPE_DIM], kv[..., MLA_NOPE_DIM:]
    k_pe = jnp.broadcast_to(k_rope[:, :, None, :], (b, s, MLA_HEADS, MLA_ROPE_DIM))
    k = jnp.concatenate([k_nope, k_pe], axis=-1)
    q = rms_norm(q, qk_g[0])
    k = rms_norm(k, qk_g[1])
    q = jnp.concatenate([q[..., :MLA_NOPE_DIM], apply_rope(q[..., MLA_NOPE_DIM:], cos, sin)], axis=-1)
    k = jnp.concatenate([k[..., :MLA_NOPE_DIM], apply_rope(k[..., MLA_NOPE_DIM:], cos, sin)], axis=-1)
    scale = MLA_QK_DIM ** -0.5

    def block(start, qb):
        sc = jnp.einsum('bqhe,bkhe->bhqk', qb, k).astype(jnp.float32) * scale
        p = jax.nn.softmax(sc, axis=-1).astype(v.dtype)
        return jnp.einsum('bhqk,bkhe->bqhe', p, v)

    return sweep_query_blocks(block, q).reshape(b, s, MLA_HEADS * MLA_V_DIM)


def dilated_branch(q, k, v, bias_table, dilation, half_steps):
    b, s, h, e = q.shape
    n_sub = s // dilation
    nb = -(-n_sub // DIL_BLOCK)
    lp = nb * DIL_BLOCK

    def to_sub(a):
        a = a.reshape(b, n_sub, dilation, h, e).transpose(0, 2, 3, 1, 4)
        return jnp.pad(a, ((0, 0), (0, 0), (0, 0), (0, lp - n_sub), (0, 0)))

    def band(a):
        a = jnp.pad(a, ((0, 0), (0, 0), (0, 0), (DIL_BLOCK, DIL_BLOCK), (0, 0)))
        a = a.reshape(b, dilation, h, nb + 2, DIL_BLOCK, e)
        return jnp.concatenate([a[:, :, :, :-2], a[:, :, :, 1:-1], a[:, :, :, 2:]], axis=4)

    qb = to_sub(q).reshape(b, dilation, h, nb, DIL_BLOCK, e)
    kb = band(to_sub(k))
    vb = band(to_sub(v))
    scores = jnp.einsum('brhnqe,brhnke->brhnqk', qb, kb).astype(jnp.float32) * (e ** -0.5)
    qi = jnp.arange(DIL_BLOCK, dtype=jnp.int32)
    kj = jnp.arange(3 * DIL_BLOCK, dtype=jnp.int32) - DIL_BLOCK
    rel = kj[None, :] - qi[:, None]
    k_sub = (jnp.arange(nb, dtype=jnp.int32) * DIL_BLOCK)[:, None] + kj[None, :]
    valid = (jnp.abs(rel) <= half_steps)[None] & ((k_sub >= 0) & (k_sub < n_sub))[:, None, :]
    bias = jnp.transpose(bias_table[rel_bucket(rel * dilation)], (2, 0, 1))
    scores = jnp.where(valid, scores + bias[:, None], NEG_INF)
    lse = jax.nn.logsumexp(scores, axis=-1)
    p = jnp.exp(scores - lse[..., None])
    o = jnp.einsum('brhnqk,brhnke->brhnqe', p.astype(v.dtype), vb)
    o = o.reshape(b, dilation, h, lp, e)[:, :, :, :n_sub].transpose(0, 3, 1, 2, 4).reshape(b, s, h, e)
    lse = lse.reshape(b, dilation, h, lp)[..., :n_sub].transpose(0, 3, 1, 2).reshape(b, s, h)
    return o, lse


def dilated_mixer(q, k, v, qk_g, bias_table):
    b, s, _ = q.shape
    q = rms_norm(q.reshape(b, s, DIL_HEADS, HEAD_DIM), qk_g[0])
    k = rms_norm(k.reshape(b, s, DIL_HEADS, HEAD_DIM), qk_g[1])
    v = v.reshape(b, s, DIL_HEADS, HEAD_DIM)
    outs, lses = [], []
    for window, dilation in DIL_PATTERNS:
        o, l = dilated_branch(q, k, v, bias_table, dilation, window // (2 * dilation))
        outs.append(o)
        lses.append(l)
    wts = jax.nn.softmax(jnp.stack(lses, axis=0), axis=0).astype(q.dtype)
    o = jnp.sum(wts[..., None] * jnp.stack(outs, axis=0), axis=0)
    return o.reshape(b, s, DIL_HEADS * HEAD_DIM)


def gqa_mixer(q, k, v, qk_g, row_cos, row_sin, col_cos, col_sin):
    b, s, _ = q.shape
    q = rms_norm(q.reshape(b, s, GQA_HEADS, HEAD_DIM), qk_g[0])
    k = rms_norm(k.reshape(b, s, GQA_KV_HEADS, HEAD_DIM), qk_g[1])
    v = v.reshape(b, s, GQA_KV_HEADS, HEAD_DIM)
    q = axial_rope(q, row_cos, row_sin, col_cos, col_sin).reshape(b, s, GQA_KV_HEADS, GQA_GROUP, HEAD_DIM)
    k = axial_rope(k, row_cos, row_sin, col_cos, col_sin)
    scale = HEAD_DIM ** -0.5

    def block(start, qb):
        sc = jnp.einsum('bqhge,bkhe->bhgqk', qb, k).astype(jnp.float32) * scale
        p = jax.nn.softmax(sc, axis=-1).astype(v.dtype)
        return jnp.einsum('bhgqk,bkhe->bqhge', p, v)

    return sweep_query_blocks(block, q).reshape(b, s, GQA_HEADS * HEAD_DIM)


def diff_mixer(q, k, v, qk_g, lam_vecs, subln_g, bias_table, lambda_init):
    b, s, _ = q.shape
    q = rms_norm(q.reshape(b, s, DIFF_HEADS, 2, DIFF_QK_DIM), qk_g[0])
    k = rms_norm(k.reshape(b, s, DIFF_HEADS, 2, DIFF_QK_DIM), qk_g[1])
    v = v.reshape(b, s, DIFF_HEADS, DIFF_V_DIM)
    lv = lam_vecs.astype(jnp.float32)
    lam = jnp.exp(jnp.sum(lv[0] * lv[1])) - jnp.exp(jnp.sum(lv[2] * lv[3])) + lambda_init
    kpos = jnp.arange(s, dtype=jnp.int32)
    scale = DIFF_QK_DIM ** -0.5

    def block(start, qb):
        sc = jnp.einsum('bqhce,bkhce->bchqk', qb, k).astype(jnp.float32) * scale
        qpos = start + jnp.arange(Q_BLOCK, dtype=jnp.int32)
        bias = jnp.transpose(bias_table[rel_bucket(kpos[None, :] - qpos[:, None])], (2, 0, 1))
        p = jax.nn.softmax(sc + bias[None, None], axis=-1)
        attn = p[:, 0] - lam * p[:, 1]
        return jnp.einsum('bhqk,bkhe->bqhe', attn.astype(v.dtype), v)

    o = sweep_query_blocks(block, q)
    o = rms_norm(o, subln_g) * (1.0 - lambda_init)
    return o.reshape(b, s, DIFF_HEADS * DIFF_V_DIM)


def hier_moe(h, wg, bg, we, be, w_gate, w_up, w_down):
    b, s, d = h.shape
    t = h.reshape(b * s, d)
    n_tok = t.shape[0]
    g_prob = jax.nn.softmax(jnp.einsum('td,dg->tg', t, wg).astype(jnp.float32) + bg.astype(jnp.float32), axis=-1)
    g_w, g_idx = lax.top_k(g_prob, 1)
    e_logits = jnp.einsum('td,de->te', t, we).astype(jnp.float32) + be.astype(jnp.float32)
    e_logits = e_logits.reshape(n_tok, N_GROUPS, EXPERTS_PER_GROUP)
    sel = jnp.broadcast_to(g_idx[:, :, None], (n_tok, 1, EXPERTS_PER_GROUP))
    e_logits = jnp.take_along_axis(e_logits, sel, axis=1)[:, 0]
    e_w, e_idx = lax.top_k(jax.nn.softmax(e_logits, axis=-1), TOP_K)
    e_w = e_w / jnp.sum(e_w, axis=-1, keepdims=True)
    expert_id = g_idx * EXPERTS_PER_GROUP + e_idx
    gate = jnp.sum(jax.nn.one_hot(expert_id, N_EXPERTS, dtype=jnp.float32) * (g_w * e_w)[..., None], axis=1)
    y = jnp.zeros_like(t)
    for e in range(N_EXPERTS):
        hid = jax.nn.silu(t @ w_gate[e]) * (t @ w_up[e])
        y = y + gate[:, e:e + 1].astype(t.dtype) * (hid @ w_down[e])
    return y.reshape(b, s, d)


def setup_inputs(seed: int = 0) -> dict:
    key = jax.random.key(seed)
    ks = jax.random.split(key, 24)
    f32 = jnp.float32

    def nrm(k, shape, scale):
        return jax.random.normal(k, shape, f32) * scale

    def gain(k, shape):
        return 1.0 + 0.02 * jax.random.normal(k, shape, f32)

    return {
        'x': nrm(ks[0], (BATCH, SEQ, D_MODEL), 1.0),
        'rel_bias': nrm(ks[1], (NUM_BUCKETS, NUM_BIAS_HEADS), 0.5),
        'norm1_g': gain(ks[2], (DEPTH, D_MODEL)),
        'w_in': nrm(ks[3], (DEPTH, D_MODEL, IN_COLS), D_MODEL ** -0.5),
        'mla_q_norm_g': gain(ks[4], (DEPTH, MLA_Q_RANK)),
        'mla_kv_norm_g': gain(ks[5], (DEPTH, MLA_KV_RANK)),
        'mla_w_uq': nrm(ks[6], (DEPTH, MLA_Q_RANK, MLA_HEADS * MLA_QK_DIM), MLA_Q_RANK ** -0.5),
        'mla_w_ukv': nrm(ks[7], (DEPTH, MLA_KV_RANK, MLA_HEADS * (MLA_NOPE_DIM + MLA_V_DIM)), MLA_KV_RANK ** -0.5),
        'mla_qk_g': gain(ks[8], (DEPTH, 2, MLA_QK_DIM)),
        'dil_qk_g': gain(ks[9], (DEPTH, 2, HEAD_DIM)),
        'gqa_qk_g': gain(ks[10], (DEPTH, 2, HEAD_DIM)),
        'diff_qk_g': gain(ks[11], (DEPTH, 2, DIFF_QK_DIM)),
        'diff_lambda': nrm(ks[12], (DEPTH, 4, DIFF_QK_DIM), 0.1),
        'diff_subln_g': gain(ks[13], (DEPTH, DIFF_V_DIM)),
        'mix_beta': gain(ks[14], (DEPTH, D_MIX)),
        'w_out': nrm(ks[15], (DEPTH, D_MIX, D_MODEL), D_MIX ** -0.5),
        'norm2_g': gain(ks[16], (DEPTH, D_MODEL)),
        'router_group_w': nrm(ks[17], (DEPTH, D_MODEL, N_GROUPS), D_MODEL ** -0.5),
        'router_group_b': nrm(ks[18], (DEPTH, N_GROUPS), 0.01),
        'router_expert_w': nrm(ks[19], (DEPTH, D_MODEL, N_EXPERTS), D_MODEL ** -0.5),
        'router_expert_b': nrm(ks[20], (DEPTH, N_EXPERTS), 0.01),
        'expert_w_gate': nrm(ks[21], (DEPTH, N_EXPERTS, D_MODEL, D_FF_EXPERT), D_MODEL ** -0.5),
        'expert_w_up': nrm(ks[22], (DEPTH, N_EXPERTS, D_MODEL, D_FF_EXPERT), D_MODEL ** -0.5),
        'expert_w_down': nrm(ks[23], (DEPTH, N_EXPERTS, D_FF_EXPERT, D_MODEL), D_FF_EXPERT ** -0.5),
    }


def reference(x, rel_bias, norm1_g, w_in, mla_q_norm_g, mla_kv_norm_g, mla_w_uq, mla_w_ukv, mla_qk_g,
              dil_qk_g, gqa_qk_g, diff_qk_g, diff_lambda, diff_subln_g, mix_beta, w_out, norm2_g,
              router_group_w, router_group_b, router_expert_w, router_expert_b,
              expert_w_gate, expert_w_up, expert_w_down):
    b, s, _ = x.shape
    rows = s // GRID_W
    pos = jnp.arange(s, dtype=jnp.int32)
    row_pos = jnp.repeat(jnp.arange(rows, dtype=jnp.int32), GRID_W)
    col_pos = pos - row_pos * GRID_W
    mla_cos, mla_sin = rope_cos_sin(pos, MLA_ROPE_DIM)
    row_cos, row_sin = rope_cos_sin(row_pos, AXIAL_DIM)
    col_cos, col_sin = rope_cos_sin(col_pos, AXIAL_DIM)
    split_points = np.cumsum(IN_SIZES)[:-1].tolist()
    bias_b = rel_bias[:, :DIL_HEADS]
    bias_d = rel_bias[:, DIL_HEADS:]

    for layer in range(DEPTH):
        h = rms_norm(x, norm1_g[layer])
        proj = jnp.einsum('bsd,dc->bsc', h, w_in[layer])
        (a_cq, a_ckv, a_kr, b_q, b_k, b_v, c_q, c_k, c_v, d_q, d_k, d_v) = jnp.split(proj, split_points, axis=-1)
        y_a = mla_mixer(a_cq, a_ckv, a_kr, mla_q_norm_g[layer], mla_kv_norm_g[layer], mla_w_uq[layer],
                        mla_w_ukv[layer], mla_qk_g[layer], mla_cos, mla_sin)
        y_b = dilated_mixer(b_q, b_k, b_v, dil_qk_g[layer], bias_b)
        y_c = gqa_mixer(c_q, c_k, c_v, gqa_qk_g[layer], row_cos, row_sin, col_cos, col_sin)
        lambda_init = 0.8 - 0.6 * math.exp(-0.3 * layer)
        y_d = diff_mixer(d_q, d_k, d_v, diff_qk_g[layer], diff_lambda[layer], diff_subln_g[layer], bias_d, lambda_init)
        mixed = jnp.concatenate([rms_norm(y_a), rms_norm(y_b), rms_norm(y_c), y_d], axis=-1) * mix_beta[layer]
        x = x + jnp.einsum('bsm,md->bsd', mixed, w_out[layer])
        x = x + hier_moe(rms_norm(x, norm2_g[layer]), router_group_w[layer], router_group_b[layer],
                         router_expert_w[layer], router_expert_b[layer], expert_w_gate[layer],
                         expert_w_up[layer], expert_w_down[layer])
    return x
```

```python
import math
import contextlib
import numpy as np
import ml_dtypes
import concourse.bass as bass
import concourse.mybir as mybir
from concourse.bass_utils import run_bass_kernel_spmd

F32 = mybir.dt.float32
BF16 = mybir.dt.bfloat16
AF = mybir.ActivationFunctionType
ALU = mybir.AluOpType
AX = mybir.AxisListType

NCORES = 8
D = 1024
S = 2048
BATCH = 16
SEQ_PER_CORE = BATCH // NCORES
T = SEQ_PER_CORE * S
NT = T // 128
DEPTH = 2
IN_COLS = 2464
EPS = 1e-6
NE = 16
DFF = 512
STRIP_W = 3968
STRIP_OFF = 1920

C_CQ, C_CKV, C_KR = 0, 256, 384
C_BQ, C_BK, C_BV = 416, 672, 928
C_CQ2, C_CK2, C_CV2 = 1184, 1440, 1568
C_DQ, C_DK, C_DV = 1696, 1952, 2208
SL_AQ, SL_AK, SL_BQ, SL_BK, SL_CQ, SL_CK, SL_DQ, SL_DK = 0, 4, 8, 10, 12, 14, 16, 18
NSLOT = 20
V_A, V_B, V_C, V_D = 0, 256, 512, 640
NV = 896


class Op:
    __slots__ = ("eng", "fn", "deps", "signal", "semval", "chan", "chanval", "pos", "epoch")

    def __init__(self, eng, fn, chan=None):
        self.eng = eng
        self.fn = fn
        self.deps = []
        self.signal = False
        self.semval = None
        self.chan = chan
        self.chanval = None
        self.pos = 0


class Prog:
    ENGS = ("pe", "act", "dve", "pool", "sp")

    def __init__(self, nc):
        self.nc = nc
        self.ops = {e: [] for e in self.ENGS}
        self.writers = {}
        self.readers = {}
        self.chan_count = {}
        self.dma_since_barrier = {}
        self.epoch = 0
        self.chan_map = {}

    @staticmethod
    def _key(o):
        return (o.eng, o.chan)

    def _prune_add(self, lst, o):
        k = self._key(o)
        lst[:] = [x for x in lst if self._key(x) != k]
        lst.append(o)

    def op(self, eng, fn, reads=(), writes=(), accum=(), chan=None, extra_deps=()):
        assert fn is not None or not (reads or writes or accum)
        if chan is not None:
            m = self.chan_map.setdefault(self.epoch, {})
            if chan not in m:
                m[chan] = len(m)
            chan = m[chan]
        o = Op(eng, fn, chan)
        o.epoch = self.epoch
        deps = list(extra_deps)
        for r in reads:
            deps += self.writers.get(r, [])
        for w in tuple(writes) + tuple(accum):
            deps += self.writers.get(w, [])
            deps += self.readers.get(w, [])
        for r in reads:
            self._prune_add(self.readers.setdefault(r, []), o)
        for w in writes:
            self.writers[w] = [o]
            self.readers[w] = []
        for w in accum:
            self._prune_add(self.writers.setdefault(w, []), o)
        if chan is not None:
            self.chan_count[chan] = self.chan_count.get(chan, 0) + 16
            o.chanval = self.chan_count[chan]
            self.dma_since_barrier[chan] = o
        best = {}
        for d in deps:
            if d is o or d.fn is None:
                continue
            if eng == "pe" and d.eng == "pe" and d.chan is None:
                continue
            k = self._key(d)
            b = best.get(k)
            if b is None:
                best[k] = d
            elif d.chan is not None:
                if d.chanval > b.chanval:
                    best[k] = d
            elif d.pos > b.pos:
                best[k] = d
        o.deps = list(best.values())
        for d in o.deps:
            if d.chan is None:
                d.signal = True
        o.pos = len(self.ops[eng])
        self.ops[eng].append(o)
        return o

    def barrier(self):
        deps = []
        for e in self.ENGS:
            for x in reversed(self.ops[e]):
                if x.chan is None and x.fn is not None:
                    deps.append(x)
                    break
        deps += list(self.dma_since_barrier.values())
        self.dma_since_barrier = {}
        for e in self.ENGS:
            self.op(e, None, extra_deps=deps)
        self.writers = {}
        self.readers = {}
        self.epoch += 1

    def emit(self):
        nc = self.nc
        for e in self.ENGS:
            c = {}
            for o in self.ops[e]:
                if o.chan is None and o.signal:
                    c[o.epoch] = c.get(o.epoch, 0) + 1
                    o.semval = c[o.epoch]
        chans = sorted(self.chan_count.keys(), key=str)
        with contextlib.ExitStack() as st:
            used = set()
            for e in self.ENGS:
                for o in self.ops[e]:
                    if o.chan is None and o.signal:
                        used.add((e, o.epoch))
            esem = {k: st.enter_context(nc.semaphore("sem_%s_%d" % k)) for k in sorted(used)}
            print("[prog] semaphores: %d engine, %d dma channels" % (len(esem), len(chans)))
            csem = {c: st.enter_context(nc.semaphore("ch_%d" % i)) for i, c in enumerate(chans)}
            block = st.enter_context(nc.Block())

            def run(ename):
                def body(eng):
                    waited = {}
                    for o in self.ops[ename]:
                        for d in o.deps:
                            if d.chan is not None:
                                s, v = csem[d.chan], d.chanval
                            else:
                                s, v = esem[(d.eng, d.epoch)], d.semval
                            if waited.get(id(s), 0) >= v:
                                continue
                            waited[id(s)] = v
                            eng.wait_ge(s, v)
                        if o.fn is None:
                            continue
                        ins = o.fn(eng)
                        if o.chan is not None:
                            ins.then_inc(csem[o.chan], 16)
                        elif o.signal:
                            ins.then_inc(esem[(ename, o.epoch)], 1)
                return body

            block.tensor(run("pe"))
            block.scalar(run("act"))
            block.vector(run("dve"))
            block.gpsimd(run("pool"))
            block.sync(run("sp"))


class Arena:
    def __init__(self, tensor, ncols):
        self.t = tensor
        self.n = ncols
        self.off = 0

    def mark(self):
        return self.off

    def reset(self, m):
        self.off = m

    def take(self, shape, dtype):
        p = shape[0]
        nfree = int(np.prod(shape[1:]))
        nbytes = nfree * (4 if dtype == F32 else 2)
        ncol = (nbytes + 3) // 4
        ncol = (ncol + 7) // 8 * 8
        assert self.off + ncol <= self.n, ("arena overflow", self.off, ncol, self.n)
        a = self.t[0:p, self.off:self.off + ncol]
        self.off += ncol
        if dtype != F32:
            a = a.bitcast(dtype)
        a = a[:, 0:nfree]
        if len(shape) > 2:
            names = "abcd"[: len(shape) - 1]
            kw = {names[i]: shape[i + 1] for i in range(len(shape) - 2)}
            a = a.rearrange("p (%s) -> p %s" % (" ".join(names), " ".join(names)), **kw)
        return a


def bcast(ap, axis, shape):
    return ap.unsqueeze(axis).to_broadcast(list(shape))


def build_program(n_layers=DEPTH, stop_after=None, dbg=False):
    nc = bass.Bass("TRN2", target_bir_lowering=False)
    P = Prog(nc)

    def din(name, shape, dt=F32):
        return nc.dram_tensor(name, list(shape), dt, kind="ExternalInput").ap()

    def dscr(name, shape, dt):
        return nc.dram_tensor(name, list(shape), dt, kind="Internal").ap()

    x_in = din("x", [T, D])
    norm1_g = din("norm1_g", [DEPTH, D])
    w_in = din("w_in", [DEPTH, D, IN_COLS])
    mla_q_norm_g = din("mla_q_norm_g", [DEPTH, 256])
    mla_kv_norm_g = din("mla_kv_norm_g", [DEPTH, 128])
    mla_w_uq = din("mla_w_uq", [DEPTH, 256, 384])
    mla_w_ukv = din("mla_w_ukv", [DEPTH, 128, 512])
    mla_qk_g = din("mla_qk_g", [DEPTH, 2 * 96])
    dil_qk_g = din("dil_qk_g", [DEPTH, 2 * 64])
    gqa_qk_g = din("gqa_qk_g", [DEPTH, 2 * 64])
    diff_qk_g = din("diff_qk_g", [DEPTH, 2 * 32])
    diff_lambda = din("diff_lambda", [DEPTH, 4 * 32])
    diff_subln_g = din("diff_subln_g", [DEPTH, 64])
    mix_beta = din("mix_beta", [DEPTH, D])
    w_out = din("w_out", [DEPTH, D, D])
    norm2_g = din("norm2_g", [DEPTH, D])
    router_w = din("router_w", [DEPTH, D, 20])
    router_b = din("router_b", [DEPTH, 20])
    w_gate = din("expert_w_gate", [DEPTH, NE, D, DFF])
    w_up = din("expert_w_up", [DEPTH, NE, D, DFF])
    w_down = din("expert_w_down", [DEPTH, NE, DFF, D])
    rope_tab = din("rope_tab", [S, 6 * 32])
    strip_dil = din("strip_dil", [4, 128, STRIP_W])
    strip_dif = din("strip_dif", [4, 128, STRIP_W])
    strip_mult = din("strip_mult", [128, STRIP_W])
    out_d = nc.dram_tensor("out", [T, D], F32, kind="ExternalOutput").ap()

    QT = dscr("QT", [NSLOT, 128, T], BF16)
    VS = dscr("VS", [T, NV], BF16)
    Y = dscr("Y", [T, D], F32)
    X1 = dscr("X1", [T, D], F32)
    X2 = dscr("X2", [T, D], F32)
    SD = dscr("SD", [8, 128, STRIP_W], BF16)
    dbg_out = {}

    ARENA_COLS = 196 * 256
    arena_t = nc.alloc_sbuf_tensor("arena", [128, ARENA_COLS], F32)
    AR = Arena(arena_t, ARENA_COLS)
    psum_t = nc.alloc_psum_tensor("psum", [128, 4096], F32)

    def bank(i):
        return psum_t[:, i * 512:(i + 1) * 512]

    def bank_bf(i):
        return psum_t[:, i * 512:(i + 1) * 512].bitcast(BF16)

    def dma(q, out, in_, reads=(), writes=(), accum=(), chan=None, slow=False):
        if slow:
            fn = lambda e: e.dma_start(out=out, in_=in_, allow_slow_non_contiguous=True)
        else:
            fn = lambda e: e.dma_start(out=out, in_=in_)
        return P.op(q, fn, reads=reads, writes=writes, accum=accum, chan=chan)

    def tt(eng, out, in0, in1, op, reads, writes, accum=()):
        return P.op(eng, lambda e: e.tensor_tensor(out=out, in0=in0, in1=in1, op=op), reads=reads, writes=writes, accum=accum)

    def ts(eng, out, in0, s1, s2, op0, op1, reads, writes):
        if s2 is None:
            return P.op(eng, lambda e: e.tensor_scalar(out=out, in0=in0, scalar1=s1, scalar2=None, op0=op0),
                        reads=reads, writes=writes)
        return P.op(eng, lambda e: e.tensor_scalar(out=out, in0=in0, scalar1=s1, scalar2=s2, op0=op0, op1=op1),
                    reads=reads, writes=writes)

    def act(out, in_, func, reads, writes, scale=1.0, bias=None, accum_out=None, accum=()):
        def fn(e):
            kw = {}
            if bias is not None:
                kw["bias"] = bias
            if accum_out is not None:
                kw["accum_out"] = accum_out
            return e.activation(out=out, in_=in_, func=func, scale=scale, **kw)
        return P.op("act", fn, reads=reads, writes=writes, accum=accum)

    def red(out, in_, op, reads, writes, axis=AX.X, accum=()):
        return P.op("dve", lambda e: e.tensor_reduce(out=out, in_=in_, axis=axis, op=op), reads=reads, writes=writes, accum=accum)

    def recip(out, in_, reads, writes):
        return P.op("dve", lambda e: e.reciprocal(out=out, in_=in_), reads=reads, writes=writes)

    def rstd_chain(ssq, inv_e, n, tag, scratch):
        if isinstance(inv_e, float):
            act(ssq, ssq, AF.Sqrt, [tag], [tag], scale=inv_e, bias=eps_t[:, 0:1])
        else:
            tt("dve", ssq, ssq, inv_e, ALU.mult, [tag], [tag])
            act(ssq, ssq, AF.Sqrt, [tag], [tag], bias=eps_t[:, 0:1])
        recip(ssq, ssq, [tag], [tag])

    ident_f = AR.take([128, 128], F32)
    ident_b = AR.take([128, 128], BF16)
    eps_t = AR.take([128, 8], F32)
    gv = AR.take([128, DEPTH, 32], F32)
    GV_G1, GV_G2, GV_BETA, GV_QN, GV_KVN, GV_SUB = 0, 8, 16, 24, 26, 27
    gbt = AR.take([128, DEPTH, 704], F32)
    GB_MLA, GB_DIL, GB_GQA, GB_DIF, GB_LAM, GB_RB = 0, 192, 320, 448, 512, 640
    neglam = AR.take([128, DEPTH], F32)
    inv_e1 = AR.take([128, 33], F32)

    P.op("pool", lambda e: e.memset(ident_f, 1.0), writes=["ident_f"])
    P.op("pool", lambda e: e.affine_select(out=ident_f, in_=ident_f, pattern=[[-1, 128]], compare_op=ALU.is_equal,
                                           fill=0.0, base=0, channel_multiplier=1), reads=["ident_f"], writes=["ident_f"])
    P.op("dve", lambda e: e.tensor_copy(out=ident_b, in_=ident_f), reads=["ident_f"], writes=["ident_b"])
    P.op("dve", lambda e: e.memset(eps_t, EPS), writes=["eps"])
    P.op("dve", lambda e: e.memset(inv_e1[:, 0:1], 1.0 / 256), writes=["inv_e1"])
    P.op("dve", lambda e: e.memset(inv_e1[:, 1:2], 1.0 / 128), accum=["inv_e1"])
    P.op("dve", lambda e: e.memset(inv_e1[:, 2:3], 1.0), accum=["inv_e1"])
    P.op("dve", lambda e: e.memset(inv_e1[:, 3:17], 1.0 / 64), accum=["inv_e1"])
    P.op("dve", lambda e: e.memset(inv_e1[:, 17:33], 1.0 / 32), accum=["inv_e1"])
    for L in range(DEPTH):
        for (src, off, n) in ((norm1_g, GV_G1, 8), (norm2_g, GV_G2, 8), (mix_beta, GV_BETA, 8), (mla_q_norm_g, GV_QN, 2),
                              (mla_kv_norm_g, GV_KVN, 1)):
            dma("sp", gv[:, L, off:off + n], src[L].rearrange("(c p) -> p c", p=128), accum=["gv"], chan="c0", slow=True)
        for h in range(2):
            dma("sp", gv[64 * h:64 * h + 64, L, GV_SUB:GV_SUB + 1], diff_subln_g[L].rearrange("(p c) -> p c", c=1),
                accum=["gv"], chan="c0", slow=True)
        for (src, off, n) in ((mla_qk_g, GB_MLA, 192), (dil_qk_g, GB_DIL, 128), (gqa_qk_g, GB_GQA, 128),
                              (diff_qk_g, GB_DIF, 64), (diff_lambda, GB_LAM, 128), (router_b, GB_RB, 20)):
            dma("sp", gbt[:, L, off:off + n], src[L].partition_broadcast(128), accum=["gbt"], chan="c0")
    for L in range(DEPTH):
        li = 0.8 - 0.6 * math.exp(-0.3 * L)
        P.op("dve", (lambda L=L, li=li: (lambda e: e.tensor_scalar(
            out=gv[:, L, GV_BETA + 6:GV_BETA + 8], in0=gv[:, L, GV_BETA + 6:GV_BETA + 8],
            scalar1=gv[:, L, GV_SUB:GV_SUB + 1], scalar2=1.0 - li, op0=ALU.mult, op1=ALU.mult)))(),
            reads=["gv"], writes=["gv"])
        lt = AR.take([128, 64], F32)
        lv = gbt[:, L, GB_LAM:GB_LAM + 128].rearrange("p (a b c) -> p a b c", a=2, b=2)
        tt("dve", lt.rearrange("p (a c) -> p a c", a=2), lv[:, :, 0, :], lv[:, :, 1, :], ALU.mult, ["gbt"], [("lt", L)])
        ls = AR.take([128, 2], F32)
        red(ls, lt.rearrange("p (a c) -> p a c", a=2), ALU.add, [("lt", L)], [("ls", L)])
        act(ls, ls, AF.Exp, [("ls", L)], [("ls", L)])
        tt("dve", neglam[:, L:L + 1], ls[:, 1:2], ls[:, 0:1], ALU.subtract, [("ls", L)], [("nl", L)])
        ts("dve", neglam[:, L:L + 1], neglam[:, L:L + 1], -li, None, ALU.add, None, [("nl", L)], [("nl", L)])
    m0 = AR.mark()
    smul = AR.take([128, STRIP_W], F32)
    dma("sp", smul, strip_mult, writes=["smul"], chan="c1")
    sfs = [AR.take([128, STRIP_W], F32) for _ in range(2)]
    sbs = [AR.take([128, STRIP_W], BF16) for _ in range(2)]
    for i in range(8):
        sf = sfs[i % 2]
        sb = sbs[i % 2]
        src = strip_dil[i] if i < 4 else strip_dif[i - 4]
        dma("sp", sf, src, writes=[("sf", i % 2)], chan=("c2", i % 2))
        act(sf, sf, AF.Exp, [("sf", i % 2)], [("sf", i % 2)])
        if i < 4:
            tt("dve", sb, sf, smul, ALU.mult, [("sf", i % 2), "smul"], [("sb", i % 2)])
        else:
            P.op("dve", (lambda a=sb, b=sf: (lambda e: e.tensor_copy(out=a, in_=b)))(), reads=[("sf", i % 2)],
                 writes=[("sb", i % 2)])
        dma("sp", SD[i], sb, reads=[("sb", i % 2)], accum=["SD"], chan=("c3", i % 2))
    P.barrier()
    AR.reset(m0)
    persist_mark = AR.mark()

    def phase_A(L, x_src):
        TB = 2
        m = AR.mark()
        w_in_sb = AR.take([128, 8, IN_COLS], BF16)
        w_uq_sb = AR.take([128, 2, 384], BF16)
        w_ukv_sb = AR.take([128, 512], BF16)
        for c in range(8):
            dma("pool", w_in_sb[:, c, :], w_in[L, c * 128:(c + 1) * 128, :], accum=["w_in"], chan="a_w")
        for c in range(2):
            dma("pool", w_uq_sb[:, c, :], mla_w_uq[L, c * 128:(c + 1) * 128, :], accum=["w_in"], chan="a_w")
        dma("pool", w_ukv_sb, mla_w_ukv[L], accum=["w_in"], chan="a_w")
        xt = [AR.take([128, TB, D], F32) for _ in range(2)]
        hb1 = AR.take([128, TB, D], BF16)
        hb = [hb1, hb1]
        hT = [AR.take([128, 8, 128], BF16) for _ in range(2)]
        ss1 = [AR.take([128, 2 * TB], F32) for _ in range(2)]
        junk = AR.take([128, D], F32)
        pj = AR.take([128, TB, IN_COLS], F32)
        sq = AR.take([128, TB, C_DV], F32)
        ssg = AR.take([128, TB, 33], F32)
        cqn = AR.take([128, TB, 384], BF16)
        cT = AR.take([128, 3, 128], BF16)
        q2 = AR.take([128, TB, 384], F32)
        kv2 = AR.take([128, TB, 512], F32)
        ss2 = AR.take([128, TB, 8], F32)
        qk_a = AR.take([128, TB, 8, 96], F32)
        qk_c = AR.take([128, TB, 6, 64], F32)
        rt1 = AR.take([128, TB, 12, 32], F32)
        rt2 = AR.take([128, TB, 12, 32], F32)
        rope = [AR.take([128, TB, 6, 32], F32) for _ in range(2)]
        qktm = AR.take([128, TB, NSLOT, 128], BF16)
        vst = AR.take([128, TB, NV], BF16)
        qst = [AR.take([128, NSLOT, 512], BF16) for _ in range(2)]
        P.op("pool", lambda e: e.memset(qktm, 0.0), writes=["qktm"])
        gb = lambda off, n: gbt[:, L, off:off + n]

        nbatch = NT // TB
        for b in range(nbatch):
            bp = b % 2
            tok0 = b * TB * 128
            R = lambda name: (name, bp)
            dma("sp", xt[bp], x_src[tok0:tok0 + TB * 128, :].rearrange("(t p) d -> p t d", p=128), writes=[R("xt")],
                chan=("a_x", bp))
            tpos = (tok0 % S)
            dma("sp", rope[bp].rearrange("p t a b -> p t (a b)"),
                rope_tab[tpos:tpos + TB * 128, :].rearrange("(t p) c -> p t c", p=128), writes=[R("rope")], chan=("a_r", bp))
            for t in range(TB):
                act(junk, xt[bp][:, t, :], AF.Square, [R("xt")], [R("ss1")] if t == 0 else [], accum_out=ss1[bp][:, t:t + 1],
                    accum=[] if t == 0 else [R("ss1")])
            rstd_chain(ss1[bp][:, 0:TB], 1.0 / D, TB, R("ss1"), None)
            for t in range(TB):
                act(hb[bp][:, t, :], xt[bp][:, t, :], AF.Copy, [R("xt"), R("ss1")], [("hb", t)], scale=ss1[bp][:, t:t + 1])
            for t in range(TB):
                tp = t % 2
                tokt = tok0 + t * 128
                def trf(e, t=t, bp=bp):
                    ins = None
                    for c in range(8):
                        ins = e.transpose(out=bank_bf(7)[:, c * 128:(c + 1) * 128], in_=hb[bp][:, t, c * 128:(c + 1) * 128],
                                          identity=ident_b)
                    return ins
                P.op("pe", trf, reads=[("hb", t)], writes=["ps7"])
                tt("dve", hT[tp], bank_bf(7).rearrange("p (c k) -> p c k", c=8),
                   bcast(gv[:, L, GV_G1:GV_G1 + 8], 2, [128, 8, 128]), ALU.mult, ["ps7"], [("hT", tp)])
                def mmf(e, tp=tp):
                    ins = None
                    for j in range(5):
                        w = 512 if j < 4 else IN_COLS - 2048
                        for c in range(8):
                            ins = e.matmul(bank(j)[:, 0:w], lhsT=hT[tp][:, c, :], rhs=w_in_sb[:, c, j * 512:j * 512 + w],
                                           start=(c == 0), stop=(c == 7))
                    return ins
                P.op("pe", mmf, reads=[("hT", tp), "w_in"], writes=["ps0", "ps1", "ps2", "ps3", "ps4"])
                for j in range(5):
                    w = 512 if j < 4 else IN_COLS - 2048
                    eng = "act" if j % 2 == 0 else "dve"
                    if eng == "act":
                        act(pj[:, t, j * 512:j * 512 + w], bank(j)[:, 0:w], AF.Copy, ["ps%d" % j], [], accum=[("pj", t)])
                    else:
                        P.op("dve", (lambda j=j, w=w, t=t: (lambda e: e.tensor_copy(out=pj[:, t, j * 512:j * 512 + w],
                                                                                   in_=bank(j)[:, 0:w])))(),
                             reads=["ps%d" % j], accum=[("pj", t)])
            PJ = [("pj", t) for t in range(TB)]
            act(sq[:, :, 0:C_BV], pj[:, :, 0:C_BV], AF.Square, PJ, ["sq"])
            P.op("act", lambda e: e.activation(out=sq[:, :, C_CQ2:C_CV2], in_=pj[:, :, C_CQ2:C_CV2], func=AF.Square),
                 reads=PJ, accum=["sq"])
            P.op("act", lambda e: e.activation(out=sq[:, :, C_DQ:C_DV], in_=pj[:, :, C_DQ:C_DV], func=AF.Square),
                 reads=PJ, accum=["sq"])
            red(ssg[:, :, 0:1], sq[:, :, 0:256].rearrange("p t (a e) -> p t a e", a=1), ALU.add, ["sq"], ["ssg"])
            P.op("dve", lambda e: e.tensor_reduce(out=ssg[:, :, 1:2], in_=sq[:, :, 256:384].rearrange("p t (a e) -> p t a e", a=1),
                                                  axis=AX.X, op=ALU.add), reads=["sq"], accum=["ssg"])
            P.op("dve", lambda e: e.tensor_reduce(out=ssg[:, :, 2:3], in_=sq[:, :, 384:416].rearrange("p t (a e) -> p t a e", a=1),
                                                  axis=AX.X, op=ALU.add), reads=["sq"], accum=["ssg"])
            P.op("dve", lambda e: e.tensor_reduce(out=ssg[:, :, 3:11], in_=sq[:, :, C_BQ:C_BV].rearrange("p t (a e) -> p t a e", a=8),
                                                  axis=AX.X, op=ALU.add), reads=["sq"], accum=["ssg"])
            P.op("dve", lambda e: e.tensor_reduce(out=ssg[:, :, 11:17], in_=sq[:, :, C_CQ2:C_CV2].rearrange("p t (a e) -> p t a e", a=6),
                                                  axis=AX.X, op=ALU.add), reads=["sq"], accum=["ssg"])
            P.op("dve", lambda e: e.tensor_reduce(out=ssg[:, :, 17:33], in_=sq[:, :, C_DQ:C_DV].rearrange("p t (a e) -> p t a e", a=16),
                                                  axis=AX.X, op=ALU.add), reads=["sq"], accum=["ssg"])
            sskr = ss1[bp][:, TB:2 * TB]
            P.op("dve", (lambda sskr=sskr: (lambda e: e.tensor_copy(out=sskr, in_=ssg[:, :, 2])))(), reads=["ssg"], writes=[R("sskr")])
            rstd_chain(ssg, bcast(inv_e1, 1, [128, TB, 33]), 33, "ssg", None)
            tt("dve", cqn[:, :, 0:256], pj[:, :, 0:256], ssg[:, :, 0:1].to_broadcast([128, TB, 256]), ALU.mult,
               PJ + ["ssg"], ["cqn"])
            P.op("dve", lambda e: e.tensor_tensor(out=cqn[:, :, 256:384], in0=pj[:, :, 256:384],
                                                  in1=ssg[:, :, 1:2].to_broadcast([128, TB, 128]), op=ALU.mult),
                 reads=PJ + ["ssg"], accum=["cqn"])
            for t in range(TB):
                def trc(e, t=t):
                    ins = None
                    for c in range(3):
                        ins = e.transpose(out=bank_bf(7)[:, c * 128:(c + 1) * 128], in_=cqn[:, t, c * 128:(c + 1) * 128],
                                          identity=ident_b)
                    return ins
                P.op("pe", trc, reads=["cqn"], writes=["ps7"])
                tt("dve", cT, bank_bf(7)[:, 0:384].rearrange("p (c k) -> p c k", c=3),
                   bcast(gv[:, L, GV_QN:GV_QN + 3], 2, [128, 3, 128]), ALU.mult, ["ps7"], ["cT"])
                def mm2(e):
                    for c in range(2):
                        e.matmul(bank(5)[:, 0:384], lhsT=cT[:, c, :], rhs=w_uq_sb[:, c, :], start=(c == 0), stop=(c == 1))
                    return e.matmul(bank(6), lhsT=cT[:, 2, :], rhs=w_ukv_sb, start=True, stop=True)
                P.op("pe", mm2, reads=["cT", "w_in"], writes=["ps5", "ps6"])
                act(q2[:, t, :], bank(5)[:, 0:384], AF.Copy, ["ps5"], [("q2", t)])
                P.op("dve", (lambda t=t: (lambda e: e.tensor_copy(out=kv2[:, t, :], in_=bank(6))))(), reads=["ps6"],
                     writes=[("kv2", t)])
            Q2 = [("q2", t) for t in range(TB)]
            KV2 = [("kv2", t) for t in range(TB)]
            kv4 = kv2.rearrange("p t (h e) -> p t h e", h=4)
            act(sq[:, :, 0:384], q2, AF.Square, Q2, ["sq"])
            P.op("act", lambda e: e.activation(out=sq[:, :, 384:896], in_=kv2, func=AF.Square), reads=KV2, accum=["sq"])
            red(ss2[:, :, 0:4], sq[:, :, 0:384].rearrange("p t (h e) -> p t h e", h=4), ALU.add, ["sq"], ["ss2"])
            P.op("dve", lambda e: e.tensor_reduce(out=ss2[:, :, 4:8],
                                                  in_=sq[:, :, 384:896].rearrange("p t (h e) -> p t h e", h=4)[:, :, :, 0:64],
                                                  axis=AX.X, op=ALU.add), reads=["sq"], accum=["ss2"])
            tt("dve", ss2[:, :, 4:8], ss2[:, :, 4:8], bcast(sskr, 2, [128, TB, 4]), ALU.add, ["ss2", R("sskr")], ["ss2"])
            rstd_chain(ss2, 1.0 / 96, 8, "ss2", None)
            qa4 = qk_a.rearrange("p t h e -> p (t h) e")
            tt("dve", qk_a[:, :, 0:4, :], q2.rearrange("p t (h e) -> p t h e", h=4),
               bcast(ss2[:, :, 0:4], 3, [128, TB, 4, 96]), ALU.mult, Q2 + ["ss2"], ["qk_a"])
            P.op("dve", lambda e: e.tensor_tensor(out=qk_a[:, :, 4:8, 0:64], in0=kv4[:, :, :, 0:64],
                                                  in1=bcast(ss2[:, :, 4:8], 3, [128, TB, 4, 64]), op=ALU.mult),
                 reads=KV2 + ["ss2"], accum=["qk_a"])
            for t in range(TB):
                P.op("dve", (lambda t=t: (lambda e: e.tensor_tensor(
                    out=qk_a[:, t, 4:8, 64:96], in0=bcast(pj[:, t, C_KR:C_KR + 32], 1, [128, 4, 32]),
                    in1=bcast(ss2[:, t, 4:8], 2, [128, 4, 32]), op=ALU.mult)))(), reads=PJ + ["ss2"], accum=["qk_a"])
            for t in range(TB):
                g2v = gb(GB_MLA, 192).rearrange("p (a e) -> p a e", a=2)
                P.op("dve", (lambda t=t, g2v=g2v: (lambda e: e.tensor_tensor(
                    out=qk_a[:, t].rearrange("p (a h) e -> p a h e", a=2), in0=qk_a[:, t].rearrange("p (a h) e -> p a h e", a=2),
                    in1=bcast(g2v, 2, [128, 2, 4, 96]), op=ALU.mult)))(), reads=["qk_a", "gbt"], writes=["qk_a"] if t == TB - 1 else [],
                    accum=[] if t == TB - 1 else ["qk_a"])
            def rope_apply(x4, nh, cos4, sin4, tag, tmp1, tmp2):
                tt("dve", tmp1, x4, cos4, ALU.mult, [tag, R("rope")], [tag + "_t1"])
                tt("dve", tmp2[:, :, :, 0:16], x4[:, :, :, 16:32], sin4[:, :, :, 0:16], ALU.mult, [tag, R("rope")], [tag + "_t2"])
                P.op("dve", lambda e: e.tensor_tensor(out=tmp2[:, :, :, 16:32], in0=x4[:, :, :, 0:16], in1=sin4[:, :, :, 16:32],
                                                      op=ALU.mult), reads=[tag, R("rope")], accum=[tag + "_t2"])
                tt("dve", x4, tmp1, tmp2, ALU.add, [tag + "_t1", tag + "_t2"], [tag])
            rp = rope[bp]
            rope_apply(qk_a[:, :, :, 64:96], 8, bcast(rp[:, :, 0, :], 2, [128, TB, 8, 32]), bcast(rp[:, :, 1, :], 2, [128, TB, 8, 32]),
                       "qk_a", rt1[:, :, 0:8, :], rt2[:, :, 0:8, :])
            P.op("act", lambda e: e.activation(out=qktm[:, :, SL_AQ:SL_AQ + 8, 0:96], in_=qk_a, func=AF.Copy), reads=["qk_a"],
                 accum=["qktm"])
            dq = pj[:, :, C_BQ:C_BV].rearrange("p t (h e) -> p t h e", h=8)
            dst = qktm[:, :, SL_BQ:SL_BQ + 4, :].rearrange("p t s (h e) -> p t (s h) e", h=2)
            tt("dve", dst, dq, bcast(ssg[:, :, 3:11], 3, [128, TB, 8, 64]), ALU.mult, PJ + ["ssg"], [], accum=["qktm"])
            for t in range(TB):
                gd = gb(GB_DIL, 128).rearrange("p (a e) -> p a e", a=2)
                dv = qktm[:, t, SL_BQ:SL_BQ + 4, :].rearrange("p (a s) (h e) -> p a (s h) e", a=2, h=2)
                P.op("dve", (lambda dv=dv, gd=gd: (lambda e: e.tensor_tensor(out=dv, in0=dv, in1=bcast(gd, 2, [128, 2, 4, 64]),
                                                                             op=ALU.mult)))(), reads=["qktm", "gbt"], accum=["qktm"])
            fq = pj[:, :, C_DQ:C_DV].rearrange("p t (h e) -> p t h e", h=16)
            fst = qktm[:, :, SL_DQ:SL_DQ + 4, :].rearrange("p t s (h e) -> p t (s h) e", h=4)
            P.op("dve", lambda e: e.tensor_tensor(out=fst, in0=fq, in1=bcast(ssg[:, :, 17:33], 3, [128, TB, 16, 32]), op=ALU.mult),
                 reads=PJ + ["ssg"], accum=["qktm"])
            for t in range(TB):
                gf = gb(GB_DIF, 64).rearrange("p (a e) -> p a e", a=2)
                fv = qktm[:, t, SL_DQ:SL_DQ + 4, :].rearrange("p (a s) (h e) -> p a (s h) e", a=2, h=4)
                P.op("dve", (lambda fv=fv, gf=gf: (lambda e: e.tensor_tensor(out=fv, in0=fv, in1=bcast(gf, 2, [128, 2, 8, 32]),
                                                                             op=ALU.mult)))(), reads=["qktm", "gbt"], accum=["qktm"])
            cq = pj[:, :, C_CQ2:C_CV2].rearrange("p t (h e) -> p t h e", h=6)
            tt("dve", qk_c, cq, bcast(ssg[:, :, 11:17], 3, [128, TB, 6, 64]), ALU.mult, PJ + ["ssg"], ["qk_c"])
            ggq = gb(GB_GQA, 64)
            ggk = gb(GB_GQA + 64, 64)
            tt("dve", qk_c[:, :, 0:4, :], qk_c[:, :, 0:4, :], bcast(bcast(ggq, 1, [128, 4, 64]), 1, [128, TB, 4, 64]), ALU.mult,
               ["qk_c", "gbt"], ["qk_c"])
            tt("dve", qk_c[:, :, 4:6, :], qk_c[:, :, 4:6, :], bcast(bcast(ggk, 1, [128, 2, 64]), 1, [128, TB, 2, 64]), ALU.mult,
               ["qk_c", "gbt"], ["qk_c"])
            for t in range(TB):
                xc = qk_c[:, t].rearrange("p h (a e) -> p h a e", a=2)
                cosv = bcast(rp[:, t, 2:6, :].rearrange("p (a b) e -> p a b e", b=2)[:, :, 0, :], 1, [128, 6, 2, 32])
                sinv = bcast(rp[:, t, 2:6, :].rearrange("p (a b) e -> p a b e", b=2)[:, :, 1, :], 1, [128, 6, 2, 32])
                t1 = rt1[:, t].rearrange("p (h a) e -> p h a e", a=2)
                t2 = rt2[:, t].rearrange("p (h a) e -> p h a e", a=2)
                tag = "qk_c"
                last = (t == TB - 1)
                P.op("dve", (lambda t1=t1, xc=xc, cosv=cosv: (lambda e: e.tensor_tensor(out=t1, in0=xc, in1=cosv, op=ALU.mult)))(),
                     reads=[tag, R("rope")], writes=[("ct1", t)])
                P.op("dve", (lambda t2=t2, xc=xc, sinv=sinv: (lambda e: e.tensor_tensor(
                    out=t2[:, :, :, 0:16], in0=xc[:, :, :, 16:32], in1=sinv[:, :, :, 0:16], op=ALU.mult)))(),
                    reads=[tag, R("rope")], writes=[("ct2", t)])
                P.op("dve", (lambda t2=t2, xc=xc, sinv=sinv: (lambda e: e.tensor_tensor(
                    out=t2[:, :, :, 16:32], in0=xc[:, :, :, 0:16], in1=sinv[:, :, :, 16:32], op=ALU.mult)))(),
                    reads=[tag, R("rope")], accum=[("ct2", t)])
                qd = qktm[:, t, SL_CQ:SL_CQ + 2, :].rearrange("p s (h a e) -> p (s h) a e", h=2, a=2)
                kd = qktm[:, t, SL_CK:SL_CK + 2, :].rearrange("p s (d f) -> p s d f", d=2)
                P.op("dve", (lambda qd=qd, t1=t1, t2=t2: (lambda e: e.tensor_tensor(out=qd, in0=t1[:, 0:4], in1=t2[:, 0:4], op=ALU.add)))(),
                     reads=[("ct1", t), ("ct2", t)], accum=["qktm"])
                k1 = bcast(rt1[:, t, 8:12, :].rearrange("p (h a) e -> p h (a e)", a=2), 2, [128, 2, 2, 64])
                k2 = bcast(rt2[:, t, 8:12, :].rearrange("p (h a) e -> p h (a e)", a=2), 2, [128, 2, 2, 64])
                P.op("dve", (lambda kd=kd, k1=k1, k2=k2: (lambda e: e.tensor_tensor(out=kd, in0=k1, in1=k2, op=ALU.add)))(),
                     reads=[("ct1", t), ("ct2", t)], accum=["qktm"])
            P.op("act", lambda e: e.activation(out=vst[:, :, V_A:V_A + 256].rearrange("p t (h e) -> p t h e", h=4),
                                               in_=kv4[:, :, :, 64:128], func=AF.Copy), reads=KV2, writes=["vst"])
            P.op("act", lambda e: e.activation(out=vst[:, :, V_B:V_B + 256], in_=pj[:, :, C_BV:C_BV + 256], func=AF.Copy),
                 reads=PJ, accum=["vst"])
            P.op("act", lambda e: e.activation(out=vst[:, :, V_C:V_C + 128], in_=pj[:, :, C_CV2:C_CV2 + 128], func=AF.Copy),
                 reads=PJ, accum=["vst"])
            P.op("act", lambda e: e.activation(out=vst[:, :, V_D:V_D + 256], in_=pj[:, :, C_DV:C_DV + 256], func=AF.Copy),
                 reads=PJ, accum=["vst"])
            dma("sp", VS[tok0:tok0 + TB * 128, :].rearrange("(t p) c -> p t c", p=128), vst, reads=["vst"], accum=["VS"],
                chan="a_vs")
            qb = (b // 2) % 2
            for t in range(TB):
                col0 = ((b % 2) * TB + t) * 128
                for g3 in range(3):
                    s0 = g3 * 8
                    ns = min(8, NSLOT - s0)
                    def trq(e, t=t, s0=s0, ns=ns, g3=g3):
                        ins = None
                        for s_ in range(ns):
                            ins = e.transpose(out=bank_bf(5 + g3)[:, s_ * 128:(s_ + 1) * 128], in_=qktm[:, t, s0 + s_, :],
                                              identity=ident_b)
                        return ins
                    P.op("pe", trq, reads=["qktm"], writes=["ps%d" % (5 + g3)])
                    src = bank_bf(5 + g3)[:, 0:ns * 128].rearrange("p (s k) -> p s k", s=ns)
                    dstq = qst[qb][:, s0:s0 + ns, col0:col0 + 128]
                    if g3 == 1:
                        P.op("act", (lambda dstq=dstq, src=src: (lambda e: e.activation(out=dstq, in_=src, func=AF.Copy)))(),
                             reads=["ps%d" % (5 + g3)], accum=[("qst", qb)])
                    else:
                        P.op("dve", (lambda dstq=dstq, src=src: (lambda e: e.tensor_copy(out=dstq, in_=src)))(),
                             reads=["ps%d" % (5 + g3)], accum=[("qst", qb)])
            if b % 2 == 1:
                tq0 = (b - 1) * TB * 128
                dma("sp", QT[:, :, tq0:tq0 + 512].rearrange("s p k -> p s k"), qst[qb], reads=[("qst", qb)], accum=["QT"],
                    chan=("a_qt", qb))
        P.barrier()
        AR.reset(m)

    def phase_B(L):
        m = AR.mark()
        vsb = [AR.take([128, 16, 4, 128], BF16) for _ in range(2)]
        ot = [AR.take([128, 512], F32) for _ in range(2)]
        for i in range(2):
            P.op("pool", (lambda v: (lambda e: e.memset(v, 1.0)))(vsb[i]), writes=[("vsb", i)])
        qT = [AR.take([128, 4, S], BF16) for _ in range(2)]
        kT = [AR.take([128, 4, S], BF16) for _ in range(2)]
        strips = AR.take([128, 4, STRIP_W], BF16)
        pt = [AR.take([128, 512], BF16) for _ in range(6)]
        SBANKS = (0, 1, 2, 5, 6)
        ybuf = [AR.take([128, 4, 256], F32) for _ in range(2)]
        rec = AR.take([128, 8], F32)
        t0b = AR.take([128, 4, 64], F32)
        t1b = AR.take([128, 4, 64], F32)

        mixers = [("A", SL_AQ, 4, SL_AK, 4, V_A, 4, None),
                  ("C", SL_CQ, 2, SL_CK, 2, V_C, 2, None),
                  ("B", SL_BQ, 2, SL_BK, 2, V_B, 4, 0),
                  ("D", SL_DQ, 2, SL_DK, 2, V_D, 4, 4)]
        ycol = {"A": 0, "B": 256, "C": 512, "D": 768}
        units = [(mx, sq_) for mx in mixers for sq_ in range(SEQ_PER_CORE)]
        state = {"step": 0, "ob": 0, "yb": 0, "ot": 0}
        cur_strip = [None]

        def load_unit(u, par):
            (name, qs, nq, ks, nk, vc, nvh, sbase), sq_ = u
            tk0 = sq_ * S
            for i in range(nq):
                dma("sp", qT[par][:, i, :], QT[qs + i, :, tk0:tk0 + S], reads=["QT"], accum=[("qT", par)], chan=("b_q", par))
            if name == "D":
                P.op("pool", (lambda a=kT[par]: (lambda e: e.memset(a, 0.0)))(), writes=[("kT", par)])
                for i in range(2):
                    for r0 in (0, 64):
                        dma("sp", kT[par][r0:r0 + 32, i, :], QT[ks + i, r0:r0 + 32, tk0:tk0 + S], reads=["QT"],
                            accum=[("kT", par)], chan=("b_k", par))
                        dma("sp", kT[par][r0 + 32:r0 + 64, 2 + i, :], QT[ks + i, r0 + 32:r0 + 64, tk0:tk0 + S], reads=["QT"],
                            accum=[("kT", par)], chan=("b_k", par))
            else:
                for i in range(nk):
                    dma("sp", kT[par][:, i, :], QT[ks + i, :, tk0:tk0 + S], reads=["QT"], accum=[("kT", par)], chan=("b_k", par))
            for h in range(nvh):
                dma("sp", vsb[par][:, :, h, 0:64],
                    VS[tk0:tk0 + S, vc + h * 64:vc + (h + 1) * 64].rearrange("(c p) e -> p c e", p=128),
                    reads=["VS"], accum=[("vsb", par)], chan=("b_v", par))

        def head_maps(name):
            hm = []
            if name == "A":
                for h in range(4):
                    hm.append((h, 0, 96, h, h, 96 ** -0.5, None, h * 64, "plain"))
            elif name == "C":
                for h in range(4):
                    hm.append((h // 2, 64 * (h % 2), 64, h // 2, h // 2, 64 ** -0.5, None, h * 64, "plain"))
            elif name == "B":
                for h in range(4):
                    hm.append((h // 2, 64 * (h % 2), 64, h // 2, h, 64 ** -0.5, h, h * 64, "plain"))
            else:
                for h in range(4):
                    for c in range(2):
                        hm.append((h // 2, 64 * (h % 2), 64, (h // 2) + 2 * c, h, 32 ** -0.5, h, h * 64, "d%d" % c))
            return hm

        load_unit(units[0], 0)
        for ui, u in enumerate(units):
            par = ui % 2
            (name, qs, nq, ks, nk, vc, nvh, sbase), sq_ = u
            if ui + 1 < len(units):
                load_unit(units[ui + 1], 1 - par)
            if sbase is not None and cur_strip[0] != sbase:
                for i in range(4):
                    dma("sp", strips[:, i, :], SD[sbase + i], reads=["SD"], accum=["strips"], chan="b_s")
                cur_strip[0] = sbase
            hms = head_maps(name)
            for qc in range(4):
                yb = state["yb"]
                state["yb"] = 1 - yb
                tiles = []
                for hi, hmv in enumerate(hms):
                    kcs = list(range(16))
                    if name == "B":
                        kcs = [kc for kc in kcs if not (kc * 128 - qc * 512 - 511 > 1024 or kc * 128 + 127 - qc * 512 < -1024)]
                    for j, kc in enumerate(kcs):
                        tiles.append((hi, hmv, kc, j == 0, j == len(kcs) - 1))
                LOOK = 4
                ob_of_head = {}

                def emit_scores(idx):
                    hi, hmv, kc, first, last = tiles[idx]
                    qsl, r0, nr, kt, vh, scale, sidx, ycl, kind = hmv
                    step = state["step"] + idx
                    sb_ = SBANKS[step % 5]
                    krow0 = r0
                    lhsT = kT[par][krow0:krow0 + nr, kt, kc * 128:(kc + 1) * 128]
                    rhs = qT[par][r0:r0 + nr, qsl, qc * 512:(qc + 1) * 512]
                    P.op("pe", (lambda sb_=sb_, lhsT=lhsT, rhs=rhs: (lambda e: e.matmul(bank(sb_), lhsT=lhsT, rhs=rhs, start=True,
                                                                                      stop=True)))(),
                         reads=[("qT", par), ("kT", par)], writes=["ps%d" % sb_])

                def emit_rest(idx):
                    hi, hmv, kc, first, last = tiles[idx]
                    qsl, r0, nr, kt, vh, scale, sidx, ycl, kind = hmv
                    step = state["step"] + idx
                    sb_ = SBANKS[step % 5]
                    pb = step % 6
                    if first:
                        ob_of_head[hi] = state["ob"]
                        state["ob"] = 1 - state["ob"]
                    ob = 3 + ob_of_head[hi]
                    act(pt[pb], bank(sb_), AF.Exp, ["ps%d" % sb_], [("pt", pb)], scale=scale)
                    if sidx is not None:
                        j0 = STRIP_OFF - kc * 128 + qc * 512
                        eng = "dve"
                        tt(eng, pt[pb], pt[pb], strips[:, sidx, j0:j0 + 512], ALU.mult, [("pt", pb), "strips"], [("pt", pb)])
                    def pv(e, pb=pb, ob=ob, kc=kc, vh=vh, first=first, last=last, par=par):
                        return e.matmul(bank(ob), lhsT=vsb[par][:, kc, vh, :], rhs=pt[pb], start=first, stop=last)
                    P.op("pe", pv, reads=[("pt", pb), ("vsb", par)], writes=["ps%d" % ob])
                    if last:
                        def fin(ob=ob, kind=kind, ycl=ycl):
                            oi = state["ot"]
                            state["ot"] = 1 - oi
                            otb = ot[oi]
                            act(otb, bank(ob), AF.Copy, ["ps%d" % ob], [("ot", oi)])
                            def trf(e, otb=otb):
                                ins = None
                                for qs_ in range(4):
                                    ins = e.transpose(out=bank(7)[:, qs_ * 128:(qs_ + 1) * 128], in_=otb[:, qs_ * 128:(qs_ + 1) * 128],
                                                      identity=ident_f)
                                return ins
                            P.op("pe", trf, reads=[("ot", oi)], writes=["ps7"])
                            o3 = bank(7).rearrange("p (q e) -> p q e", q=4)
                            rc = rec[:, 0:4] if kind != "d1" else rec[:, 4:8]
                            rtag = "rec0" if kind != "d1" else "rec1"
                            recip(rc, o3[:, :, 64], ["ps7"], [rtag])
                            yv = ybuf[yb][:, :, ycl:ycl + 64]
                            if kind == "plain":
                                P.op("dve", (lambda yv=yv, o3=o3, rc=rc: (lambda e: e.tensor_tensor(
                                    out=yv, in0=o3[:, :, 0:64], in1=bcast(rc, 2, [128, 4, 64]), op=ALU.mult)))(),
                                    reads=["ps7", rtag], accum=[("ybuf", yb)])
                            elif kind == "d0":
                                tt("dve", t0b, o3[:, :, 0:64], bcast(rc, 2, [128, 4, 64]), ALU.mult, ["ps7", rtag], ["t0b"])
                            else:
                                tt("dve", t1b, o3[:, :, 0:64], bcast(rc, 2, [128, 4, 64]), ALU.mult, ["ps7", rtag], ["t1b"])
                                P.op("dve", (lambda yv=yv: (lambda e: e.scalar_tensor_tensor(
                                    out=yv, in0=t1b, scalar=neglam[:, L:L + 1], in1=t0b, op0=ALU.mult, op1=ALU.add)))(),
                                    reads=["t0b", "t1b"], accum=[("ybuf", yb)])
                        pending.append((idx + 2, fin))

                n = len(tiles)
                pending = []
                for i in range(min(LOOK, n)):
                    emit_scores(i)
                for i in range(n):
                    if i + LOOK < n:
                        emit_scores(i + LOOK)
                    emit_rest(i)
                    while pending and pending[0][0] <= i:
                        pending.pop(0)[1]()
                while pending:
                    pending.pop(0)[1]()
                state["step"] += n
                tk = sq_ * S + qc * 512
                dma("sp", Y[tk:tk + 512, ycol[name]:ycol[name] + 256].rearrange("(q p) c -> p q c", p=128), ybuf[yb],
                    reads=[("ybuf", yb)], accum=["Y"], chan=("b_y", yb))
        P.barrier()
        AR.reset(m)

    def phase_C(L, x_src):
        m = AR.mark()
        w_out_sb = AR.take([128, 8, D], BF16)
        for c in range(8):
            dma("pool", w_out_sb[:, c, :], w_out[L, c * 128:(c + 1) * 128, :], accum=["w_out"], chan="c_w")
        yt = [AR.take([128, D], F32) for _ in range(2)]
        xt = [AR.take([128, D], F32) for _ in range(2)]
        sqc = AR.take([128, D], F32)
        ssc = [AR.take([128, 8], F32) for _ in range(2)]
        ybf = [AR.take([128, D], BF16) for _ in range(2)]
        mT = [AR.take([128, 8, 128], BF16) for _ in range(2)]
        xo = [AR.take([128, D], F32) for _ in range(2)]
        inv_c = AR.take([128, 8], F32)
        P.op("dve", lambda e: e.memset(inv_c[:, 0:3], 1.0 / 256), writes=["inv_c"])
        P.op("dve", lambda e: e.memset(inv_c[:, 3:8], 1.0 / 64), accum=["inv_c"])
        for t in range(NT):
            p = t % 2
            R = lambda n_: (n_, p)
            dma("sp", yt[p], Y[t * 128:(t + 1) * 128, :], reads=["Y"], writes=[R("yt")], chan=("c_y", p))
            dma("sp", xt[p], x_src[t * 128:(t + 1) * 128, :], reads=["Xsrc"], writes=[R("xt")], chan=("c_x", p))
            act(sqc, yt[p], AF.Square, [R("yt")], ["sqc"])
            red(ssc[p][:, 0:3], sqc[:, 0:768].rearrange("p (a e) -> p a e", a=3), ALU.add, ["sqc"], [R("ssc")])
            P.op("dve", (lambda p=p: (lambda e: e.tensor_reduce(out=ssc[p][:, 3:7], in_=sqc[:, 768:1024].rearrange("p (a e) -> p a e", a=4),
                                                                 axis=AX.X, op=ALU.add)))(), reads=["sqc"], accum=[R("ssc")])
            rstd_chain(ssc[p][:, 0:7], inv_c[:, 0:7], 7, R("ssc"), None)
            tt("dve", ybf[p][:, 0:768].rearrange("p (a e) -> p a e", a=3), yt[p][:, 0:768].rearrange("p (a e) -> p a e", a=3),
               bcast(ssc[p][:, 0:3], 2, [128, 3, 256]), ALU.mult, [R("yt"), R("ssc")], [R("ybf")])
            P.op("dve", (lambda p=p: (lambda e: e.tensor_tensor(
                out=ybf[p][:, 768:1024].rearrange("p (a e) -> p a e", a=4), in0=yt[p][:, 768:1024].rearrange("p (a e) -> p a e", a=4),
                in1=bcast(ssc[p][:, 3:7], 2, [128, 4, 64]), op=ALU.mult)))(), reads=[R("yt"), R("ssc")], accum=[R("ybf")])
            def trf(e, p=p):
                ins = None
                for c in range(8):
                    ins = e.transpose(out=bank_bf(7)[:, c * 128:(c + 1) * 128], in_=ybf[p][:, c * 128:(c + 1) * 128], identity=ident_b)
                return ins
            P.op("pe", trf, reads=[R("ybf")], writes=["ps7"])
            tt("dve", mT[p], bank_bf(7).rearrange("p (c k) -> p c k", c=8), bcast(gv[:, L, GV_BETA:GV_BETA + 8], 2, [128, 8, 128]),
               ALU.mult, ["ps7"], [R("mT")])
            def mmf(e, p=p):
                ins = None
                for j in range(2):
                    for c in range(8):
                        ins = e.matmul(bank(j), lhsT=mT[p][:, c, :], rhs=w_out_sb[:, c, j * 512:(j + 1) * 512], start=(c == 0),
                                       stop=(c == 7))
                return ins
            P.op("pe", mmf, reads=[R("mT"), "w_out"], writes=["ps0", "ps1"])
            tt("dve", xo[p][:, 0:512], xt[p][:, 0:512], bank(0), ALU.add, [R("xt"), "ps0"], [R("xo0")])
            tt("dve", xo[p][:, 512:1024], xt[p][:, 512:1024], bank(1), ALU.add, [R("xt"), "ps1"], [R("xo1")])
            dma("pool", X1[t * 128:(t + 1) * 128, :], xo[p], reads=[R("xo0"), R("xo1")], accum=["X1"], chan=("c_o", p))
        P.barrier()
        AR.reset(m)

    def phase_D(L, dst):
        m = AR.mark()
        wr = AR.take([128, 8, 20], F32)
        dma("sp", wr, router_w[L].rearrange("(c p) n -> p c n", p=128), writes=["wr"], chan="d_wr")
        hT = AR.take([128, 8, S], BF16)
        hTf = AR.take([128, 8, 128], F32)
        yacc = AR.take([128, 16, D], F32)
        lg = AR.take([128, 16, 20], F32)
        gate = AR.take([128, 16, 16], F32)
        xs = [AR.take([128, D], F32) for _ in range(2)]
        hf = [AR.take([128, D], F32) for _ in range(2)]
        ss = [AR.take([128, 2], F32) for _ in range(2)]
        wg = [AR.take([128, 8, DFF], BF16) for _ in range(2)]
        wu = [AR.take([128, 8, DFF], BF16) for _ in range(2)]
        wd = [AR.take([128, 4, D], BF16) for _ in range(2)]
        sg = [AR.take([128, 512], BF16) for _ in range(2)]
        hid = [AR.take([128, 4, 512], BF16) for _ in range(2)]
        r1 = AR.take([128, 16, 4], F32)
        r2 = AR.take([128, 16, 4], F32)
        r3 = AR.take([128, 16], F32)
        r4 = AR.take([128, 16], F32)
        e1 = AR.take([128, 16, 16], F32)
        e2 = AR.take([128, 16, 16], F32)
        r5 = AR.take([128, 16], F32)

        def load_expert(e_, par, seqi):
            for c in range(0, 8, 4):
                dma("pool", wg[par][:, c:c + 4, :], w_gate[L, e_, c * 128:(c + 4) * 128, :].rearrange("(c p) f -> p c f", p=128),
                    accum=[("wg", par)], chan=("d_w", par))
                dma("pool", wu[par][:, c:c + 4, :], w_up[L, e_, c * 128:(c + 4) * 128, :].rearrange("(c p) f -> p c f", p=128),
                    accum=[("wu", par)], chan=("d_w", par))
            dma("pool", wd[par], w_down[L, e_].rearrange("(c p) d -> p c d", p=128), accum=[("wd", par)], chan=("d_w", par))

        for sq_ in range(SEQ_PER_CORE):
            load_expert(0, 0, sq_)
            for t in range(16):
                p = t % 2
                R = lambda n_: (n_, p)
                tok = sq_ * S + t * 128
                dma("sp", xs[p], X1[tok:tok + 128, :], reads=["X1"], writes=[R("xs")], chan=("d_x", p))
                act(hf[p], xs[p], AF.Square, [R("xs")], [R("ss"), R("hf")], accum_out=ss[p][:, 0:1])
                rstd_chain(ss[p][:, 0:1], 1.0 / D, 1, R("ss"), None)
                act(hf[p], xs[p], AF.Copy, [R("xs"), R("ss")], [R("hf")], scale=ss[p][:, 0:1])
                P.op("pool", (lambda t=t, p=p: (lambda e: e.tensor_copy(out=yacc[:, t, :], in_=xs[p])))(), reads=[R("xs")],
                     writes=[("yacc", t)])
                for half in range(2):
                    def trf(e, p=p, half=half):
                        ins = None
                        for c in range(4):
                            cc = half * 4 + c
                            ins = e.transpose(out=bank(6 + half)[:, c * 128:(c + 1) * 128], in_=hf[p][:, cc * 128:(cc + 1) * 128],
                                              identity=ident_f)
                        return ins
                    P.op("pe", trf, reads=[R("hf")], writes=["ps%d" % (6 + half)])
                    g2 = gv[:, L, GV_G2 + 4 * half:GV_G2 + 4 * half + 4]
                    src = bank(6 + half).rearrange("p (c k) -> p c k", c=4)
                    P.op("dve", (lambda src=src, g2=g2, half=half: (lambda e: e.tensor_tensor(
                        out=hTf[:, 4 * half:4 * half + 4, :], in0=src, in1=bcast(g2, 2, [128, 4, 128]), op=ALU.mult)))(),
                        reads=["ps%d" % (6 + half)], accum=["hTf"] if half else [], writes=[] if half else ["hTf"])
                    P.op("act", (lambda src=src, half=half, t=t: (lambda e: e.activation(
                        out=hT[:, 4 * half:4 * half + 4, t * 128:(t + 1) * 128], in_=hTf[:, 4 * half:4 * half + 4, :], func=AF.Copy)))(),
                        reads=["hTf"], accum=["hT"])
                def mmr(e):
                    ins = None
                    for c in range(8):
                        ins = e.matmul(bank(5)[:, 0:20], lhsT=hTf[:, c, :], rhs=wr[:, c, :], start=(c == 0), stop=(c == 7))
                    return ins
                P.op("pe", mmr, reads=["hTf", "wr"], writes=["ps5"])
                P.op("dve", (lambda t=t: (lambda e: e.tensor_tensor(out=lg[:, t, :], in0=bank(5)[:, 0:20],
                                                                    in1=gbt[:, L, GB_RB:GB_RB + 20], op=ALU.add)))(),
                     reads=["ps5"], accum=["lg"])
            gl = lg[:, :, 0:4]
            el = lg[:, :, 4:20].rearrange("p t (g j) -> p t g j", g=4)
            red(r3, gl, ALU.max, ["lg"], ["r3"])
            tt("dve", r1, gl, bcast(r3, 2, [128, 16, 4]), ALU.is_ge, ["lg", "r3"], ["r1"])
            tt("dve", r2, gl, bcast(r3, 2, [128, 16, 4]), ALU.subtract, ["lg", "r3"], ["r2"])
            act(r2, r2, AF.Exp, ["r2"], ["r2"])
            red(r4, r2, ALU.add, ["r2"], ["r4"])
            recip(r4, r4, ["r4"], ["r4"])
            ts("dve", r1, r1, -1.0, 1.0e4, ALU.add, ALU.mult, ["r1"], ["r1"])
            tt("dve", e1.rearrange("p t (g j) -> p t g j", g=4), el, bcast(r1, 3, [128, 16, 4, 4]), ALU.add, ["lg", "r1"], ["e1"])
            red(r3, e1, ALU.max, ["e1"], ["r3"])
            tt("dve", e1, e1, bcast(r3, 2, [128, 16, 16]), ALU.subtract, ["e1", "r3"], ["e1"])
            ts("dve", e2, e1, 0.0, -2.0, ALU.is_ge, ALU.mult, ["e1"], ["e2"])
            act(e1, e1, AF.Exp, ["e1"], ["e1"])
            tt("dve", e2, e2, e1, ALU.add, ["e2", "e1"], ["e2"])
            red(r5, e2, ALU.max, ["e2"], ["r5"])
            tt("dve", e2, e1, bcast(r5, 2, [128, 16, 16]), ALU.is_ge, ["e1", "r5"], ["e2"])
            tt("dve", e2, e2, e1, ALU.mult, ["e2", "e1"], ["e2"])
            red(r5, e2, ALU.add, ["e2"], ["r5"])
            recip(r5, r5, ["r5"], ["r5"])
            tt("dve", r5, r5, r4, ALU.mult, ["r5", "r4"], ["r5"])
            tt("dve", gate, e2, bcast(r5, 2, [128, 16, 16]), ALU.mult, ["e2", "r5"], ["gate"])
            step = 0
            for e_ in range(NE):
                par = e_ % 2
                if e_ + 1 < NE:
                    load_expert(e_ + 1, 1 - par, sq_)
                for blk in range(4):
                    hb_ = (e_ * 4 + blk) % 2
                    for fc in range(4):
                        gp = step % 2
                        step += 1
                        def gu(e, gp=gp, fc=fc, blk=blk, par=par):
                            ins = None
                            for c in range(8):
                                ins = e.matmul(bank(gp), lhsT=wg[par][:, c, fc * 128:(fc + 1) * 128],
                                               rhs=hT[:, c, blk * 512:(blk + 1) * 512], start=(c == 0), stop=(c == 7))
                            for c in range(8):
                                ins = e.matmul(bank(2 + gp), lhsT=wu[par][:, c, fc * 128:(fc + 1) * 128],
                                               rhs=hT[:, c, blk * 512:(blk + 1) * 512], start=(c == 0), stop=(c == 7))
                            return ins
                        P.op("pe", gu, reads=["hT", ("wg", par), ("wu", par)], writes=["ps%d" % gp, "ps%d" % (2 + gp)])
                        act(sg[gp], bank(gp), AF.Silu, ["ps%d" % gp], [("sg", gp)])
                        P.op("dve", (lambda hb_=hb_, fc=fc, gp=gp: (lambda e: e.tensor_tensor(
                            out=hid[hb_][:, fc, :], in0=bank(2 + gp), in1=sg[gp], op=ALU.mult)))(),
                            reads=["ps%d" % (2 + gp), ("sg", gp)], accum=[("hid", hb_)] if fc else [],
                            writes=[] if fc else [("hid", hb_)])
                    for tt_ in range(4):
                        t = blk * 4 + tt_
                        for half in range(2):
                            ob = 4 + (tt_ * 2 + half) % 2
                            def dn(e, hb_=hb_, tt_=tt_, half=half, ob=ob, par=par):
                                ins = None
                                for fc in range(4):
                                    ins = e.matmul(bank(ob), lhsT=hid[hb_][:, fc, tt_ * 128:(tt_ + 1) * 128],
                                                   rhs=wd[par][:, fc, half * 512:(half + 1) * 512], start=(fc == 0), stop=(fc == 3))
                                return ins
                            P.op("pe", dn, reads=[("hid", hb_), ("wd", par)], writes=["ps%d" % ob])
                            ya = yacc[:, t, half * 512:(half + 1) * 512]
                            P.op("dve", (lambda ya=ya, ob=ob, t=t, e_=e_: (lambda e: e.scalar_tensor_tensor(
                                out=ya, in0=bank(ob), scalar=gate[:, t, e_:e_ + 1], in1=ya, op0=ALU.mult, op1=ALU.add)))(),
                                reads=["ps%d" % ob, "gate", ("yacc", t)], writes=[("yacc", t)])
            for t in range(16):
                tok = sq_ * S + t * 128
                dma("sp", dst[tok:tok + 128, :], yacc[:, t, :], reads=[("yacc", t)], accum=["DST"], chan="d_o")
        P.barrier()
        AR.reset(m)

    cur = x_in
    for L in range(n_layers):
        phase_A(L, cur)
        if stop_after == ("A", L):
            break
        phase_B(L)
        if stop_after == ("B", L):
            break
        phase_C(L, cur)
        if stop_after == ("C", L):
            break
        dst = out_d if L == n_layers - 1 else X2
        phase_D(L, dst)
        cur = X2
    if dbg:
        for name, src, shape, dt in (("dbg_Y", Y, [T, D], F32), ("dbg_X1", X1, [T, D], F32), ("dbg_QT", QT, [NSLOT, 128, T], BF16),
                                     ("dbg_VS", VS, [T, NV], BF16)):
            o = nc.dram_tensor(name, shape, dt, kind="ExternalOutput").ap()
            dma("sp", o, src, chan="dbg")
        P.barrier()
    P.emit()
    return nc


def _rel_bucket(rel):
    half = 16
    max_exact = 8
    n = np.abs(rel)
    nf = np.maximum(n, 1).astype(np.float32)
    log_ratio = np.log(nf / max_exact) / math.log(1024 / max_exact)
    large = np.minimum(max_exact + (log_ratio * (half - max_exact)).astype(np.int32), half - 1)
    return np.where(rel > 0, half, 0) + np.where(n < max_exact, n, large)


def _rope_tab():
    def cs(pos, dim):
        inv = 1.0 / (10000.0 ** (np.arange(0, dim, 2, dtype=np.float32) / dim))
        ang = pos.astype(np.float32)[:, None] * inv[None, :]
        ang = np.concatenate([ang, ang], -1)
        c, s = np.cos(ang), np.sin(ang)
        s = np.concatenate([-s[:, :dim // 2], s[:, dim // 2:]], -1)
        return c.astype(np.float32), s.astype(np.float32)
    pos = np.arange(S)
    tabs = []
    for p in (pos, pos // 64, pos % 64):
        c, s = cs(p, 32)
        tabs += [c, s]
    return np.ascontiguousarray(np.stack(tabs, 1).reshape(S, 6 * 32))


def _strip_index():
    p = np.arange(128)[:, None]
    j = np.arange(STRIP_W)[None, :]
    delta = p - j + STRIP_OFF
    return delta


_CACHE = {}


def _prepare_consts():
    if "c" in _CACHE:
        return _CACHE["c"]
    delta = _strip_index()
    bidx = _rel_bucket(delta)
    ad = np.abs(delta)
    mult = ((ad <= 64).astype(np.float32) + ((delta % 4 == 0) & (ad <= 256)).astype(np.float32)
            + ((delta % 16 == 0) & (ad <= 1024)).astype(np.float32))
    _CACHE["c"] = (bidx, np.ascontiguousarray(mult), _rope_tab())
    return _CACHE["c"]


def _in_maps(inputs):
    bidx, mult, rope = _prepare_consts()
    f = lambda a: np.ascontiguousarray(np.asarray(a, dtype=np.float32))
    rel_bias = f(inputs["rel_bias"])
    g = rel_bias[bidx]
    strip_dil = np.ascontiguousarray(np.transpose(g[:, :, 0:4], (2, 0, 1)))
    strip_dif = np.ascontiguousarray(np.transpose(g[:, :, 4:8], (2, 0, 1)))
    common = {
        "norm1_g": f(inputs["norm1_g"]), "w_in": f(inputs["w_in"]), "mla_q_norm_g": f(inputs["mla_q_norm_g"]),
        "mla_kv_norm_g": f(inputs["mla_kv_norm_g"]), "mla_w_uq": f(inputs["mla_w_uq"]), "mla_w_ukv": f(inputs["mla_w_ukv"]),
        "mla_qk_g": f(inputs["mla_qk_g"]).reshape(DEPTH, 192), "dil_qk_g": f(inputs["dil_qk_g"]).reshape(DEPTH, 128),
        "gqa_qk_g": f(inputs["gqa_qk_g"]).reshape(DEPTH, 128), "diff_qk_g": f(inputs["diff_qk_g"]).reshape(DEPTH, 64),
        "diff_lambda": f(inputs["diff_lambda"]).reshape(DEPTH, 128), "diff_subln_g": f(inputs["diff_subln_g"]),
        "mix_beta": f(inputs["mix_beta"]), "w_out": f(inputs["w_out"]), "norm2_g": f(inputs["norm2_g"]),
        "router_w": np.ascontiguousarray(np.concatenate([f(inputs["router_group_w"]), f(inputs["router_expert_w"])], -1)),
        "router_b": np.ascontiguousarray(np.concatenate([f(inputs["router_group_b"]), f(inputs["router_expert_b"])], -1)),
        "expert_w_gate": f(inputs["expert_w_gate"]), "expert_w_up": f(inputs["expert_w_up"]),
        "expert_w_down": f(inputs["expert_w_down"]),
        "rope_tab": rope, "strip_dil": strip_dil, "strip_dif": strip_dif, "strip_mult": mult,
    }
    x = f(inputs["x"])
    maps = []
    for c in range(NCORES):
        mp = dict(common)
        mp["x"] = np.ascontiguousarray(x[c * SEQ_PER_CORE:(c + 1) * SEQ_PER_CORE].reshape(T, D))
        maps.append(mp)
    return maps


def kernel(**inputs):
    if "nc" not in _CACHE:
        _CACHE["nc"] = build_program()
    nc = _CACHE["nc"]
    maps = _in_maps(inputs)
    res = run_bass_kernel_spmd(nc, maps, core_ids=list(range(NCORES)))
    out = np.stack([np.asarray(r["out"]).reshape(SEQ_PER_CORE, S, D) for r in res.results], 0)
    return out.reshape(BATCH, S, D).astype(np.float32)
```

```python
import math
import contextlib
import numpy as np
import ml_dtypes
import concourse.bass as bass
import concourse.mybir as mybir
from concourse.bass_utils import run_bass_kernel_spmd

F32 = mybir.dt.float32
BF16 = mybir.dt.bfloat16
AF = mybir.ActivationFunctionType
ALU = mybir.AluOpType
AX = mybir.AxisListType

NCORES = 8
D = 1024
S = 2048
BATCH = 16
SEQ_PER_CORE = BATCH // NCORES
T = SEQ_PER_CORE * S
NT = T // 128
DEPTH = 2
IN_COLS = 2464
EPS = 1e-6
NE = 16
DFF = 512
STRIP_W = 3968
STRIP_OFF = 1920

C_CQ, C_CKV, C_KR = 0, 256, 384
C_BQ, C_BK, C_BV = 416, 672, 928
C_CQ2, C_CK2, C_CV2 = 1184, 1440, 1568
C_DQ, C_DK, C_DV = 1696, 1952, 2208
SL_AQ, SL_AK, SL_BQ, SL_BK, SL_CQ, SL_CK, SL_DQ, SL_DK = 0, 4, 8, 10, 12, 14, 16, 18
NSLOT = 20
V_A, V_B, V_C, V_D = 0, 256, 512, 640
NV = 896


class Op:
    __slots__ = ("eng", "fn", "deps", "signal", "semval", "chan", "chanval", "pos", "epoch")

    def __init__(self, eng, fn, chan=None):
        self.eng = eng
        self.fn = fn
        self.deps = []
        self.signal = False
        self.semval = None
        self.chan = chan
        self.chanval = None
        self.pos = 0


class Prog:
    ENGS = ("pe", "act", "dve", "pool", "sp")

    def __init__(self, nc):
        self.nc = nc
        self.ops = {e: [] for e in self.ENGS}
        self.writers = {}
        self.readers = {}
        self.chan_count = {}
        self.dma_since_barrier = {}
        self.epoch = 0
        self.chan_map = {}

    @staticmethod
    def _key(o):
        return (o.eng, o.chan)

    def _prune_add(self, lst, o):
        k = self._key(o)
        lst[:] = [x for x in lst if self._key(x) != k]
        lst.append(o)

    def op(self, eng, fn, reads=(), writes=(), accum=(), chan=None, extra_deps=()):
        assert fn is not None or not (reads or writes or accum)
        if chan is not None:
            m = self.chan_map.setdefault(self.epoch, {})
            if chan not in m:
                m[chan] = len(m)
            chan = m[chan]
        o = Op(eng, fn, chan)
        o.epoch = self.epoch
        deps = list(extra_deps)
        for r in reads:
            deps += self.writers.get(r, [])
        for w in tuple(writes) + tuple(accum):
            deps += self.writers.get(w, [])
            deps += self.readers.get(w, [])
        for r in reads:
            self._prune_add(self.readers.setdefault(r, []), o)
        for w in writes:
            self.writers[w] = [o]
            self.readers[w] = []
        for w in accum:
            self._prune_add(self.writers.setdefault(w, []), o)
        if chan is not None:
            self.chan_count[chan] = self.chan_count.get(chan, 0) + 16
            o.chanval = self.chan_count[chan]
            self.dma_since_barrier[chan] = o
        best = {}
        for d in deps:
            if d is o or d.fn is None:
                continue
            if eng == "pe" and d.eng == "pe" and d.chan is None:
                continue
            k = self._key(d)
            b = best.get(k)
            if b is None:
                best[k] = d
            elif d.chan is not None:
                if d.chanval > b.chanval:
                    best[k] = d
            elif d.pos > b.pos:
                best[k] = d
        o.deps = list(best.values())
        for d in o.deps:
            if d.chan is None:
                d.signal = True
        o.pos = len(self.ops[eng])
        self.ops[eng].append(o)
        return o

    def barrier(self):
        deps = []
        for e in self.ENGS:
            for x in reversed(self.ops[e]):
                if x.chan is None and x.fn is not None:
                    deps.append(x)
                    break
        deps += list(self.dma_since_barrier.values())
        self.dma_since_barrier = {}
        for e in self.ENGS:
            self.op(e, None, extra_deps=deps)
        self.writers = {}
        self.readers = {}
        self.epoch += 1

    def emit(self):
        nc = self.nc
        for e in self.ENGS:
            c = {}
            for o in self.ops[e]:
                if o.chan is None and o.signal:
                    c[o.epoch] = c.get(o.epoch, 0) + 1
                    o.semval = c[o.epoch]
        chans = sorted(self.chan_count.keys(), key=str)
        with contextlib.ExitStack() as st:
            used = set()
            for e in self.ENGS:
                for o in self.ops[e]:
                    if o.chan is None and o.signal:
                        used.add((e, o.epoch))
            esem = {k: st.enter_context(nc.semaphore("sem_%s_%d" % k)) for k in sorted(used)}
            print("[prog] semaphores: %d engine, %d dma channels" % (len(esem), len(chans)))
            csem = {c: st.enter_context(nc.semaphore("ch_%d" % i)) for i, c in enumerate(chans)}
            block = st.enter_context(nc.Block())

            def run(ename):
                def body(eng):
                    waited = {}
                    for o in self.ops[ename]:
                        for d in o.deps:
                            if d.chan is not None:
                                s, v = csem[d.chan], d.chanval
                            else:
                                s, v = esem[(d.eng, d.epoch)], d.semval
                            if waited.get(id(s), 0) >= v:
                                continue
                            waited[id(s)] = v
                            eng.wait_ge(s, v)
                        if o.fn is None:
                            continue
                        ins = o.fn(eng)
                        if o.chan is not None:
                            ins.then_inc(csem[o.chan], 16)
                        elif o.signal:
                            ins.then_inc(esem[(ename, o.epoch)], 1)
                return body

            block.tensor(run("pe"))
            block.scalar(run("act"))
            block.vector(run("dve"))
            block.gpsimd(run("pool"))
            block.sync(run("sp"))


class Arena:
    def __init__(self, tensor, ncols):
        self.t = tensor
        self.n = ncols
        self.off = 0

    def mark(self):
        return self.off

    def reset(self, m):
        self.off = m

    def take(self, shape, dtype):
        p = shape[0]
        nfree = int(np.prod(shape[1:]))
        nbytes = nfree * (4 if dtype == F32 else 2)
        ncol = (nbytes + 3) // 4
        ncol = (ncol + 7) // 8 * 8
        assert self.off + ncol <= self.n, ("arena overflow", self.off, ncol, self.n)
        a = self.t[0:p, self.off:self.off + ncol]
        self.off += ncol
        if dtype != F32:
            a = a.bitcast(dtype)
        a = a[:, 0:nfree]
        if len(shape) > 2:
            names = "abcd"[: len(shape) - 1]
            kw = {names[i]: shape[i + 1] for i in range(len(shape) - 2)}
            a = a.rearrange("p (%s) -> p %s" % (" ".join(names), " ".join(names)), **kw)
        return a


def bcast(ap, axis, shape):
    return ap.unsqueeze(axis).to_broadcast(list(shape))


def build_program(n_layers=DEPTH, stop_after=None, dbg=False):
    nc = bass.Bass("TRN2", target_bir_lowering=False)
    P = Prog(nc)

    def din(name, shape, dt=F32):
        return nc.dram_tensor(name, list(shape), dt, kind="ExternalInput").ap()

    def dscr(name, shape, dt):
        return nc.dram_tensor(name, list(shape), dt, kind="Internal").ap()

    x_in = din("x", [T, D])
    norm1_g = din("norm1_g", [DEPTH, D])
    w_in = din("w_in", [DEPTH, D, IN_COLS])
    mla_q_norm_g = din("mla_q_norm_g", [DEPTH, 256])
    mla_kv_norm_g = din("mla_kv_norm_g", [DEPTH, 128])
    mla_w_uq = din("mla_w_uq", [DEPTH, 256, 384])
    mla_w_ukv = din("mla_w_ukv", [DEPTH, 128, 512])
    mla_qk_g = din("mla_qk_g", [DEPTH, 2 * 96])
    dil_qk_g = din("dil_qk_g", [DEPTH, 2 * 64])
    gqa_qk_g = din("gqa_qk_g", [DEPTH, 2 * 64])
    diff_qk_g = din("diff_qk_g", [DEPTH, 2 * 32])
    diff_lambda = din("diff_lambda", [DEPTH, 4 * 32])
    diff_subln_g = din("diff_subln_g", [DEPTH, 64])
    mix_beta = din("mix_beta", [DEPTH, D])
    w_out = din("w_out", [DEPTH, D, D])
    norm2_g = din("norm2_g", [DEPTH, D])
    router_w = din("router_w", [DEPTH, D, 20])
    router_b = din("router_b", [DEPTH, 20])
    w_gate = din("expert_w_gate", [DEPTH, NE, D, DFF])
    w_up = din("expert_w_up", [DEPTH, NE, D, DFF])
    w_down = din("expert_w_down", [DEPTH, NE, DFF, D])
    rope_tab = din("rope_tab", [S, 6 * 32])
    strip_dil = din("strip_dil", [4, 128, STRIP_W])
    strip_dif = din("strip_dif", [4, 128, STRIP_W])
    strip_mult = din("strip_mult", [128, STRIP_W])
    out_d = nc.dram_tensor("out", [T, D], F32, kind="ExternalOutput").ap()

    QT = dscr("QT", [NSLOT, 128, T], BF16)
    VS = dscr("VS", [T, NV], BF16)
    Y = dscr("Y", [T, D], F32)
    X1 = dscr("X1", [T, D], F32)
    X2 = dscr("X2", [T, D], F32)
    SD = dscr("SD", [8, 128, STRIP_W], BF16)
    dbg_out = {}

    ARENA_COLS = 196 * 256
    arena_t = nc.alloc_sbuf_tensor("arena", [128, ARENA_COLS], F32)
    AR = Arena(arena_t, ARENA_COLS)
    psum_t = nc.alloc_psum_tensor("psum", [128, 4096], F32)

    def bank(i):
        return psum_t[:, i * 512:(i + 1) * 512]

    def bank_bf(i):
        return psum_t[:, i * 512:(i + 1) * 512].bitcast(BF16)

    def dma(q, out, in_, reads=(), writes=(), accum=(), chan=None, slow=False):
        if slow:
            fn = lambda e: e.dma_start(out=out, in_=in_, allow_slow_non_contiguous=True)
        else:
            fn = lambda e: e.dma_start(out=out, in_=in_)
        return P.op(q, fn, reads=reads, writes=writes, accum=accum, chan=chan)

    def tt(eng, out, in0, in1, op, reads, writes, accum=()):
        return P.op(eng, lambda e: e.tensor_tensor(out=out, in0=in0, in1=in1, op=op), reads=reads, writes=writes, accum=accum)

    def ts(eng, out, in0, s1, s2, op0, op1, reads, writes):
        if s2 is None:
            return P.op(eng, lambda e: e.tensor_scalar(out=out, in0=in0, scalar1=s1, scalar2=None, op0=op0),
                        reads=reads, writes=writes)
        return P.op(eng, lambda e: e.tensor_scalar(out=out, in0=in0, scalar1=s1, scalar2=s2, op0=op0, op1=op1),
                    reads=reads, writes=writes)

    def act(out, in_, func, reads, writes, scale=1.0, bias=None, accum_out=None, accum=()):
        def fn(e):
            kw = {}
            if bias is not None:
                kw["bias"] = bias
            if accum_out is not None:
                kw["accum_out"] = accum_out
            return e.activation(out=out, in_=in_, func=func, scale=scale, **kw)
        return P.op("act", fn, reads=reads, writes=writes, accum=accum)

    def red(out, in_, op, reads, writes, axis=AX.X, accum=()):
        return P.op("dve", lambda e: e.tensor_reduce(out=out, in_=in_, axis=axis, op=op), reads=reads, writes=writes, accum=accum)

    def recip(out, in_, reads, writes):
        return P.op("dve", lambda e: e.reciprocal(out=out, in_=in_), reads=reads, writes=writes)

    def rstd_chain(ssq, inv_e, n, tag, scratch):
        if isinstance(inv_e, float):
            act(ssq, ssq, AF.Sqrt, [tag], [tag], scale=inv_e, bias=eps_t[:, 0:1])
        else:
            tt("dve", ssq, ssq, inv_e, ALU.mult, [tag], [tag])
            act(ssq, ssq, AF.Sqrt, [tag], [tag], bias=eps_t[:, 0:1])
        recip(ssq, ssq, [tag], [tag])

    ident_f = AR.take([128, 128], F32)
    ident_b = AR.take([128, 128], BF16)
    eps_t = AR.take([128, 8], F32)
    gv = AR.take([128, DEPTH, 32], F32)
    GV_G1, GV_G2, GV_BETA, GV_QN, GV_KVN, GV_SUB = 0, 8, 16, 24, 26, 27
    gbt = AR.take([128, DEPTH, 704], F32)
    GB_MLA, GB_DIL, GB_GQA, GB_DIF, GB_LAM, GB_RB = 0, 192, 320, 448, 512, 640
    neglam = AR.take([128, DEPTH], F32)
    inv_e1 = AR.take([128, 33], F32)
    warm = AR.take([128, 512], BF16)

    P.op("pool", lambda e: e.memset(ident_f, 1.0), writes=["ident_f"])
    P.op("pool", lambda e: e.affine_select(out=ident_f, in_=ident_f, pattern=[[-1, 128]], compare_op=ALU.is_equal,
                                           fill=0.0, base=0, channel_multiplier=1), reads=["ident_f"], writes=["ident_f"])
    P.op("dve", lambda e: e.tensor_copy(out=ident_b, in_=ident_f), reads=["ident_f"], writes=["ident_b"])
    P.op("dve", lambda e: e.memset(eps_t, EPS), writes=["eps"])
    P.op("dve", lambda e: e.memset(warm, 1.0), writes=["warm"])

    def pe_warmup(n=40):
        def fn(e):
            ins = None
            for i in range(n):
                ins = e.matmul(bank(7), lhsT=ident_b, rhs=warm, start=True, stop=True)
            return ins
        P.op("pe", fn, reads=["warm_c"], writes=["ps7"])
    P.op("dve", lambda e: e.memset(inv_e1[:, 0:1], 1.0 / 256), writes=["inv_e1"])
    P.op("dve", lambda e: e.memset(inv_e1[:, 1:2], 1.0 / 128), accum=["inv_e1"])
    P.op("dve", lambda e: e.memset(inv_e1[:, 2:3], 1.0), accum=["inv_e1"])
    P.op("dve", lambda e: e.memset(inv_e1[:, 3:17], 1.0 / 64), accum=["inv_e1"])
    P.op("dve", lambda e: e.memset(inv_e1[:, 17:33], 1.0 / 32), accum=["inv_e1"])
    for L in range(DEPTH):
        for (src, off, n) in ((norm1_g, GV_G1, 8), (norm2_g, GV_G2, 8), (mix_beta, GV_BETA, 8), (mla_q_norm_g, GV_QN, 2),
                              (mla_kv_norm_g, GV_KVN, 1)):
            dma("sp", gv[:, L, off:off + n], src[L].rearrange("(c p) -> p c", p=128), accum=["gv"], chan="c0", slow=True)
        for h in range(2):
            dma("sp", gv[64 * h:64 * h + 64, L, GV_SUB:GV_SUB + 1], diff_subln_g[L].rearrange("(p c) -> p c", c=1),
                accum=["gv"], chan="c0", slow=True)
        for (src, off, n) in ((mla_qk_g, GB_MLA, 192), (dil_qk_g, GB_DIL, 128), (gqa_qk_g, GB_GQA, 128),
                              (diff_qk_g, GB_DIF, 64), (diff_lambda, GB_LAM, 128), (router_b, GB_RB, 20)):
            dma("sp", gbt[:, L, off:off + n], src[L].partition_broadcast(128), accum=["gbt"], chan="c0")
    for L in range(DEPTH):
        li = 0.8 - 0.6 * math.exp(-0.3 * L)
        P.op("dve", (lambda L=L, li=li: (lambda e: e.tensor_scalar(
            out=gv[:, L, GV_BETA + 6:GV_BETA + 8], in0=gv[:, L, GV_BETA + 6:GV_BETA + 8],
            scalar1=gv[:, L, GV_SUB:GV_SUB + 1], scalar2=1.0 - li, op0=ALU.mult, op1=ALU.mult)))(),
            reads=["gv"], writes=["gv"])
        lt = AR.take([128, 64], F32)
        lv = gbt[:, L, GB_LAM:GB_LAM + 128].rearrange("p (a b c) -> p a b c", a=2, b=2)
        tt("dve", lt.rearrange("p (a c) -> p a c", a=2), lv[:, :, 0, :], lv[:, :, 1, :], ALU.mult, ["gbt"], [("lt", L)])
        ls = AR.take([128, 2], F32)
        red(ls, lt.rearrange("p (a c) -> p a c", a=2), ALU.add, [("lt", L)], [("ls", L)])
        act(ls, ls, AF.Exp, [("ls", L)], [("ls", L)])
        tt("dve", neglam[:, L:L + 1], ls[:, 1:2], ls[:, 0:1], ALU.subtract, [("ls", L)], [("nl", L)])
        ts("dve", neglam[:, L:L + 1], neglam[:, L:L + 1], -li, None, ALU.add, None, [("nl", L)], [("nl", L)])
    m0 = AR.mark()
    smul = AR.take([128, STRIP_W], F32)
    dma("sp", smul, strip_mult, writes=["smul"], chan="c1")
    sfs = [AR.take([128, STRIP_W], F32) for _ in range(2)]
    sbs = [AR.take([128, STRIP_W], BF16) for _ in range(2)]
    for i in range(8):
        sf = sfs[i % 2]
        sb = sbs[i % 2]
        src = strip_dil[i] if i < 4 else strip_dif[i - 4]
        dma("sp", sf, src, writes=[("sf", i % 2)], chan=("c2", i % 2))
        act(sf, sf, AF.Exp, [("sf", i % 2)], [("sf", i % 2)])
        if i < 4:
            tt("dve", sb, sf, smul, ALU.mult, [("sf", i % 2), "smul"], [("sb", i % 2)])
        else:
            P.op("dve", (lambda a=sb, b=sf: (lambda e: e.tensor_copy(out=a, in_=b)))(), reads=[("sf", i % 2)],
                 writes=[("sb", i % 2)])
        dma("sp", SD[i], sb, reads=[("sb", i % 2)], accum=["SD"], chan=("c3", i % 2))
    P.barrier()
    AR.reset(m0)
    persist_mark = AR.mark()

    def phase_A(L, x_src):
        TB = 2
        m = AR.mark()
        w_in_sb = AR.take([128, 8, IN_COLS], BF16)
        w_uq_sb = AR.take([128, 2, 384], BF16)
        w_ukv_sb = AR.take([128, 512], BF16)
        for c in range(8):
            dma("pool", w_in_sb[:, c, :], w_in[L, c * 128:(c + 1) * 128, :], accum=["w_in"], chan="a_w")
        for c in range(2):
            dma("pool", w_uq_sb[:, c, :], mla_w_uq[L, c * 128:(c + 1) * 128, :], accum=["w_in"], chan="a_w")
        dma("pool", w_ukv_sb, mla_w_ukv[L], accum=["w_in"], chan="a_w")
        xt = [AR.take([128, TB, D], F32) for _ in range(2)]
        hb1 = AR.take([128, TB, D], BF16)
        hb = [hb1, hb1]
        hT = [AR.take([128, 8, 128], BF16) for _ in range(2)]
        ss1 = [AR.take([128, 2 * TB], F32) for _ in range(2)]
        junk = AR.take([128, D], F32)
        pj = AR.take([128, TB, IN_COLS], F32)
        sq = AR.take([128, TB, C_DV], F32)
        ssg = AR.take([128, TB, 33], F32)
        cqn = AR.take([128, TB, 384], BF16)
        cT = AR.take([128, 3, 128], BF16)
        q2 = AR.take([128, TB, 384], F32)
        kv2 = AR.take([128, TB, 512], F32)
        ss2 = AR.take([128, TB, 8], F32)
        qk_a = AR.take([128, TB, 8, 96], F32)
        qk_c = AR.take([128, TB, 6, 64], F32)
        rt1 = AR.take([128, TB, 12, 32], F32)
        rt2 = AR.take([128, TB, 12, 32], F32)
        rope = [AR.take([128, TB, 6, 32], F32) for _ in range(2)]
        qktm = AR.take([128, TB, NSLOT, 128], BF16)
        vst = AR.take([128, TB, NV], BF16)
        qst = [AR.take([128, NSLOT, 512], BF16) for _ in range(2)]
        P.op("pool", lambda e: e.memset(qktm, 0.0), writes=["qktm"])
        gb = lambda off, n: gbt[:, L, off:off + n]

        nbatch = NT // TB
        for b in range(nbatch):
            bp = b % 2
            tok0 = b * TB * 128
            R = lambda name: (name, bp)
            dma("sp", xt[bp], x_src[tok0:tok0 + TB * 128, :].rearrange("(t p) d -> p t d", p=128), writes=[R("xt")],
                chan=("a_x", bp))
            tpos = (tok0 % S)
            dma("sp", rope[bp].rearrange("p t a b -> p t (a b)"),
                rope_tab[tpos:tpos + TB * 128, :].rearrange("(t p) c -> p t c", p=128), writes=[R("rope")], chan=("a_r", bp))
            for t in range(TB):
                act(junk, xt[bp][:, t, :], AF.Square, [R("xt")], [R("ss1")] if t == 0 else [], accum_out=ss1[bp][:, t:t + 1],
                    accum=[] if t == 0 else [R("ss1")])
            rstd_chain(ss1[bp][:, 0:TB], 1.0 / D, TB, R("ss1"), None)
            for t in range(TB):
                act(hb[bp][:, t, :], xt[bp][:, t, :], AF.Copy, [R("xt"), R("ss1")], [("hb", t)], scale=ss1[bp][:, t:t + 1])
            for t in range(TB):
                tp = t % 2
                tokt = tok0 + t * 128
                def trf(e, t=t, bp=bp):
                    ins = None
                    for c in range(8):
                        ins = e.transpose(out=bank_bf(7)[:, c * 128:(c + 1) * 128], in_=hb[bp][:, t, c * 128:(c + 1) * 128],
                                          identity=ident_b)
                    return ins
                P.op("pe", trf, reads=[("hb", t)], writes=["ps7"])
                tt("dve", hT[tp], bank_bf(7).rearrange("p (c k) -> p c k", c=8),
                   bcast(gv[:, L, GV_G1:GV_G1 + 8], 2, [128, 8, 128]), ALU.mult, ["ps7"], [("hT", tp)])
                def mmf(e, tp=tp):
                    ins = None
                    for j in range(5):
                        w = 512 if j < 4 else IN_COLS - 2048
                        for c in range(8):
                            ins = e.matmul(bank(j)[:, 0:w], lhsT=hT[tp][:, c, :], rhs=w_in_sb[:, c, j * 512:j * 512 + w],
                                           start=(c == 0), stop=(c == 7))
                    return ins
                P.op("pe", mmf, reads=[("hT", tp), "w_in"], writes=["ps0", "ps1", "ps2", "ps3", "ps4"])
                for j in range(5):
                    w = 512 if j < 4 else IN_COLS - 2048
                    eng = "act" if j % 2 == 0 else "dve"
                    if eng == "act":
                        act(pj[:, t, j * 512:j * 512 + w], bank(j)[:, 0:w], AF.Copy, ["ps%d" % j], [], accum=[("pj", t)])
                    else:
                        P.op("dve", (lambda j=j, w=w, t=t: (lambda e: e.tensor_copy(out=pj[:, t, j * 512:j * 512 + w],
                                                                                   in_=bank(j)[:, 0:w])))(),
                             reads=["ps%d" % j], accum=[("pj", t)])
            PJ = [("pj", t) for t in range(TB)]
            act(sq[:, :, 0:C_BV], pj[:, :, 0:C_BV], AF.Square, PJ, ["sq"])
            P.op("act", lambda e: e.activation(out=sq[:, :, C_CQ2:C_CV2], in_=pj[:, :, C_CQ2:C_CV2], func=AF.Square),
                 reads=PJ, accum=["sq"])
            P.op("act", lambda e: e.activation(out=sq[:, :, C_DQ:C_DV], in_=pj[:, :, C_DQ:C_DV], func=AF.Square),
                 reads=PJ, accum=["sq"])
            red(ssg[:, :, 0:1], sq[:, :, 0:256].rearrange("p t (a e) -> p t a e", a=1), ALU.add, ["sq"], ["ssg"])
            P.op("dve", lambda e: e.tensor_reduce(out=ssg[:, :, 1:2], in_=sq[:, :, 256:384].rearrange("p t (a e) -> p t a e", a=1),
                                                  axis=AX.X, op=ALU.add), reads=["sq"], accum=["ssg"])
            P.op("dve", lambda e: e.tensor_reduce(out=ssg[:, :, 2:3], in_=sq[:, :, 384:416].rearrange("p t (a e) -> p t a e", a=1),
                                                  axis=AX.X, op=ALU.add), reads=["sq"], accum=["ssg"])
            P.op("dve", lambda e: e.tensor_reduce(out=ssg[:, :, 3:11], in_=sq[:, :, C_BQ:C_BV].rearrange("p t (a e) -> p t a e", a=8),
                                                  axis=AX.X, op=ALU.add), reads=["sq"], accum=["ssg"])
            P.op("dve", lambda e: e.tensor_reduce(out=ssg[:, :, 11:17], in_=sq[:, :, C_CQ2:C_CV2].rearrange("p t (a e) -> p t a e", a=6),
                                                  axis=AX.X, op=ALU.add), reads=["sq"], accum=["ssg"])
            P.op("dve", lambda e: e.tensor_reduce(out=ssg[:, :, 17:33], in_=sq[:, :, C_DQ:C_DV].rearrange("p t (a e) -> p t a e", a=16),
                                                  axis=AX.X, op=ALU.add), reads=["sq"], accum=["ssg"])
            sskr = ss1[bp][:, TB:2 * TB]
            P.op("dve", (lambda sskr=sskr: (lambda e: e.tensor_copy(out=sskr, in_=ssg[:, :, 2])))(), reads=["ssg"], writes=[R("sskr")])
            rstd_chain(ssg, bcast(inv_e1, 1, [128, TB, 33]), 33, "ssg", None)
            tt("dve", cqn[:, :, 0:256], pj[:, :, 0:256], ssg[:, :, 0:1].to_broadcast([128, TB, 256]), ALU.mult,
               PJ + ["ssg"], ["cqn"])
            P.op("dve", lambda e: e.tensor_tensor(out=cqn[:, :, 256:384], in0=pj[:, :, 256:384],
                                                  in1=ssg[:, :, 1:2].to_broadcast([128, TB, 128]), op=ALU.mult),
                 reads=PJ + ["ssg"], accum=["cqn"])
            for t in range(TB):
                def trc(e, t=t):
                    ins = None
                    for c in range(3):
                        ins = e.transpose(out=bank_bf(7)[:, c * 128:(c + 1) * 128], in_=cqn[:, t, c * 128:(c + 1) * 128],
                                          identity=ident_b)
                    return ins
                P.op("pe", trc, reads=["cqn"], writes=["ps7"])
                tt("dve", cT, bank_bf(7)[:, 0:384].rearrange("p (c k) -> p c k", c=3),
                   bcast(gv[:, L, GV_QN:GV_QN + 3], 2, [128, 3, 128]), ALU.mult, ["ps7"], ["cT"])
                def mm2(e):
                    for c in range(2):
                        e.matmul(bank(5)[:, 0:384], lhsT=cT[:, c, :], rhs=w_uq_sb[:, c, :], start=(c == 0), stop=(c == 1))
                    return e.matmul(bank(6), lhsT=cT[:, 2, :], rhs=w_ukv_sb, start=True, stop=True)
                P.op("pe", mm2, reads=["cT", "w_in"], writes=["ps5", "ps6"])
                act(q2[:, t, :], bank(5)[:, 0:384], AF.Copy, ["ps5"], [("q2", t)])
                P.op("dve", (lambda t=t: (lambda e: e.tensor_copy(out=kv2[:, t, :], in_=bank(6))))(), reads=["ps6"],
                     writes=[("kv2", t)])
            Q2 = [("q2", t) for t in range(TB)]
            KV2 = [("kv2", t) for t in range(TB)]
            kv4 = kv2.rearrange("p t (h e) -> p t h e", h=4)
            act(sq[:, :, 0:384], q2, AF.Square, Q2, ["sq"])
            P.op("act", lambda e: e.activation(out=sq[:, :, 384:896], in_=kv2, func=AF.Square), reads=KV2, accum=["sq"])
            red(ss2[:, :, 0:4], sq[:, :, 0:384].rearrange("p t (h e) -> p t h e", h=4), ALU.add, ["sq"], ["ss2"])
            P.op("dve", lambda e: e.tensor_reduce(out=ss2[:, :, 4:8],
                                                  in_=sq[:, :, 384:896].rearrange("p t (h e) -> p t h e", h=4)[:, :, :, 0:64],
                                                  axis=AX.X, op=ALU.add), reads=["sq"], accum=["ss2"])
            tt("dve", ss2[:, :, 4:8], ss2[:, :, 4:8], bcast(sskr, 2, [128, TB, 4]), ALU.add, ["ss2", R("sskr")], ["ss2"])
            rstd_chain(ss2, 1.0 / 96, 8, "ss2", None)
            qa4 = qk_a.rearrange("p t h e -> p (t h) e")
            tt("dve", qk_a[:, :, 0:4, :], q2.rearrange("p t (h e) -> p t h e", h=4),
               bcast(ss2[:, :, 0:4], 3, [128, TB, 4, 96]), ALU.mult, Q2 + ["ss2"], ["qk_a"])
            P.op("dve", lambda e: e.tensor_tensor(out=qk_a[:, :, 4:8, 0:64], in0=kv4[:, :, :, 0:64],
                                                  in1=bcast(ss2[:, :, 4:8], 3, [128, TB, 4, 64]), op=ALU.mult),
                 reads=KV2 + ["ss2"], accum=["qk_a"])
            for t in range(TB):
                P.op("dve", (lambda t=t: (lambda e: e.tensor_tensor(
                    out=qk_a[:, t, 4:8, 64:96], in0=bcast(pj[:, t, C_KR:C_KR + 32], 1, [128, 4, 32]),
                    in1=bcast(ss2[:, t, 4:8], 2, [128, 4, 32]), op=ALU.mult)))(), reads=PJ + ["ss2"], accum=["qk_a"])
            for t in range(TB):
                g2v = gb(GB_MLA, 192).rearrange("p (a e) -> p a e", a=2)
                P.op("dve", (lambda t=t, g2v=g2v: (lambda e: e.tensor_tensor(
                    out=qk_a[:, t].rearrange("p (a h) e -> p a h e", a=2), in0=qk_a[:, t].rearrange("p (a h) e -> p a h e", a=2),
                    in1=bcast(g2v, 2, [128, 2, 4, 96]), op=ALU.mult)))(), reads=["qk_a", "gbt"], writes=["qk_a"] if t == TB - 1 else [],
                    accum=[] if t == TB - 1 else ["qk_a"])
            def rope_apply(x4, nh, cos4, sin4, tag, tmp1, tmp2):
                tt("dve", tmp1, x4, cos4, ALU.mult, [tag, R("rope")], [tag + "_t1"])
                tt("dve", tmp2[:, :, :, 0:16], x4[:, :, :, 16:32], sin4[:, :, :, 0:16], ALU.mult, [tag, R("rope")], [tag + "_t2"])
                P.op("dve", lambda e: e.tensor_tensor(out=tmp2[:, :, :, 16:32], in0=x4[:, :, :, 0:16], in1=sin4[:, :, :, 16:32],
                                                      op=ALU.mult), reads=[tag, R("rope")], accum=[tag + "_t2"])
                tt("dve", x4, tmp1, tmp2, ALU.add, [tag + "_t1", tag + "_t2"], [tag])
            rp = rope[bp]
            rope_apply(qk_a[:, :, :, 64:96], 8, bcast(rp[:, :, 0, :], 2, [128, TB, 8, 32]), bcast(rp[:, :, 1, :], 2, [128, TB, 8, 32]),
                       "qk_a", rt1[:, :, 0:8, :], rt2[:, :, 0:8, :])
            P.op("act", lambda e: e.activation(out=qktm[:, :, SL_AQ:SL_AQ + 8, 0:96], in_=qk_a, func=AF.Copy), reads=["qk_a"],
                 accum=["qktm"])
            dq = pj[:, :, C_BQ:C_BV].rearrange("p t (h e) -> p t h e", h=8)
            dst = qktm[:, :, SL_BQ:SL_BQ + 4, :].rearrange("p t s (h e) -> p t (s h) e", h=2)
            tt("dve", dst, dq, bcast(ssg[:, :, 3:11], 3, [128, TB, 8, 64]), ALU.mult, PJ + ["ssg"], [], accum=["qktm"])
            for t in range(TB):
                gd = gb(GB_DIL, 128).rearrange("p (a e) -> p a e", a=2)
                dv = qktm[:, t, SL_BQ:SL_BQ + 4, :].rearrange("p (a s) (h e) -> p a (s h) e", a=2, h=2)
                P.op("dve", (lambda dv=dv, gd=gd: (lambda e: e.tensor_tensor(out=dv, in0=dv, in1=bcast(gd, 2, [128, 2, 4, 64]),
                                                                             op=ALU.mult)))(), reads=["qktm", "gbt"], accum=["qktm"])
            fq = pj[:, :, C_DQ:C_DV].rearrange("p t (h e) -> p t h e", h=16)
            fst = qktm[:, :, SL_DQ:SL_DQ + 4, :].rearrange("p t s (h e) -> p t (s h) e", h=4)
            P.op("dve", lambda e: e.tensor_tensor(out=fst, in0=fq, in1=bcast(ssg[:, :, 17:33], 3, [128, TB, 16, 32]), op=ALU.mult),
                 reads=PJ + ["ssg"], accum=["qktm"])
            for t in range(TB):
                gf = gb(GB_DIF, 64).rearrange("p (a e) -> p a e", a=2)
                fv = qktm[:, t, SL_DQ:SL_DQ + 4, :].rearrange("p (a s) (h e) -> p a (s h) e", a=2, h=4)
                P.op("dve", (lambda fv=fv, gf=gf: (lambda e: e.tensor_tensor(out=fv, in0=fv, in1=bcast(gf, 2, [128, 2, 8, 32]),
                                                                             op=ALU.mult)))(), reads=["qktm", "gbt"], accum=["qktm"])
            cq = pj[:, :, C_CQ2:C_CV2].rearrange("p t (h e) -> p t h e", h=6)
            tt("dve", qk_c, cq, bcast(ssg[:, :, 11:17], 3, [128, TB, 6, 64]), ALU.mult, PJ + ["ssg"], ["qk_c"])
            ggq = gb(GB_GQA, 64)
            ggk = gb(GB_GQA + 64, 64)
            tt("dve", qk_c[:, :, 0:4, :], qk_c[:, :, 0:4, :], bcast(bcast(ggq, 1, [128, 4, 64]), 1, [128, TB, 4, 64]), ALU.mult,
               ["qk_c", "gbt"], ["qk_c"])
            tt("dve", qk_c[:, :, 4:6, :], qk_c[:, :, 4:6, :], bcast(bcast(ggk, 1, [128, 2, 64]), 1, [128, TB, 2, 64]), ALU.mult,
               ["qk_c", "gbt"], ["qk_c"])
            for t in range(TB):
                xc = qk_c[:, t].rearrange("p h (a e) -> p h a e", a=2)
                cosv = bcast(rp[:, t, 2:6, :].rearrange("p (a b) e -> p a b e", b=2)[:, :, 0, :], 1, [128, 6, 2, 32])
                sinv = bcast(rp[:, t, 2:6, :].rearrange("p (a b) e -> p a b e", b=2)[:, :, 1, :], 1, [128, 6, 2, 32])
                t1 = rt1[:, t].rearrange("p (h a) e -> p h a e", a=2)
                t2 = rt2[:, t].rearrange("p (h a) e -> p h a e", a=2)
                tag = "qk_c"
                last = (t == TB - 1)
                P.op("dve", (lambda t1=t1, xc=xc, cosv=cosv: (lambda e: e.tensor_tensor(out=t1, in0=xc, in1=cosv, op=ALU.mult)))(),
                     reads=[tag, R("rope")], writes=[("ct1", t)])
                P.op("dve", (lambda t2=t2, xc=xc, sinv=sinv: (lambda e: e.tensor_tensor(
                    out=t2[:, :, :, 0:16], in0=xc[:, :, :, 16:32], in1=sinv[:, :, :, 0:16], op=ALU.mult)))(),
                    reads=[tag, R("rope")], writes=[("ct2", t)])
                P.op("dve", (lambda t2=t2, xc=xc, sinv=sinv: (lambda e: e.tensor_tensor(
                    out=t2[:, :, :, 16:32], in0=xc[:, :, :, 0:16], in1=sinv[:, :, :, 16:32], op=ALU.mult)))(),
                    reads=[tag, R("rope")], accum=[("ct2", t)])
                qd = qktm[:, t, SL_CQ:SL_CQ + 2, :].rearrange("p s (h a e) -> p (s h) a e", h=2, a=2)
                kd = qktm[:, t, SL_CK:SL_CK + 2, :].rearrange("p s (d f) -> p s d f", d=2)
                P.op("dve", (lambda qd=qd, t1=t1, t2=t2: (lambda e: e.tensor_tensor(out=qd, in0=t1[:, 0:4], in1=t2[:, 0:4], op=ALU.add)))(),
                     reads=[("ct1", t), ("ct2", t)], accum=["qktm"])
                k1 = bcast(rt1[:, t, 8:12, :].rearrange("p (h a) e -> p h (a e)", a=2), 2, [128, 2, 2, 64])
                k2 = bcast(rt2[:, t, 8:12, :].rearrange("p (h a) e -> p h (a e)", a=2), 2, [128, 2, 2, 64])
                P.op("dve", (lambda kd=kd, k1=k1, k2=k2: (lambda e: e.tensor_tensor(out=kd, in0=k1, in1=k2, op=ALU.add)))(),
                     reads=[("ct1", t), ("ct2", t)], accum=["qktm"])
            P.op("act", lambda e: e.activation(out=vst[:, :, V_A:V_A + 256].rearrange("p t (h e) -> p t h e", h=4),
                                               in_=kv4[:, :, :, 64:128], func=AF.Copy), reads=KV2, writes=["vst"])
            P.op("act", lambda e: e.activation(out=vst[:, :, V_B:V_B + 256], in_=pj[:, :, C_BV:C_BV + 256], func=AF.Copy),
                 reads=PJ, accum=["vst"])
            P.op("act", lambda e: e.activation(out=vst[:, :, V_C:V_C + 128], in_=pj[:, :, C_CV2:C_CV2 + 128], func=AF.Copy),
                 reads=PJ, accum=["vst"])
            P.op("act", lambda e: e.activation(out=vst[:, :, V_D:V_D + 256], in_=pj[:, :, C_DV:C_DV + 256], func=AF.Copy),
                 reads=PJ, accum=["vst"])
            dma("sp", VS[tok0:tok0 + TB * 128, :].rearrange("(t p) c -> p t c", p=128), vst, reads=["vst"], accum=["VS"],
                chan="a_vs")
            qb = (b // 2) % 2
            for t in range(TB):
                col0 = ((b % 2) * TB + t) * 128
                for g3 in range(3):
                    s0 = g3 * 8
                    ns = min(8, NSLOT - s0)
                    def trq(e, t=t, s0=s0, ns=ns, g3=g3):
                        ins = None
                        for s_ in range(ns):
                            ins = e.transpose(out=bank_bf(5 + g3)[:, s_ * 128:(s_ + 1) * 128], in_=qktm[:, t, s0 + s_, :],
                                              identity=ident_b)
                        return ins
                    P.op("pe", trq, reads=["qktm"], writes=["ps%d" % (5 + g3)])
                    src = bank_bf(5 + g3)[:, 0:ns * 128].rearrange("p (s k) -> p s k", s=ns)
                    dstq = qst[qb][:, s0:s0 + ns, col0:col0 + 128]
                    if g3 == 1:
                        P.op("act", (lambda dstq=dstq, src=src: (lambda e: e.activation(out=dstq, in_=src, func=AF.Copy)))(),
                             reads=["ps%d" % (5 + g3)], accum=[("qst", qb)])
                    else:
                        P.op("dve", (lambda dstq=dstq, src=src: (lambda e: e.tensor_copy(out=dstq, in_=src)))(),
                             reads=["ps%d" % (5 + g3)], accum=[("qst", qb)])
            if b % 2 == 1:
                tq0 = (b - 1) * TB * 128
                dma("sp", QT[:, :, tq0:tq0 + 512].rearrange("s p k -> p s k"), qst[qb], reads=[("qst", qb)], accum=["QT"],
                    chan=("a_qt", qb))
        P.barrier()
        AR.reset(m)

    def phase_B(L):
        m = AR.mark()
        vsb = [AR.take([128, 16, 4, 128], BF16) for _ in range(2)]
        ot = [AR.take([128, 512], F32) for _ in range(2)]
        for i in range(2):
            P.op("pool", (lambda v: (lambda e: e.memset(v, 1.0)))(vsb[i]), writes=[("vsb", i)])
        qT = [AR.take([128, 4, S], BF16) for _ in range(2)]
        kT = [AR.take([128, 8, S], BF16) for _ in range(2)]
        strips = AR.take([128, 4, STRIP_W], BF16)
        pt = [AR.take([128, 512], BF16) for _ in range(6)]
        SBANKS = (0, 1, 2, 5, 6)
        ybuf = [AR.take([128, 4, 256], F32) for _ in range(2)]
        rec = AR.take([128, 8], F32)
        t0b = AR.take([128, 4, 64], F32)
        t1b = AR.take([128, 4, 64], F32)

        mixers = [("A", SL_AQ, 4, SL_AK, 4, V_A, 4, None),
                  ("C", SL_CQ, 2, SL_CK, 2, V_C, 2, None),
                  ("B", SL_BQ, 2, SL_BK, 2, V_B, 4, 0),
                  ("D", SL_DQ, 2, SL_DK, 2, V_D, 4, 4)]
        ycol = {"A": 0, "B": 256, "C": 512, "D": 768}
        units = [(mx, sq_) for mx in mixers for sq_ in range(SEQ_PER_CORE)]
        state = {"step": 0, "ob": 0, "yb": 0, "ot": 0}
        cur_strip = [None]

        def load_unit(u, par):
            (name, qs, nq, ks, nk, vc, nvh, sbase), sq_ = u
            tk0 = sq_ * S
            for i in range(nq):
                dma("sp", qT[par][:, i, :], QT[qs + i, :, tk0:tk0 + S], reads=["QT"], accum=[("qT", par)], chan=("b_q", par))
            if name == "A":
                for i in range(nk):
                    dma("sp", kT[par][:, i, :], QT[ks + i, :, tk0:tk0 + S], reads=["QT"], accum=[("kT", par)], chan=("b_k", par))
            else:
                ntile = 8 if name == "D" else 4
                P.op("pool", (lambda a=kT[par][:, 0:ntile, :]: (lambda e: e.memset(a, 0.0)))(), writes=[("kT", par)])
                for i in range(2):
                    for hh in range(2):
                        h = 2 * i + hh
                        if name == "D":
                            for c in range(2):
                                r0 = 64 * hh + 32 * c
                                dma("sp", kT[par][r0:r0 + 32, 2 * h + c, :], QT[ks + i, r0:r0 + 32, tk0:tk0 + S], reads=["QT"],
                                    accum=[("kT", par)], chan=("b_k", par))
                        else:
                            r0 = 64 * hh
                            dma("sp", kT[par][r0:r0 + 64, h, :], QT[ks + i, r0:r0 + 64, tk0:tk0 + S], reads=["QT"],
                                accum=[("kT", par)], chan=("b_k", par))
            for h in range(nvh):
                dma("sp", vsb[par][:, :, h, 0:64],
                    VS[tk0:tk0 + S, vc + h * 64:vc + (h + 1) * 64].rearrange("(c p) e -> p c e", p=128),
                    reads=["VS"], accum=[("vsb", par)], chan=("b_v", par))

        def head_maps(name):
            hm = []
            if name == "A":
                for h in range(4):
                    hm.append((h, 0, 96, h, h, 96 ** -0.5, None, h * 64, "plain"))
            elif name == "C":
                for h in range(4):
                    hm.append((h // 2, 0, 128, h, h // 2, 64 ** -0.5, None, h * 64, "plain"))
            elif name == "B":
                for h in range(4):
                    hm.append((h // 2, 0, 128, h, h, 64 ** -0.5, h, h * 64, "plain"))
            else:
                for h in range(4):
                    for c in range(2):
                        hm.append((h // 2, 0, 128, 2 * h + c, h, 32 ** -0.5, h, h * 64, "d%d" % c))
            return hm

        load_unit(units[0], 0)
        pe_warmup()
        for ui, u in enumerate(units):
            par = ui % 2
            (name, qs, nq, ks, nk, vc, nvh, sbase), sq_ = u
            if ui + 1 < len(units):
                load_unit(units[ui + 1], 1 - par)
            if sbase is not None and cur_strip[0] != sbase:
                for i in range(4):
                    dma("sp", strips[:, i, :], SD[sbase + i], reads=["SD"], accum=["strips"], chan="b_s")
                cur_strip[0] = sbase
            hms = head_maps(name)
            for qc in range(4):
                yb = state["yb"]
                state["yb"] = 1 - yb
                tiles = []
                for hi, hmv in enumerate(hms):
                    kcs = list(range(16))
                    if name == "B":
                        kcs = [kc for kc in kcs if not (kc * 128 - qc * 512 - 511 > 1024 or kc * 128 + 127 - qc * 512 < -1024)]
                    for j, kc in enumerate(kcs):
                        tiles.append((hi, hmv, kc, j == 0, j == len(kcs) - 1))
                LOOK = 4
                ob_of_head = {}

                def emit_scores(idx):
                    hi, hmv, kc, first, last = tiles[idx]
                    qsl, r0, nr, kt, vh, scale, sidx, ycl, kind = hmv
                    step = state["step"] + idx
                    sb_ = SBANKS[step % 5]
                    krow0 = r0
                    lhsT = kT[par][krow0:krow0 + nr, kt, kc * 128:(kc + 1) * 128]
                    rhs = qT[par][r0:r0 + nr, qsl, qc * 512:(qc + 1) * 512]
                    P.op("pe", (lambda sb_=sb_, lhsT=lhsT, rhs=rhs: (lambda e: e.matmul(bank(sb_), lhsT=lhsT, rhs=rhs, start=True,
                                                                                      stop=True)))(),
                         reads=[("qT", par), ("kT", par)], writes=["ps%d" % sb_])

                def emit_rest(idx):
                    hi, hmv, kc, first, last = tiles[idx]
                    qsl, r0, nr, kt, vh, scale, sidx, ycl, kind = hmv
                    step = state["step"] + idx
                    sb_ = SBANKS[step % 5]
                    pb = step % 6
                    if first:
                        ob_of_head[hi] = state["ob"]
                        state["ob"] = 1 - state["ob"]
                    ob = 3 + ob_of_head[hi]
                    act(pt[pb], bank(sb_), AF.Exp, ["ps%d" % sb_], [("pt", pb)], scale=scale)
                    if sidx is not None:
                        j0 = STRIP_OFF - kc * 128 + qc * 512
                        eng = "dve"
                        tt(eng, pt[pb], pt[pb], strips[:, sidx, j0:j0 + 512], ALU.mult, [("pt", pb), "strips"], [("pt", pb)])
                    def pv(e, pb=pb, ob=ob, kc=kc, vh=vh, first=first, last=last, par=par):
                        return e.matmul(bank(ob), lhsT=vsb[par][:, kc, vh, :], rhs=pt[pb], start=first, stop=last)
                    P.op("pe", pv, reads=[("pt", pb), ("vsb", par)], writes=["ps%d" % ob])
                    if last:
                        def fin(ob=ob, kind=kind, ycl=ycl):
                            oi = state["ot"]
                            state["ot"] = 1 - oi
                            otb = ot[oi]
                            act(otb, bank(ob), AF.Copy, ["ps%d" % ob], [("ot", oi)])
                            def trf(e, otb=otb):
                                ins = None
                                for qs_ in range(4):
                                    ins = e.transpose(out=bank(7)[:, qs_ * 128:(qs_ + 1) * 128], in_=otb[:, qs_ * 128:(qs_ + 1) * 128],
                                                      identity=ident_f)
                                return ins
                            P.op("pe", trf, reads=[("ot", oi)], writes=["ps7"])
                            o3 = bank(7).rearrange("p (q e) -> p q e", q=4)
                            rc = rec[:, 0:4] if kind != "d1" else rec[:, 4:8]
                            rtag = "rec0" if kind != "d1" else "rec1"
                            recip(rc, o3[:, :, 64], ["ps7"], [rtag])
                            yv = ybuf[yb][:, :, ycl:ycl + 64]
                            if kind == "plain":
                                P.op("dve", (lambda yv=yv, o3=o3, rc=rc: (lambda e: e.tensor_tensor(
                                    out=yv, in0=o3[:, :, 0:64], in1=bcast(rc, 2, [128, 4, 64]), op=ALU.mult)))(),
                                    reads=["ps7", rtag], accum=[("ybuf", yb)])
                            elif kind == "d0":
                                tt("dve", t0b, o3[:, :, 0:64], bcast(rc, 2, [128, 4, 64]), ALU.mult, ["ps7", rtag], ["t0b"])
                            else:
                                tt("dve", t1b, o3[:, :, 0:64], bcast(rc, 2, [128, 4, 64]), ALU.mult, ["ps7", rtag], ["t1b"])
                                P.op("dve", (lambda yv=yv: (lambda e: e.scalar_tensor_tensor(
                                    out=yv, in0=t1b, scalar=neglam[:, L:L + 1], in1=t0b, op0=ALU.mult, op1=ALU.add)))(),
                                    reads=["t0b", "t1b"], accum=[("ybuf", yb)])
                        pending.append((idx + 2, fin))

                n = len(tiles)
                pending = []
                for i in range(min(LOOK, n)):
                    emit_scores(i)
                for i in range(n):
                    if i + LOOK < n:
                        emit_scores(i + LOOK)
                    emit_rest(i)
                    while pending and pending[0][0] <= i:
                        pending.pop(0)[1]()
                while pending:
                    pending.pop(0)[1]()
                state["step"] += n
                tk = sq_ * S + qc * 512
                dma("sp", Y[tk:tk + 512, ycol[name]:ycol[name] + 256].rearrange("(q p) c -> p q c", p=128), ybuf[yb],
                    reads=[("ybuf", yb)], accum=["Y"], chan=("b_y", yb))
        P.barrier()
        AR.reset(m)

    def phase_C(L, x_src):
        m = AR.mark()
        w_out_sb = AR.take([128, 8, D], BF16)
        for c in range(8):
            dma("pool", w_out_sb[:, c, :], w_out[L, c * 128:(c + 1) * 128, :], accum=["w_out"], chan="c_w")
        yt = [AR.take([128, D], F32) for _ in range(2)]
        xt = [AR.take([128, D], F32) for _ in range(2)]
        sqc = AR.take([128, D], F32)
        ssc = [AR.take([128, 8], F32) for _ in range(2)]
        ybf = [AR.take([128, D], BF16) for _ in range(2)]
        mT = [AR.take([128, 8, 128], BF16) for _ in range(2)]
        xo = [AR.take([128, D], F32) for _ in range(2)]
        inv_c = AR.take([128, 8], F32)
        P.op("dve", lambda e: e.memset(inv_c[:, 0:3], 1.0 / 256), writes=["inv_c"])
        P.op("dve", lambda e: e.memset(inv_c[:, 3:8], 1.0 / 64), accum=["inv_c"])
        for t in range(NT):
            p = t % 2
            R = lambda n_: (n_, p)
            dma("sp", yt[p], Y[t * 128:(t + 1) * 128, :], reads=["Y"], writes=[R("yt")], chan=("c_y", p))
            dma("sp", xt[p], x_src[t * 128:(t + 1) * 128, :], reads=["Xsrc"], writes=[R("xt")], chan=("c_x", p))
            act(sqc, yt[p], AF.Square, [R("yt")], ["sqc"])
            red(ssc[p][:, 0:3], sqc[:, 0:768].rearrange("p (a e) -> p a e", a=3), ALU.add, ["sqc"], [R("ssc")])
            P.op("dve", (lambda p=p: (lambda e: e.tensor_reduce(out=ssc[p][:, 3:7], in_=sqc[:, 768:1024].rearrange("p (a e) -> p a e", a=4),
                                                                 axis=AX.X, op=ALU.add)))(), reads=["sqc"], accum=[R("ssc")])
            rstd_chain(ssc[p][:, 0:7], inv_c[:, 0:7], 7, R("ssc"), None)
            tt("dve", ybf[p][:, 0:768].rearrange("p (a e) -> p a e", a=3), yt[p][:, 0:768].rearrange("p (a e) -> p a e", a=3),
               bcast(ssc[p][:, 0:3], 2, [128, 3, 256]), ALU.mult, [R("yt"), R("ssc")], [R("ybf")])
            P.op("dve", (lambda p=p: (lambda e: e.tensor_tensor(
                out=ybf[p][:, 768:1024].rearrange("p (a e) -> p a e", a=4), in0=yt[p][:, 768:1024].rearrange("p (a e) -> p a e", a=4),
                in1=bcast(ssc[p][:, 3:7], 2, [128, 4, 64]), op=ALU.mult)))(), reads=[R("yt"), R("ssc")], accum=[R("ybf")])
            def trf(e, p=p):
                ins = None
                for c in range(8):
                    ins = e.transpose(out=bank_bf(7)[:, c * 128:(c + 1) * 128], in_=ybf[p][:, c * 128:(c + 1) * 128], identity=ident_b)
                return ins
            P.op("pe", trf, reads=[R("ybf")], writes=["ps7"])
            tt("dve", mT[p], bank_bf(7).rearrange("p (c k) -> p c k", c=8), bcast(gv[:, L, GV_BETA:GV_BETA + 8], 2, [128, 8, 128]),
               ALU.mult, ["ps7"], [R("mT")])
            def mmf(e, p=p):
                ins = None
                for j in range(2):
                    for c in range(8):
                        ins = e.matmul(bank(j), lhsT=mT[p][:, c, :], rhs=w_out_sb[:, c, j * 512:(j + 1) * 512], start=(c == 0),
                                       stop=(c == 7))
                return ins
            P.op("pe", mmf, reads=[R("mT"), "w_out"], writes=["ps0", "ps1"])
            tt("dve", xo[p][:, 0:512], xt[p][:, 0:512], bank(0), ALU.add, [R("xt"), "ps0"], [R("xo0")])
            tt("dve", xo[p][:, 512:1024], xt[p][:, 512:1024], bank(1), ALU.add, [R("xt"), "ps1"], [R("xo1")])
            dma("pool", X1[t * 128:(t + 1) * 128, :], xo[p], reads=[R("xo0"), R("xo1")], accum=["X1"], chan=("c_o", p))
        P.barrier()
        AR.reset(m)

    def phase_D(L, dst):
        m = AR.mark()
        wr = AR.take([128, 8, 20], F32)
        dma("sp", wr, router_w[L].rearrange("(c p) n -> p c n", p=128), writes=["wr"], chan="d_wr")
        hT = AR.take([128, 8, S], BF16)
        hTf = AR.take([128, 8, 128], F32)
        yacc = AR.take([128, 16, D], F32)
        lg = AR.take([128, 16, 20], F32)
        gate = AR.take([128, 16, 16], F32)
        xs = [AR.take([128, D], F32) for _ in range(2)]
        hf = [AR.take([128, D], F32) for _ in range(2)]
        ss = [AR.take([128, 2], F32) for _ in range(2)]
        wg = [AR.take([128, 8, DFF], BF16) for _ in range(2)]
        wu = [AR.take([128, 8, DFF], BF16) for _ in range(2)]
        wd = [AR.take([128, 4, D], BF16) for _ in range(2)]
        sg = [AR.take([128, 512], BF16) for _ in range(2)]
        hid = [AR.take([128, 4, 512], BF16) for _ in range(2)]
        r1 = AR.take([128, 16, 4], F32)
        r2 = AR.take([128, 16, 4], F32)
        r3 = AR.take([128, 16], F32)
        r4 = AR.take([128, 16], F32)
        e1 = AR.take([128, 16, 16], F32)
        e2 = AR.take([128, 16, 16], F32)
        r5 = AR.take([128, 16], F32)

        def load_expert(e_, par, seqi):
            for c in range(0, 8, 4):
                dma("pool", wg[par][:, c:c + 4, :], w_gate[L, e_, c * 128:(c + 4) * 128, :].rearrange("(c p) f -> p c f", p=128),
                    accum=[("wg", par)], chan=("d_w", par))
                dma("pool", wu[par][:, c:c + 4, :], w_up[L, e_, c * 128:(c + 4) * 128, :].rearrange("(c p) f -> p c f", p=128),
                    accum=[("wu", par)], chan=("d_w", par))
            dma("pool", wd[par], w_down[L, e_].rearrange("(c p) d -> p c d", p=128), accum=[("wd", par)], chan=("d_w", par))

        for sq_ in range(SEQ_PER_CORE):
            load_expert(0, 0, sq_)
            for t in range(16):
                p = t % 2
                R = lambda n_: (n_, p)
                tok = sq_ * S + t * 128
                dma("sp", xs[p], X1[tok:tok + 128, :], reads=["X1"], writes=[R("xs")], chan=("d_x", p))
                act(hf[p], xs[p], AF.Square, [R("xs")], [R("ss"), R("hf")], accum_out=ss[p][:, 0:1])
                rstd_chain(ss[p][:, 0:1], 1.0 / D, 1, R("ss"), None)
                act(hf[p], xs[p], AF.Copy, [R("xs"), R("ss")], [R("hf")], scale=ss[p][:, 0:1])
                P.op("pool", (lambda t=t, p=p: (lambda e: e.tensor_copy(out=yacc[:, t, :], in_=xs[p])))(), reads=[R("xs")],
                     writes=[("yacc", t)])
                for half in range(2):
                    def trf(e, p=p, half=half):
                        ins = None
                        for c in range(4):
                            cc = half * 4 + c
                            ins = e.transpose(out=bank(6 + half)[:, c * 128:(c + 1) * 128], in_=hf[p][:, cc * 128:(cc + 1) * 128],
                                              identity=ident_f)
                        return ins
                    P.op("pe", trf, reads=[R("hf")], writes=["ps%d" % (6 + half)])
                    g2 = gv[:, L, GV_G2 + 4 * half:GV_G2 + 4 * half + 4]
                    src = bank(6 + half).rearrange("p (c k) -> p c k", c=4)
                    P.op("dve", (lambda src=src, g2=g2, half=half: (lambda e: e.tensor_tensor(
                        out=hTf[:, 4 * half:4 * half + 4, :], in0=src, in1=bcast(g2, 2, [128, 4, 128]), op=ALU.mult)))(),
                        reads=["ps%d" % (6 + half)], accum=["hTf"] if half else [], writes=[] if half else ["hTf"])
                    P.op("act", (lambda src=src, half=half, t=t: (lambda e: e.activation(
                        out=hT[:, 4 * half:4 * half + 4, t * 128:(t + 1) * 128], in_=hTf[:, 4 * half:4 * half + 4, :], func=AF.Copy)))(),
                        reads=["hTf"], accum=["hT"])
                def mmr(e):
                    ins = None
                    for c in range(8):
                        ins = e.matmul(bank(5)[:, 0:20], lhsT=hTf[:, c, :], rhs=wr[:, c, :], start=(c == 0), stop=(c == 7))
                    return ins
                P.op("pe", mmr, reads=["hTf", "wr"], writes=["ps5"])
                P.op("dve", (lambda t=t: (lambda e: e.tensor_tensor(out=lg[:, t, :], in0=bank(5)[:, 0:20],
                                                                    in1=gbt[:, L, GB_RB:GB_RB + 20], op=ALU.add)))(),
                     reads=["ps5"], accum=["lg"])
            gl = lg[:, :, 0:4]
            el = lg[:, :, 4:20].rearrange("p t (g j) -> p t g j", g=4)
            red(r3, gl, ALU.max, ["lg"], ["r3"])
            tt("dve", r1, gl, bcast(r3, 2, [128, 16, 4]), ALU.is_ge, ["lg", "r3"], ["r1"])
            tt("dve", r2, gl, bcast(r3, 2, [128, 16, 4]), ALU.subtract, ["lg", "r3"], ["r2"])
            act(r2, r2, AF.Exp, ["r2"], ["r2"])
            red(r4, r2, ALU.add, ["r2"], ["r4"])
            recip(r4, r4, ["r4"], ["r4"])
            ts("dve", r1, r1, -1.0, 1.0e4, ALU.add, ALU.mult, ["r1"], ["r1"])
            tt("dve", e1.rearrange("p t (g j) -> p t g j", g=4), el, bcast(r1, 3, [128, 16, 4, 4]), ALU.add, ["lg", "r1"], ["e1"])
            red(r3, e1, ALU.max, ["e1"], ["r3"])
            tt("dve", e1, e1, bcast(r3, 2, [128, 16, 16]), ALU.subtract, ["e1", "r3"], ["e1"])
            ts("dve", e2, e1, 0.0, -2.0, ALU.is_ge, ALU.mult, ["e1"], ["e2"])
            act(e1, e1, AF.Exp, ["e1"], ["e1"])
            tt("dve", e2, e2, e1, ALU.add, ["e2", "e1"], ["e2"])
            red(r5, e2, ALU.max, ["e2"], ["r5"])
            tt("dve", e2, e1, bcast(r5, 2, [128, 16, 16]), ALU.is_ge, ["e1", "r5"], ["e2"])
            tt("dve", e2, e2, e1, ALU.mult, ["e2", "e1"], ["e2"])
            red(r5, e2, ALU.add, ["e2"], ["r5"])
            recip(r5, r5, ["r5"], ["r5"])
            tt("dve", r5, r5, r4, ALU.mult, ["r5", "r4"], ["r5"])
            tt("dve", gate, e2, bcast(r5, 2, [128, 16, 16]), ALU.mult, ["e2", "r5"], ["gate"])
            step = 0
            for e_ in range(NE):
                par = e_ % 2
                if e_ + 1 < NE:
                    load_expert(e_ + 1, 1 - par, sq_)
                for blk in range(4):
                    hb_ = (e_ * 4 + blk) % 2
                    for fc in range(4):
                        gp = step % 2
                        step += 1
                        def gu(e, gp=gp, fc=fc, blk=blk, par=par):
                            ins = None
                            for c in range(8):
                                ins = e.matmul(bank(gp), lhsT=wg[par][:, c, fc * 128:(fc + 1) * 128],
                                               rhs=hT[:, c, blk * 512:(blk + 1) * 512], start=(c == 0), stop=(c == 7))
                            for c in range(8):
                                ins = e.matmul(bank(2 + gp), lhsT=wu[par][:, c, fc * 128:(fc + 1) * 128],
                                               rhs=hT[:, c, blk * 512:(blk + 1) * 512], start=(c == 0), stop=(c == 7))
                            return ins
                        P.op("pe", gu, reads=["hT", ("wg", par), ("wu", par)], writes=["ps%d" % gp, "ps%d" % (2 + gp)])
                        act(sg[gp], bank(gp), AF.Silu, ["ps%d" % gp], [("sg", gp)])
                        P.op("dve", (lambda hb_=hb_, fc=fc, gp=gp: (lambda e: e.tensor_tensor(
                            out=hid[hb_][:, fc, :], in0=bank(2 + gp), in1=sg[gp], op=ALU.mult)))(),
                            reads=["ps%d" % (2 + gp), ("sg", gp)], accum=[("hid", hb_)] if fc else [],
                            writes=[] if fc else [("hid", hb_)])
                    for tt_ in range(4):
                        t = blk * 4 + tt_
                        for half in range(2):
                            ob = 4 + (tt_ * 2 + half) % 2
                            def dn(e, hb_=hb_, tt_=tt_, half=half, ob=ob, par=par):
                                ins = None
                                for fc in range(4):
                                    ins = e.matmul(bank(ob), lhsT=hid[hb_][:, fc, tt_ * 128:(tt_ + 1) * 128],
                                                   rhs=wd[par][:, fc, half * 512:(half + 1) * 512], start=(fc == 0), stop=(fc == 3))
                                return ins
                            P.op("pe", dn, reads=[("hid", hb_), ("wd", par)], writes=["ps%d" % ob])
                            ya = yacc[:, t, half * 512:(half + 1) * 512]
                            P.op("dve", (lambda ya=ya, ob=ob, t=t, e_=e_: (lambda e: e.scalar_tensor_tensor(
                                out=ya, in0=bank(ob), scalar=gate[:, t, e_:e_ + 1], in1=ya, op0=ALU.mult, op1=ALU.add)))(),
                                reads=["ps%d" % ob, "gate", ("yacc", t)], writes=[("yacc", t)])
            for t in range(16):
                tok = sq_ * S + t * 128
                dma("sp", dst[tok:tok + 128, :], yacc[:, t, :], reads=[("yacc", t)], accum=["DST"], chan="d_o")
        P.barrier()
        AR.reset(m)

    cur = x_in
    for L in range(n_layers):
        phase_A(L, cur)
        if stop_after == ("A", L):
            break
        phase_B(L)
        if stop_after == ("B", L):
            break
        phase_C(L, cur)
        if stop_after == ("C", L):
            break
        dst = out_d if L == n_layers - 1 else X2
        phase_D(L, dst)
        cur = X2
    if dbg:
        for name, src, shape, dt in (("dbg_Y", Y, [T, D], F32), ("dbg_X1", X1, [T, D], F32), ("dbg_QT", QT, [NSLOT, 128, T], BF16),
                                     ("dbg_VS", VS, [T, NV], BF16)):
            o = nc.dram_tensor(name, shape, dt, kind="ExternalOutput").ap()
            dma("sp", o, src, chan="dbg")
        P.barrier()
    P.emit()
    return nc


def _rel_bucket(rel):
    half = 16
    max_exact = 8
    n = np.abs(rel)
    nf = np.maximum(n, 1).astype(np.float32)
    log_ratio = np.log(nf / max_exact) / math.log(1024 / max_exact)
    large = np.minimum(max_exact + (log_ratio * (half - max_exact)).astype(np.int32), half - 1)
    return np.where(rel > 0, half, 0) + np.where(n < max_exact, n, large)


def _rope_tab():
    def cs(pos, dim):
        inv = 1.0 / (10000.0 ** (np.arange(0, dim, 2, dtype=np.float32) / dim))
        ang = pos.astype(np.float32)[:, None] * inv[None, :]
        ang = np.concatenate([ang, ang], -1)
        c, s = np.cos(ang), np.sin(ang)
        s = np.concatenate([-s[:, :dim // 2], s[:, dim // 2:]], -1)
        return c.astype(np.float32), s.astype(np.float32)
    pos = np.arange(S)
    tabs = []
    for p in (pos, pos // 64, pos % 64):
        c, s = cs(p, 32)
        tabs += [c, s]
    return np.ascontiguousarray(np.stack(tabs, 1).reshape(S, 6 * 32))


def _strip_index():
    p = np.arange(128)[:, None]
    j = np.arange(STRIP_W)[None, :]
    delta = p - j + STRIP_OFF
    return delta


_CACHE = {}


def _prepare_consts():
    if "c" in _CACHE:
        return _CACHE["c"]
    delta = _strip_index()
    bidx = _rel_bucket(delta)
    ad = np.abs(delta)
    mult = ((ad <= 64).astype(np.float32) + ((delta % 4 == 0) & (ad <= 256)).astype(np.float32)
            + ((delta % 16 == 0) & (ad <= 1024)).astype(np.float32))
    _CACHE["c"] = (bidx, np.ascontiguousarray(mult), _rope_tab())
    return _CACHE["c"]


def _in_maps(inputs):
    bidx, mult, rope = _prepare_consts()
    f = lambda a: np.ascontiguousarray(np.asarray(a, dtype=np.float32))
    rel_bias = f(inputs["rel_bias"])
    g = rel_bias[bidx]
    strip_dil = np.ascontiguousarray(np.transpose(g[:, :, 0:4], (2, 0, 1)))
    strip_dif = np.ascontiguousarray(np.transpose(g[:, :, 4:8], (2, 0, 1)))
    common = {
        "norm1_g": f(inputs["norm1_g"]), "w_in": f(inputs["w_in"]), "mla_q_norm_g": f(inputs["mla_q_norm_g"]),
        "mla_kv_norm_g": f(inputs["mla_kv_norm_g"]), "mla_w_uq": f(inputs["mla_w_uq"]), "mla_w_ukv": f(inputs["mla_w_ukv"]),
        "mla_qk_g": f(inputs["mla_qk_g"]).reshape(DEPTH, 192), "dil_qk_g": f(inputs["dil_qk_g"]).reshape(DEPTH, 128),
        "gqa_qk_g": f(inputs["gqa_qk_g"]).reshape(DEPTH, 128), "diff_qk_g": f(inputs["diff_qk_g"]).reshape(DEPTH, 64),
        "diff_lambda": f(inputs["diff_lambda"]).reshape(DEPTH, 128), "diff_subln_g": f(inputs["diff_subln_g"]),
        "mix_beta": f(inputs["mix_beta"]), "w_out": f(inputs["w_out"]), "norm2_g": f(inputs["norm2_g"]),
        "router_w": np.ascontiguousarray(np.concatenate([f(inputs["router_group_w"]), f(inputs["router_expert_w"])], -1)),
        "router_b": np.ascontiguousarray(np.concatenate([f(inputs["router_group_b"]), f(inputs["router_expert_b"])], -1)),
        "expert_w_gate": f(inputs["expert_w_gate"]), "expert_w_up": f(inputs["expert_w_up"]),
        "expert_w_down": f(inputs["expert_w_down"]),
        "rope_tab": rope, "strip_dil": strip_dil, "strip_dif": strip_dif, "strip_mult": mult,
    }
    x = f(inputs["x"])
    maps = []
    for c in range(NCORES):
        mp = dict(common)
        mp["x"] = np.ascontiguousarray(x[c * SEQ_PER_CORE:(c + 1) * SEQ_PER_CORE].reshape(T, D))
        maps.append(mp)
    return maps


def kernel(**inputs):
    if "nc" not in _CACHE:
        _CACHE["nc"] = build_program()
    nc = _CACHE["nc"]
    maps = _in_maps(inputs)
    res = run_bass_kernel_spmd(nc, maps, core_ids=list(range(NCORES)))
    out = np.stack([np.asarray(r["out"]).reshape(SEQ_PER_CORE, S, D) for r in res.results], 0)
    return out.reshape(BATCH, S, D).astype(np.float32)
```

```python
import math
import contextlib
import numpy as np
import ml_dtypes
import concourse.bass as bass
import concourse.mybir as mybir
from concourse.bass_utils import run_bass_kernel_spmd

F32 = mybir.dt.float32
BF16 = mybir.dt.bfloat16
AF = mybir.ActivationFunctionType
ALU = mybir.AluOpType
AX = mybir.AxisListType

NCORES = 8
D = 1024
S = 2048
BATCH = 16
SEQ_PER_CORE = BATCH // NCORES
T = SEQ_PER_CORE * S
NT = T // 128
DEPTH = 2
IN_COLS = 2464
EPS = 1e-6
NE = 16
DFF = 512
STRIP_W = 3968
STRIP_OFF = 1920

C_CQ, C_CKV, C_KR = 0, 256, 384
C_BQ, C_BK, C_BV = 416, 672, 928
C_CQ2, C_CK2, C_CV2 = 1184, 1440, 1568
C_DQ, C_DK, C_DV = 1696, 1952, 2208
SL_AQ, SL_AK, SL_BQ, SL_BK, SL_CQ, SL_CK, SL_DQ, SL_DK = 0, 4, 8, 10, 12, 14, 16, 18
NSLOT = 20
V_A, V_B, V_C, V_D = 0, 256, 512, 640
NV = 896


class Op:
    __slots__ = ("eng", "fn", "deps", "signal", "semval", "chan", "chanval", "pos", "epoch")

    def __init__(self, eng, fn, chan=None):
        self.eng = eng
        self.fn = fn
        self.deps = []
        self.signal = False
        self.semval = None
        self.chan = chan
        self.chanval = None
        self.pos = 0


class Prog:
    ENGS = ("pe", "act", "dve", "pool", "sp")

    def __init__(self, nc):
        self.nc = nc
        self.ops = {e: [] for e in self.ENGS}
        self.writers = {}
        self.readers = {}
        self.chan_count = {}
        self.dma_since_barrier = {}
        self.epoch = 0
        self.chan_map = {}

    @staticmethod
    def _key(o):
        return (o.eng, o.chan)

    def _prune_add(self, lst, o):
        k = self._key(o)
        lst[:] = [x for x in lst if self._key(x) != k]
        lst.append(o)

    def op(self, eng, fn, reads=(), writes=(), accum=(), chan=None, extra_deps=()):
        assert fn is not None or not (reads or writes or accum)
        if chan is not None:
            m = self.chan_map.setdefault(self.epoch, {})
            if chan not in m:
                m[chan] = len(m)
            chan = m[chan]
        o = Op(eng, fn, chan)
        o.epoch = self.epoch
        deps = list(extra_deps)
        for r in reads:
            deps += self.writers.get(r, [])
        for w in tuple(writes) + tuple(accum):
            deps += self.writers.get(w, [])
            deps += self.readers.get(w, [])
        for r in reads:
            self._prune_add(self.readers.setdefault(r, []), o)
        for w in writes:
            self.writers[w] = [o]
            self.readers[w] = []
        for w in accum:
            self._prune_add(self.writers.setdefault(w, []), o)
        if chan is not None:
            self.chan_count[chan] = self.chan_count.get(chan, 0) + 16
            o.chanval = self.chan_count[chan]
            self.dma_since_barrier[chan] = o
        best = {}
        for d in deps:
            if d is o or d.fn is None:
                continue
            if eng == "pe" and d.eng == "pe" and d.chan is None:
                continue
            k = self._key(d)
            b = best.get(k)
            if b is None:
                best[k] = d
            elif d.chan is not None:
                if d.chanval > b.chanval:
                    best[k] = d
            elif d.pos > b.pos:
                best[k] = d
        o.deps = list(best.values())
        for d in o.deps:
            if d.chan is None:
                d.signal = True
        o.pos = len(self.ops[eng])
        self.ops[eng].append(o)
        return o

    def barrier(self):
        deps = []
        for e in self.ENGS:
            for x in reversed(self.ops[e]):
                if x.chan is None and x.fn is not None:
                    deps.append(x)
                    break
        deps += list(self.dma_since_barrier.values())
        self.dma_since_barrier = {}
        for e in self.ENGS:
            self.op(e, None, extra_deps=deps)
        self.writers = {}
        self.readers = {}
        self.epoch += 1

    def emit(self):
        nc = self.nc
        for e in self.ENGS:
            c = {}
            for o in self.ops[e]:
                if o.chan is None and o.signal:
                    c[o.epoch] = c.get(o.epoch, 0) + 1
                    o.semval = c[o.epoch]
        chans = sorted(self.chan_count.keys(), key=str)
        with contextlib.ExitStack() as st:
            used = set()
            for e in self.ENGS:
                for o in self.ops[e]:
                    if o.chan is None and o.signal:
                        used.add((e, o.epoch))
            esem = {k: st.enter_context(nc.semaphore("sem_%s_%d" % k)) for k in sorted(used)}
            print("[prog] semaphores: %d engine, %d dma channels" % (len(esem), len(chans)))
            csem = {c: st.enter_context(nc.semaphore("ch_%d" % i)) for i, c in enumerate(chans)}
            block = st.enter_context(nc.Block())

            def run(ename):
                def body(eng):
                    waited = {}
                    for o in self.ops[ename]:
                        for d in o.deps:
                            if d.chan is not None:
                                s, v = csem[d.chan], d.chanval
                            else:
                                s, v = esem[(d.eng, d.epoch)], d.semval
                            if waited.get(id(s), 0) >= v:
                                continue
                            waited[id(s)] = v
                            eng.wait_ge(s, v)
                        if o.fn is None:
                            continue
                        ins = o.fn(eng)
                        if o.chan is not None:
                            ins.then_inc(csem[o.chan], 16)
                        elif o.signal:
                            ins.then_inc(esem[(ename, o.epoch)], 1)
                return body

            block.tensor(run("pe"))
            block.scalar(run("act"))
            block.vector(run("dve"))
            block.gpsimd(run("pool"))
            block.sync(run("sp"))


class Arena:
    def __init__(self, tensor, ncols):
        self.t = tensor
        self.n = ncols
        self.off = 0

    def mark(self):
        return self.off

    def reset(self, m):
        self.off = m

    def take(self, shape, dtype):
        p = shape[0]
        nfree = int(np.prod(shape[1:]))
        nbytes = nfree * (4 if dtype == F32 else 2)
        ncol = (nbytes + 3) // 4
        ncol = (ncol + 7) // 8 * 8
        assert self.off + ncol <= self.n, ("arena overflow", self.off, ncol, self.n)
        a = self.t[0:p, self.off:self.off + ncol]
        self.off += ncol
        if dtype != F32:
            a = a.bitcast(dtype)
        a = a[:, 0:nfree]
        if len(shape) > 2:
            names = "abcd"[: len(shape) - 1]
            kw = {names[i]: shape[i + 1] for i in range(len(shape) - 2)}
            a = a.rearrange("p (%s) -> p %s" % (" ".join(names), " ".join(names)), **kw)
        return a


def bcast(ap, axis, shape):
    return ap.unsqueeze(axis).to_broadcast(list(shape))


def build_program(n_layers=DEPTH, stop_after=None, dbg=False):
    nc = bass.Bass("TRN2", target_bir_lowering=False)
    P = Prog(nc)

    def din(name, shape, dt=F32):
        return nc.dram_tensor(name, list(shape), dt, kind="ExternalInput").ap()

    def dscr(name, shape, dt):
        return nc.dram_tensor(name, list(shape), dt, kind="Internal").ap()

    x_in = din("x", [T, D])
    norm1_g = din("norm1_g", [DEPTH, D])
    w_in = din("w_in", [DEPTH, D, IN_COLS])
    mla_q_norm_g = din("mla_q_norm_g", [DEPTH, 256])
    mla_kv_norm_g = din("mla_kv_norm_g", [DEPTH, 128])
    mla_w_uq = din("mla_w_uq", [DEPTH, 256, 384])
    mla_w_ukv = din("mla_w_ukv", [DEPTH, 128, 512])
    mla_qk_g = din("mla_qk_g", [DEPTH, 2 * 96])
    dil_qk_g = din("dil_qk_g", [DEPTH, 2 * 64])
    gqa_qk_g = din("gqa_qk_g", [DEPTH, 2 * 64])
    diff_qk_g = din("diff_qk_g", [DEPTH, 2 * 32])
    diff_lambda = din("diff_lambda", [DEPTH, 4 * 32])
    diff_subln_g = din("diff_subln_g", [DEPTH, 64])
    mix_beta = din("mix_beta", [DEPTH, D])
    w_out = din("w_out", [DEPTH, D, D])
    norm2_g = din("norm2_g", [DEPTH, D])
    router_w = din("router_w", [DEPTH, D, 20])
    router_b = din("router_b", [DEPTH, 20])
    w_gate = din("expert_w_gate", [DEPTH, NE, D, DFF])
    w_up = din("expert_w_up", [DEPTH, NE, D, DFF])
    w_down = din("expert_w_down", [DEPTH, NE, DFF, D])
    rope_tab = din("rope_tab", [S, 6 * 32])
    strip_dil = din("strip_dil", [4, 128, STRIP_W])
    strip_dif = din("strip_dif", [4, 128, STRIP_W])
    strip_mult = din("strip_mult", [128, STRIP_W])
    out_d = nc.dram_tensor("out", [T, D], F32, kind="ExternalOutput").ap()

    QT = dscr("QT", [NSLOT, 128, T], BF16)
    VS = dscr("VS", [T, NV], BF16)
    Y = dscr("Y", [T, D], F32)
    X1 = dscr("X1", [T, D], F32)
    X2 = dscr("X2", [T, D], F32)
    SD = dscr("SD", [8, 128, STRIP_W], BF16)
    dbg_out = {}

    ARENA_COLS = 196 * 256
    arena_t = nc.alloc_sbuf_tensor("arena", [128, ARENA_COLS], F32)
    AR = Arena(arena_t, ARENA_COLS)
    psum_t = nc.alloc_psum_tensor("psum", [128, 4096], F32)

    def bank(i):
        return psum_t[:, i * 512:(i + 1) * 512]

    def bank_bf(i):
        return psum_t[:, i * 512:(i + 1) * 512].bitcast(BF16)

    def dma(q, out, in_, reads=(), writes=(), accum=(), chan=None, slow=False):
        if slow:
            fn = lambda e: e.dma_start(out=out, in_=in_, allow_slow_non_contiguous=True)
        else:
            fn = lambda e: e.dma_start(out=out, in_=in_)
        return P.op(q, fn, reads=reads, writes=writes, accum=accum, chan=chan)

    def tt(eng, out, in0, in1, op, reads, writes, accum=()):
        return P.op(eng, lambda e: e.tensor_tensor(out=out, in0=in0, in1=in1, op=op), reads=reads, writes=writes, accum=accum)

    def ts(eng, out, in0, s1, s2, op0, op1, reads, writes):
        if s2 is None:
            return P.op(eng, lambda e: e.tensor_scalar(out=out, in0=in0, scalar1=s1, scalar2=None, op0=op0),
                        reads=reads, writes=writes)
        return P.op(eng, lambda e: e.tensor_scalar(out=out, in0=in0, scalar1=s1, scalar2=s2, op0=op0, op1=op1),
                    reads=reads, writes=writes)

    def act(out, in_, func, reads, writes, scale=1.0, bias=None, accum_out=None, accum=()):
        def fn(e):
            kw = {}
            if bias is not None:
                kw["bias"] = bias
            if accum_out is not None:
                kw["accum_out"] = accum_out
            return e.activation(out=out, in_=in_, func=func, scale=scale, **kw)
        return P.op("act", fn, reads=reads, writes=writes, accum=accum)

    def red(out, in_, op, reads, writes, axis=AX.X, accum=()):
        return P.op("dve", lambda e: e.tensor_reduce(out=out, in_=in_, axis=axis, op=op), reads=reads, writes=writes, accum=accum)

    def recip(out, in_, reads, writes):
        return P.op("dve", lambda e: e.reciprocal(out=out, in_=in_), reads=reads, writes=writes)

    def rstd_chain(ssq, inv_e, n, tag, scratch):
        if isinstance(inv_e, float):
            act(ssq, ssq, AF.Sqrt, [tag], [tag], scale=inv_e, bias=eps_t[:, 0:1])
        else:
            tt("dve", ssq, ssq, inv_e, ALU.mult, [tag], [tag])
            act(ssq, ssq, AF.Sqrt, [tag], [tag], bias=eps_t[:, 0:1])
        recip(ssq, ssq, [tag], [tag])

    ident_f = AR.take([128, 128], F32)
    ident_b = AR.take([128, 128], BF16)
    eps_t = AR.take([128, 8], F32)
    gv = AR.take([128, DEPTH, 32], F32)
    GV_G1, GV_G2, GV_BETA, GV_QN, GV_KVN, GV_SUB = 0, 8, 16, 24, 26, 27
    gbt = AR.take([128, DEPTH, 704], F32)
    GB_MLA, GB_DIL, GB_GQA, GB_DIF, GB_LAM, GB_RB = 0, 192, 320, 448, 512, 640
    neglam = AR.take([128, DEPTH], F32)
    inv_e1 = AR.take([128, 33], F32)
    warm = AR.take([128, 512], BF16)

    P.op("pool", lambda e: e.memset(ident_f, 1.0), writes=["ident_f"])
    P.op("pool", lambda e: e.affine_select(out=ident_f, in_=ident_f, pattern=[[-1, 128]], compare_op=ALU.is_equal,
                                           fill=0.0, base=0, channel_multiplier=1), reads=["ident_f"], writes=["ident_f"])
    P.op("dve", lambda e: e.tensor_copy(out=ident_b, in_=ident_f), reads=["ident_f"], writes=["ident_b"])
    P.op("dve", lambda e: e.memset(eps_t, EPS), writes=["eps"])
    P.op("dve", lambda e: e.memset(warm, 1.0), writes=["warm"])

    def pe_warmup(n=40):
        def fn(e):
            ins = None
            for i in range(n):
                ins = e.matmul(bank(7), lhsT=ident_b, rhs=warm, start=True, stop=True)
            return ins
        P.op("pe", fn, reads=["warm_c"], writes=["ps7"])
    P.op("dve", lambda e: e.memset(inv_e1[:, 0:1], 1.0 / 256), writes=["inv_e1"])
    P.op("dve", lambda e: e.memset(inv_e1[:, 1:2], 1.0 / 128), accum=["inv_e1"])
    P.op("dve", lambda e: e.memset(inv_e1[:, 2:3], 1.0), accum=["inv_e1"])
    P.op("dve", lambda e: e.memset(inv_e1[:, 3:17], 1.0 / 64), accum=["inv_e1"])
    P.op("dve", lambda e: e.memset(inv_e1[:, 17:33], 1.0 / 32), accum=["inv_e1"])
    for L in range(DEPTH):
        for (src, off, n) in ((norm1_g, GV_G1, 8), (norm2_g, GV_G2, 8), (mix_beta, GV_BETA, 8), (mla_q_norm_g, GV_QN, 2),
                              (mla_kv_norm_g, GV_KVN, 1)):
            dma("sp", gv[:, L, off:off + n], src[L].rearrange("(c p) -> p c", p=128), accum=["gv"], chan="c0", slow=True)
        for h in range(2):
            dma("sp", gv[64 * h:64 * h + 64, L, GV_SUB:GV_SUB + 1], diff_subln_g[L].rearrange("(p c) -> p c", c=1),
                accum=["gv"], chan="c0", slow=True)
        for (src, off, n) in ((mla_qk_g, GB_MLA, 192), (dil_qk_g, GB_DIL, 128), (gqa_qk_g, GB_GQA, 128),
                              (diff_qk_g, GB_DIF, 64), (diff_lambda, GB_LAM, 128), (router_b, GB_RB, 20)):
            dma("sp", gbt[:, L, off:off + n], src[L].partition_broadcast(128), accum=["gbt"], chan="c0")
    for L in range(DEPTH):
        li = 0.8 - 0.6 * math.exp(-0.3 * L)
        P.op("dve", (lambda L=L, li=li: (lambda e: e.tensor_scalar(
            out=gv[:, L, GV_BETA + 6:GV_BETA + 8], in0=gv[:, L, GV_BETA + 6:GV_BETA + 8],
            scalar1=gv[:, L, GV_SUB:GV_SUB + 1], scalar2=1.0 - li, op0=ALU.mult, op1=ALU.mult)))(),
            reads=["gv"], writes=["gv"])
        lt = AR.take([128, 64], F32)
        lv = gbt[:, L, GB_LAM:GB_LAM + 128].rearrange("p (a b c) -> p a b c", a=2, b=2)
        tt("dve", lt.rearrange("p (a c) -> p a c", a=2), lv[:, :, 0, :], lv[:, :, 1, :], ALU.mult, ["gbt"], [("lt", L)])
        ls = AR.take([128, 2], F32)
        red(ls, lt.rearrange("p (a c) -> p a c", a=2), ALU.add, [("lt", L)], [("ls", L)])
        act(ls, ls, AF.Exp, [("ls", L)], [("ls", L)])
        tt("dve", neglam[:, L:L + 1], ls[:, 1:2], ls[:, 0:1], ALU.subtract, [("ls", L)], [("nl", L)])
        ts("dve", neglam[:, L:L + 1], neglam[:, L:L + 1], -li, None, ALU.add, None, [("nl", L)], [("nl", L)])
    m0 = AR.mark()
    smul = AR.take([128, STRIP_W], F32)
    dma("sp", smul, strip_mult, writes=["smul"], chan="c1")
    sfs = [AR.take([128, STRIP_W], F32) for _ in range(2)]
    sbs = [AR.take([128, STRIP_W], BF16) for _ in range(2)]
    for i in range(8):
        sf = sfs[i % 2]
        sb = sbs[i % 2]
        src = strip_dil[i] if i < 4 else strip_dif[i - 4]
        dma("sp", sf, src, writes=[("sf", i % 2)], chan=("c2", i % 2))
        act(sf, sf, AF.Exp, [("sf", i % 2)], [("sf", i % 2)])
        if i < 4:
            tt("dve", sb, sf, smul, ALU.mult, [("sf", i % 2), "smul"], [("sb", i % 2)])
        else:
            P.op("dve", (lambda a=sb, b=sf: (lambda e: e.tensor_copy(out=a, in_=b)))(), reads=[("sf", i % 2)],
                 writes=[("sb", i % 2)])
        dma("sp", SD[i], sb, reads=[("sb", i % 2)], accum=["SD"], chan=("c3", i % 2))
    P.barrier()
    AR.reset(m0)
    persist_mark = AR.mark()

    def phase_A(L, x_src):
        TB = 2
        m = AR.mark()
        w_in_sb = AR.take([128, 8, IN_COLS], BF16)
        w_uq_sb = AR.take([128, 2, 384], BF16)
        w_ukv_sb = AR.take([128, 512], BF16)
        for c in range(8):
            dma("pool", w_in_sb[:, c, :], w_in[L, c * 128:(c + 1) * 128, :], accum=["w_in"], chan="a_w")
        for c in range(2):
            dma("pool", w_uq_sb[:, c, :], mla_w_uq[L, c * 128:(c + 1) * 128, :], accum=["w_in"], chan="a_w")
        dma("pool", w_ukv_sb, mla_w_ukv[L], accum=["w_in"], chan="a_w")
        xt = [AR.take([128, TB, D], F32) for _ in range(2)]
        hb1 = AR.take([128, TB, D], BF16)
        hb = [hb1, hb1]
        hT = [AR.take([128, 8, 128], BF16) for _ in range(2)]
        ss1 = [AR.take([128, 2 * TB], F32) for _ in range(2)]
        junk = AR.take([128, D], F32)
        pj = AR.take([128, TB, IN_COLS], F32)
        sq = AR.take([128, TB, C_DV], F32)
        ssg = AR.take([128, TB, 33], F32)
        cqn = AR.take([128, TB, 384], BF16)
        cT = AR.take([128, 3, 128], BF16)
        q2 = AR.take([128, TB, 384], F32)
        kv2 = AR.take([128, TB, 512], F32)
        ss2 = AR.take([128, TB, 8], F32)
        qk_a = AR.take([128, TB, 8, 96], F32)
        qk_c = AR.take([128, TB, 6, 64], F32)
        rt1 = AR.take([128, TB, 12, 32], F32)
        rt2 = AR.take([128, TB, 12, 32], F32)
        rope = [AR.take([128, TB, 6, 32], F32) for _ in range(2)]
        qktm = AR.take([128, TB, NSLOT, 128], BF16)
        vst = AR.take([128, TB, NV], BF16)
        qst = [AR.take([128, NSLOT, 512], BF16) for _ in range(2)]
        P.op("pool", lambda e: e.memset(qktm, 0.0), writes=["qktm"])
        gb = lambda off, n: gbt[:, L, off:off + n]

        nbatch = NT // TB
        for b in range(nbatch):
            bp = b % 2
            tok0 = b * TB * 128
            R = lambda name: (name, bp)
            dma("sp", xt[bp], x_src[tok0:tok0 + TB * 128, :].rearrange("(t p) d -> p t d", p=128), writes=[R("xt")],
                chan=("a_x", bp))
            tpos = (tok0 % S)
            dma("sp", rope[bp].rearrange("p t a b -> p t (a b)"),
                rope_tab[tpos:tpos + TB * 128, :].rearrange("(t p) c -> p t c", p=128), writes=[R("rope")], chan=("a_r", bp))
            for t in range(TB):
                act(junk, xt[bp][:, t, :], AF.Square, [R("xt")], [R("ss1")] if t == 0 else [], accum_out=ss1[bp][:, t:t + 1],
                    accum=[] if t == 0 else [R("ss1")])
            rstd_chain(ss1[bp][:, 0:TB], 1.0 / D, TB, R("ss1"), None)
            for t in range(TB):
                act(hb[bp][:, t, :], xt[bp][:, t, :], AF.Copy, [R("xt"), R("ss1")], [("hb", t)], scale=ss1[bp][:, t:t + 1])
            for t in range(TB):
                tp = t % 2
                tokt = tok0 + t * 128
                def trf(e, t=t, bp=bp):
                    ins = None
                    for c in range(8):
                        ins = e.transpose(out=bank_bf(7)[:, c * 128:(c + 1) * 128], in_=hb[bp][:, t, c * 128:(c + 1) * 128],
                                          identity=ident_b)
                    return ins
                P.op("pe", trf, reads=[("hb", t)], writes=["ps7"])
                tt("dve", hT[tp], bank_bf(7).rearrange("p (c k) -> p c k", c=8),
                   bcast(gv[:, L, GV_G1:GV_G1 + 8], 2, [128, 8, 128]), ALU.mult, ["ps7"], [("hT", tp)])
                def mmf(e, tp=tp):
                    ins = None
                    for j in range(5):
                        w = 512 if j < 4 else IN_COLS - 2048
                        for c in range(8):
                            ins = e.matmul(bank(j)[:, 0:w], lhsT=hT[tp][:, c, :], rhs=w_in_sb[:, c, j * 512:j * 512 + w],
                                           start=(c == 0), stop=(c == 7))
                    return ins
                P.op("pe", mmf, reads=[("hT", tp), "w_in"], writes=["ps0", "ps1", "ps2", "ps3", "ps4"])
                for j in range(5):
                    w = 512 if j < 4 else IN_COLS - 2048
                    eng = "act" if j % 2 == 0 else "dve"
                    if eng == "act":
                        act(pj[:, t, j * 512:j * 512 + w], bank(j)[:, 0:w], AF.Copy, ["ps%d" % j], [], accum=[("pj", t)])
                    else:
                        P.op("dve", (lambda j=j, w=w, t=t: (lambda e: e.tensor_copy(out=pj[:, t, j * 512:j * 512 + w],
                                                                                   in_=bank(j)[:, 0:w])))(),
                             reads=["ps%d" % j], accum=[("pj", t)])
            PJ = [("pj", t) for t in range(TB)]
            act(sq[:, :, 0:C_BV], pj[:, :, 0:C_BV], AF.Square, PJ, ["sq"])
            P.op("act", lambda e: e.activation(out=sq[:, :, C_CQ2:C_CV2], in_=pj[:, :, C_CQ2:C_CV2], func=AF.Square),
                 reads=PJ, accum=["sq"])
            P.op("act", lambda e: e.activation(out=sq[:, :, C_DQ:C_DV], in_=pj[:, :, C_DQ:C_DV], func=AF.Square),
                 reads=PJ, accum=["sq"])
            red(ssg[:, :, 0:1], sq[:, :, 0:256].rearrange("p t (a e) -> p t a e", a=1), ALU.add, ["sq"], ["ssg"])
            P.op("dve", lambda e: e.tensor_reduce(out=ssg[:, :, 1:2], in_=sq[:, :, 256:384].rearrange("p t (a e) -> p t a e", a=1),
                                                  axis=AX.X, op=ALU.add), reads=["sq"], accum=["ssg"])
            P.op("dve", lambda e: e.tensor_reduce(out=ssg[:, :, 2:3], in_=sq[:, :, 384:416].rearrange("p t (a e) -> p t a e", a=1),
                                                  axis=AX.X, op=ALU.add), reads=["sq"], accum=["ssg"])
            P.op("dve", lambda e: e.tensor_reduce(out=ssg[:, :, 3:11], in_=sq[:, :, C_BQ:C_BV].rearrange("p t (a e) -> p t a e", a=8),
                                                  axis=AX.X, op=ALU.add), reads=["sq"], accum=["ssg"])
            P.op("dve", lambda e: e.tensor_reduce(out=ssg[:, :, 11:17], in_=sq[:, :, C_CQ2:C_CV2].rearrange("p t (a e) -> p t a e", a=6),
                                                  axis=AX.X, op=ALU.add), reads=["sq"], accum=["ssg"])
            P.op("dve", lambda e: e.tensor_reduce(out=ssg[:, :, 17:33], in_=sq[:, :, C_DQ:C_DV].rearrange("p t (a e) -> p t a e", a=16),
                                                  axis=AX.X, op=ALU.add), reads=["sq"], accum=["ssg"])
            sskr = ss1[bp][:, TB:2 * TB]
            P.op("dve", (lambda sskr=sskr: (lambda e: e.tensor_copy(out=sskr, in_=ssg[:, :, 2])))(), reads=["ssg"], writes=[R("sskr")])
            rstd_chain(ssg, bcast(inv_e1, 1, [128, TB, 33]), 33, "ssg", None)
            tt("dve", cqn[:, :, 0:256], pj[:, :, 0:256], ssg[:, :, 0:1].to_broadcast([128, TB, 256]), ALU.mult,
               PJ + ["ssg"], ["cqn"])
            P.op("dve", lambda e: e.tensor_tensor(out=cqn[:, :, 256:384], in0=pj[:, :, 256:384],
                                                  in1=ssg[:, :, 1:2].to_broadcast([128, TB, 128]), op=ALU.mult),
                 reads=PJ + ["ssg"], accum=["cqn"])
            for t in range(TB):
                def trc(e, t=t):
                    ins = None
                    for c in range(3):
                        ins = e.transpose(out=bank_bf(7)[:, c * 128:(c + 1) * 128], in_=cqn[:, t, c * 128:(c + 1) * 128],
                                          identity=ident_b)
                    return ins
                P.op("pe", trc, reads=["cqn"], writes=["ps7"])
                tt("dve", cT, bank_bf(7)[:, 0:384].rearrange("p (c k) -> p c k", c=3),
                   bcast(gv[:, L, GV_QN:GV_QN + 3], 2, [128, 3, 128]), ALU.mult, ["ps7"], ["cT"])
                def mm2(e):
                    for c in range(2):
                        e.matmul(bank(5)[:, 0:384], lhsT=cT[:, c, :], rhs=w_uq_sb[:, c, :], start=(c == 0), stop=(c == 1))
                    return e.matmul(bank(6), lhsT=cT[:, 2, :], rhs=w_ukv_sb, start=True, stop=True)
                P.op("pe", mm2, reads=["cT", "w_in"], writes=["ps5", "ps6"])
                act(q2[:, t, :], bank(5)[:, 0:384], AF.Copy, ["ps5"], [("q2", t)])
                P.op("dve", (lambda t=t: (lambda e: e.tensor_copy(out=kv2[:, t, :], in_=bank(6))))(), reads=["ps6"],
                     writes=[("kv2", t)])
            Q2 = [("q2", t) for t in range(TB)]
            KV2 = [("kv2", t) for t in range(TB)]
            kv4 = kv2.rearrange("p t (h e) -> p t h e", h=4)
            act(sq[:, :, 0:384], q2, AF.Square, Q2, ["sq"])
            P.op("act", lambda e: e.activation(out=sq[:, :, 384:896], in_=kv2, func=AF.Square), reads=KV2, accum=["sq"])
            red(ss2[:, :, 0:4], sq[:, :, 0:384].rearrange("p t (h e) -> p t h e", h=4), ALU.add, ["sq"], ["ss2"])
            P.op("dve", lambda e: e.tensor_reduce(out=ss2[:, :, 4:8],
                                                  in_=sq[:, :, 384:896].rearrange("p t (h e) -> p t h e", h=4)[:, :, :, 0:64],
                                                  axis=AX.X, op=ALU.add), reads=["sq"], accum=["ss2"])
            tt("dve", ss2[:, :, 4:8], ss2[:, :, 4:8], bcast(sskr, 2, [128, TB, 4]), ALU.add, ["ss2", R("sskr")], ["ss2"])
            rstd_chain(ss2, 1.0 / 96, 8, "ss2", None)
            qa4 = qk_a.rearrange("p t h e -> p (t h) e")
            tt("dve", qk_a[:, :, 0:4, :], q2.rearrange("p t (h e) -> p t h e", h=4),
               bcast(ss2[:, :, 0:4], 3, [128, TB, 4, 96]), ALU.mult, Q2 + ["ss2"], ["qk_a"])
            P.op("dve", lambda e: e.tensor_tensor(out=qk_a[:, :, 4:8, 0:64], in0=kv4[:, :, :, 0:64],
                                                  in1=bcast(ss2[:, :, 4:8], 3, [128, TB, 4, 64]), op=ALU.mult),
                 reads=KV2 + ["ss2"], accum=["qk_a"])
            for t in range(TB):
                P.op("dve", (lambda t=t: (lambda e: e.tensor_tensor(
                    out=qk_a[:, t, 4:8, 64:96], in0=bcast(pj[:, t, C_KR:C_KR + 32], 1, [128, 4, 32]),
                    in1=bcast(ss2[:, t, 4:8], 2, [128, 4, 32]), op=ALU.mult)))(), reads=PJ + ["ss2"], accum=["qk_a"])
            for t in range(TB):
                g2v = gb(GB_MLA, 192).rearrange("p (a e) -> p a e", a=2)
                P.op("dve", (lambda t=t, g2v=g2v: (lambda e: e.tensor_tensor(
                    out=qk_a[:, t].rearrange("p (a h) e -> p a h e", a=2), in0=qk_a[:, t].rearrange("p (a h) e -> p a h e", a=2),
                    in1=bcast(g2v, 2, [128, 2, 4, 96]), op=ALU.mult)))(), reads=["qk_a", "gbt"], writes=["qk_a"] if t == TB - 1 else [],
                    accum=[] if t == TB - 1 else ["qk_a"])
            def rope_apply(x4, nh, cos4, sin4, tag, tmp1, tmp2):
                tt("dve", tmp1, x4, cos4, ALU.mult, [tag, R("rope")], [tag + "_t1"])
                tt("dve", tmp2[:, :, :, 0:16], x4[:, :, :, 16:32], sin4[:, :, :, 0:16], ALU.mult, [tag, R("rope")], [tag + "_t2"])
                P.op("dve", lambda e: e.tensor_tensor(out=tmp2[:, :, :, 16:32], in0=x4[:, :, :, 0:16], in1=sin4[:, :, :, 16:32],
                                                      op=ALU.mult), reads=[tag, R("rope")], accum=[tag + "_t2"])
                tt("dve", x4, tmp1, tmp2, ALU.add, [tag + "_t1", tag + "_t2"], [tag])
            rp = rope[bp]
            rope_apply(qk_a[:, :, :, 64:96], 8, bcast(rp[:, :, 0, :], 2, [128, TB, 8, 32]), bcast(rp[:, :, 1, :], 2, [128, TB, 8, 32]),
                       "qk_a", rt1[:, :, 0:8, :], rt2[:, :, 0:8, :])
            P.op("act", lambda e: e.activation(out=qktm[:, :, SL_AQ:SL_AQ + 8, 0:96], in_=qk_a, func=AF.Copy), reads=["qk_a"],
                 accum=["qktm"])
            dq = pj[:, :, C_BQ:C_BV].rearrange("p t (h e) -> p t h e", h=8)
            dst = qktm[:, :, SL_BQ:SL_BQ + 4, :].rearrange("p t s (h e) -> p t (s h) e", h=2)
            tt("dve", dst, dq, bcast(ssg[:, :, 3:11], 3, [128, TB, 8, 64]), ALU.mult, PJ + ["ssg"], [], accum=["qktm"])
            for t in range(TB):
                gd = gb(GB_DIL, 128).rearrange("p (a e) -> p a e", a=2)
                dv = qktm[:, t, SL_BQ:SL_BQ + 4, :].rearrange("p (a s) (h e) -> p a (s h) e", a=2, h=2)
                P.op("dve", (lambda dv=dv, gd=gd: (lambda e: e.tensor_tensor(out=dv, in0=dv, in1=bcast(gd, 2, [128, 2, 4, 64]),
                                                                             op=ALU.mult)))(), reads=["qktm", "gbt"], accum=["qktm"])
            fq = pj[:, :, C_DQ:C_DV].rearrange("p t (h e) -> p t h e", h=16)
            fst = qktm[:, :, SL_DQ:SL_DQ + 4, :].rearrange("p t s (h e) -> p t (s h) e", h=4)
            P.op("dve", lambda e: e.tensor_tensor(out=fst, in0=fq, in1=bcast(ssg[:, :, 17:33], 3, [128, TB, 16, 32]), op=ALU.mult),
                 reads=PJ + ["ssg"], accum=["qktm"])
            for t in range(TB):
                gf = gb(GB_DIF, 64).rearrange("p (a e) -> p a e", a=2)
                fv = qktm[:, t, SL_DQ:SL_DQ + 4, :].rearrange("p (a s) (h e) -> p a (s h) e", a=2, h=4)
                P.op("dve", (lambda fv=fv, gf=gf: (lambda e: e.tensor_tensor(out=fv, in0=fv, in1=bcast(gf, 2, [128, 2, 8, 32]),
                                                                             op=ALU.mult)))(), reads=["qktm", "gbt"], accum=["qktm"])
            cq = pj[:, :, C_CQ2:C_CV2].rearrange("p t (h e) -> p t h e", h=6)
            tt("dve", qk_c, cq, bcast(ssg[:, :, 11:17], 3, [128, TB, 6, 64]), ALU.mult, PJ + ["ssg"], ["qk_c"])
            ggq = gb(GB_GQA, 64)
            ggk = gb(GB_GQA + 64, 64)
            tt("dve", qk_c[:, :, 0:4, :], qk_c[:, :, 0:4, :], bcast(bcast(ggq, 1, [128, 4, 64]), 1, [128, TB, 4, 64]), ALU.mult,
               ["qk_c", "gbt"], ["qk_c"])
            tt("dve", qk_c[:, :, 4:6, :], qk_c[:, :, 4:6, :], bcast(bcast(ggk, 1, [128, 2, 64]), 1, [128, TB, 2, 64]), ALU.mult,
               ["qk_c", "gbt"], ["qk_c"])
            for t in range(TB):
                xc = qk_c[:, t].rearrange("p h (a e) -> p h a e", a=2)
                cosv = bcast(rp[:, t, 2:6, :].rearrange("p (a b) e -> p a b e", b=2)[:, :, 0, :], 1, [128, 6, 2, 32])
                sinv = bcast(rp[:, t, 2:6, :].rearrange("p (a b) e -> p a b e", b=2)[:, :, 1, :], 1, [128, 6, 2, 32])
                t1 = rt1[:, t].rearrange("p (h a) e -> p h a e", a=2)
                t2 = rt2[:, t].rearrange("p (h a) e -> p h a e", a=2)
                tag = "qk_c"
                last = (t == TB - 1)
                P.op("dve", (lambda t1=t1, xc=xc, cosv=cosv: (lambda e: e.tensor_tensor(out=t1, in0=xc, in1=cosv, op=ALU.mult)))(),
                     reads=[tag, R("rope")], writes=[("ct1", t)])
                P.op("dve", (lambda t2=t2, xc=xc, sinv=sinv: (lambda e: e.tensor_tensor(
                    out=t2[:, :, :, 0:16], in0=xc[:, :, :, 16:32], in1=sinv[:, :, :, 0:16], op=ALU.mult)))(),
                    reads=[tag, R("rope")], writes=[("ct2", t)])
                P.op("dve", (lambda t2=t2, xc=xc, sinv=sinv: (lambda e: e.tensor_tensor(
                    out=t2[:, :, :, 16:32], in0=xc[:, :, :, 0:16], in1=sinv[:, :, :, 16:32], op=ALU.mult)))(),
                    reads=[tag, R("rope")], accum=[("ct2", t)])
                qd = qktm[:, t, SL_CQ:SL_CQ + 2, :].rearrange("p s (h a e) -> p (s h) a e", h=2, a=2)
                kd = qktm[:, t, SL_CK:SL_CK + 2, :].rearrange("p s (d f) -> p s d f", d=2)
                P.op("dve", (lambda qd=qd, t1=t1, t2=t2: (lambda e: e.tensor_tensor(out=qd, in0=t1[:, 0:4], in1=t2[:, 0:4], op=ALU.add)))(),
                     reads=[("ct1", t), ("ct2", t)], accum=["qktm"])
                k1 = bcast(rt1[:, t, 8:12, :].rearrange("p (h a) e -> p h (a e)", a=2), 2, [128, 2, 2, 64])
                k2 = bcast(rt2[:, t, 8:12, :].rearrange("p (h a) e -> p h (a e)", a=2), 2, [128, 2, 2, 64])
                P.op("dve", (lambda kd=kd, k1=k1, k2=k2: (lambda e: e.tensor_tensor(out=kd, in0=k1, in1=k2, op=ALU.add)))(),
                     reads=[("ct1", t), ("ct2", t)], accum=["qktm"])
            P.op("act", lambda e: e.activation(out=vst[:, :, V_A:V_A + 256].rearrange("p t (h e) -> p t h e", h=4),
                                               in_=kv4[:, :, :, 64:128], func=AF.Copy), reads=KV2, writes=["vst"])
            P.op("act", lambda e: e.activation(out=vst[:, :, V_B:V_B + 256], in_=pj[:, :, C_BV:C_BV + 256], func=AF.Copy),
                 reads=PJ, accum=["vst"])
            P.op("act", lambda e: e.activation(out=vst[:, :, V_C:V_C + 128], in_=pj[:, :, C_CV2:C_CV2 + 128], func=AF.Copy),
                 reads=PJ, accum=["vst"])
            P.op("act", lambda e: e.activation(out=vst[:, :, V_D:V_D + 256], in_=pj[:, :, C_DV:C_DV + 256], func=AF.Copy),
                 reads=PJ, accum=["vst"])
            dma("sp", VS[tok0:tok0 + TB * 128, :].rearrange("(t p) c -> p t c", p=128), vst, reads=["vst"], accum=["VS"],
                chan="a_vs")
            qb = (b // 2) % 2
            for t in range(TB):
                col0 = ((b % 2) * TB + t) * 128
                for g3 in range(3):
                    s0 = g3 * 8
                    ns = min(8, NSLOT - s0)
                    def trq(e, t=t, s0=s0, ns=ns, g3=g3):
                        ins = None
                        for s_ in range(ns):
                            ins = e.transpose(out=bank_bf(5 + g3)[:, s_ * 128:(s_ + 1) * 128], in_=qktm[:, t, s0 + s_, :],
                                              identity=ident_b)
                        return ins
                    P.op("pe", trq, reads=["qktm"], writes=["ps%d" % (5 + g3)])
                    src = bank_bf(5 + g3)[:, 0:ns * 128].rearrange("p (s k) -> p s k", s=ns)
                    dstq = qst[qb][:, s0:s0 + ns, col0:col0 + 128]
                    if g3 == 1:
                        P.op("act", (lambda dstq=dstq, src=src: (lambda e: e.activation(out=dstq, in_=src, func=AF.Copy)))(),
                             reads=["ps%d" % (5 + g3)], accum=[("qst", qb)])
                    else:
                        P.op("dve", (lambda dstq=dstq, src=src: (lambda e: e.tensor_copy(out=dstq, in_=src)))(),
                             reads=["ps%d" % (5 + g3)], accum=[("qst", qb)])
            if b % 2 == 1:
                tq0 = (b - 1) * TB * 128
                dma("sp", QT[:, :, tq0:tq0 + 512].rearrange("s p k -> p s k"), qst[qb], reads=[("qst", qb)], accum=["QT"],
                    chan=("a_qt", qb))
        P.barrier()
        AR.reset(m)

    def phase_B(L):
        m = AR.mark()
        vsb = [AR.take([128, 16, 4, 128], BF16) for _ in range(2)]
        ot = [AR.take([128, 512], F32) for _ in range(2)]
        for i in range(2):
            P.op("pool", (lambda v: (lambda e: e.memset(v, 1.0)))(vsb[i]), writes=[("vsb", i)])
        qT = [AR.take([128, 4, S], BF16) for _ in range(2)]
        kT = [AR.take([128, 8, S], BF16) for _ in range(2)]
        strips = AR.take([128, 4, STRIP_W], BF16)
        pt = [AR.take([128, 512], BF16) for _ in range(6)]
        SBANKS = (0, 1, 2, 5, 6)
        ybuf = [AR.take([128, 4, 256], F32) for _ in range(2)]
        rec = AR.take([128, 8], F32)
        t0b = AR.take([128, 4, 64], F32)
        t1b = AR.take([128, 4, 64], F32)

        mixers = [("A", SL_AQ, 4, SL_AK, 4, V_A, 4, None),
                  ("C", SL_CQ, 2, SL_CK, 2, V_C, 2, None),
                  ("B", SL_BQ, 2, SL_BK, 2, V_B, 4, 0),
                  ("D", SL_DQ, 2, SL_DK, 2, V_D, 4, 4)]
        ycol = {"A": 0, "B": 256, "C": 512, "D": 768}
        units = [(mx, sq_) for mx in mixers for sq_ in range(SEQ_PER_CORE)]
        state = {"step": 0, "ob": 0, "yb": 0, "ot": 0}
        cur_strip = [None]

        def load_unit(u, par):
            (name, qs, nq, ks, nk, vc, nvh, sbase), sq_ = u
            tk0 = sq_ * S
            for i in range(nq):
                dma("sp", qT[par][:, i, :], QT[qs + i, :, tk0:tk0 + S], reads=["QT"], accum=[("qT", par)], chan=("b_q", par))
            if name == "A":
                for i in range(nk):
                    dma("sp", kT[par][:, i, :], QT[ks + i, :, tk0:tk0 + S], reads=["QT"], accum=[("kT", par)], chan=("b_k", par))
            else:
                ntile = 8 if name == "D" else 4
                P.op("pool", (lambda a=kT[par][:, 0:ntile, :]: (lambda e: e.memset(a, 0.0)))(), writes=[("kT", par)])
                for i in range(2):
                    for hh in range(2):
                        h = 2 * i + hh
                        if name == "D":
                            for c in range(2):
                                r0 = 64 * hh + 32 * c
                                dma("sp", kT[par][r0:r0 + 32, 2 * h + c, :], QT[ks + i, r0:r0 + 32, tk0:tk0 + S], reads=["QT"],
                                    accum=[("kT", par)], chan=("b_k", par))
                        else:
                            r0 = 64 * hh
                            dma("sp", kT[par][r0:r0 + 64, h, :], QT[ks + i, r0:r0 + 64, tk0:tk0 + S], reads=["QT"],
                                accum=[("kT", par)], chan=("b_k", par))
            for h in range(nvh):
                dma("sp", vsb[par][:, :, h, 0:64],
                    VS[tk0:tk0 + S, vc + h * 64:vc + (h + 1) * 64].rearrange("(c p) e -> p c e", p=128),
                    reads=["VS"], accum=[("vsb", par)], chan=("b_v", par))

        def head_maps(name):
            hm = []
            if name == "A":
                for h in range(4):
                    hm.append((h, 0, 96, h, h, 96 ** -0.5, None, h * 64, "plain"))
            elif name == "C":
                for h in range(4):
                    hm.append((h // 2, 0, 128, h, h // 2, 64 ** -0.5, None, h * 64, "plain"))
            elif name == "B":
                for h in range(4):
                    hm.append((h // 2, 0, 128, h, h, 64 ** -0.5, h, h * 64, "plain"))
            else:
                for h in range(4):
                    for c in range(2):
                        hm.append((h // 2, 0, 128, 2 * h + c, h, 32 ** -0.5, h, h * 64, "d%d" % c))
            return hm

        load_unit(units[0], 0)
        pe_warmup()
        for ui, u in enumerate(units):
            par = ui % 2
            (name, qs, nq, ks, nk, vc, nvh, sbase), sq_ = u
            if ui + 1 < len(units):
                load_unit(units[ui + 1], 1 - par)
            if sbase is not None and cur_strip[0] != sbase:
                for i in range(4):
                    dma("sp", strips[:, i, :], SD[sbase + i], reads=["SD"], accum=["strips"], chan="b_s")
                cur_strip[0] = sbase
            hms = head_maps(name)
            for qc in range(4):
                yb = state["yb"]
                state["yb"] = 1 - yb
                tiles = []
                for hi, hmv in enumerate(hms):
                    kcs = list(range(16))
                    if name == "B":
                        kcs = [kc for kc in kcs if not (kc * 128 - qc * 512 - 511 > 1024 or kc * 128 + 127 - qc * 512 < -1024)]
                    for j, kc in enumerate(kcs):
                        tiles.append((hi, hmv, kc, j == 0, j == len(kcs) - 1))
                LOOK = 4
                ob_of_head = {}

                def emit_scores(idx):
                    hi, hmv, kc, first, last = tiles[idx]
                    qsl, r0, nr, kt, vh, scale, sidx, ycl, kind = hmv
                    step = state["step"] + idx
                    sb_ = SBANKS[step % 5]
                    krow0 = r0
                    lhsT = kT[par][krow0:krow0 + nr, kt, kc * 128:(kc + 1) * 128]
                    rhs = qT[par][r0:r0 + nr, qsl, qc * 512:(qc + 1) * 512]
                    P.op("pe", (lambda sb_=sb_, lhsT=lhsT, rhs=rhs: (lambda e: e.matmul(bank(sb_), lhsT=lhsT, rhs=rhs, start=True,
                                                                                      stop=True)))(),
                         reads=[("qT", par), ("kT", par)], writes=["ps%d" % sb_])

                def emit_rest(idx):
                    hi, hmv, kc, first, last = tiles[idx]
                    qsl, r0, nr, kt, vh, scale, sidx, ycl, kind = hmv
                    step = state["step"] + idx
                    sb_ = SBANKS[step % 5]
                    pb = step % 6
                    if first:
                        ob_of_head[hi] = state["ob"]
                        state["ob"] = 1 - state["ob"]
                    ob = 3 + ob_of_head[hi]
                    act(pt[pb], bank(sb_), AF.Exp, ["ps%d" % sb_], [("pt", pb)], scale=scale)
                    if sidx is not None:
                        j0 = STRIP_OFF - kc * 128 + qc * 512
                        eng = "dve"
                        tt(eng, pt[pb], pt[pb], strips[:, sidx, j0:j0 + 512], ALU.mult, [("pt", pb), "strips"], [("pt", pb)])
                    def pv(e, pb=pb, ob=ob, kc=kc, vh=vh, first=first, last=last, par=par):
                        return e.matmul(bank(ob), lhsT=vsb[par][:, kc, vh, :], rhs=pt[pb], start=first, stop=last)
                    P.op("pe", pv, reads=[("pt", pb), ("vsb", par)], writes=["ps%d" % ob])
                    if last:
                        def fin(ob=ob, kind=kind, ycl=ycl):
                            oi = state["ot"]
                            state["ot"] = 1 - oi
                            otb = ot[oi]
                            act(otb, bank(ob), AF.Copy, ["ps%d" % ob], [("ot", oi)])
                            def trf(e, otb=otb):
                                ins = None
                                for qs_ in range(4):
                                    ins = e.transpose(out=bank(7)[:, qs_ * 128:(qs_ + 1) * 128], in_=otb[:, qs_ * 128:(qs_ + 1) * 128],
                                                      identity=ident_f)
                                return ins
                            P.op("pe", trf, reads=[("ot", oi)], writes=["ps7"])
                            o3 = bank(7).rearrange("p (q e) -> p q e", q=4)
                            rc = rec[:, 0:4] if kind != "d1" else rec[:, 4:8]
                            rtag = "rec0" if kind != "d1" else "rec1"
                            recip(rc, o3[:, :, 64], ["ps7"], [rtag])
                            yv = ybuf[yb][:, :, ycl:ycl + 64]
                            if kind == "plain":
                                P.op("dve", (lambda yv=yv, o3=o3, rc=rc: (lambda e: e.tensor_tensor(
                                    out=yv, in0=o3[:, :, 0:64], in1=bcast(rc, 2, [128, 4, 64]), op=ALU.mult)))(),
                                    reads=["ps7", rtag], accum=[("ybuf", yb)])
                            elif kind == "d0":
                                tt("dve", t0b, o3[:, :, 0:64], bcast(rc, 2, [128, 4, 64]), ALU.mult, ["ps7", rtag], ["t0b"])
                            else:
                                tt("dve", t1b, o3[:, :, 0:64], bcast(rc, 2, [128, 4, 64]), ALU.mult, ["ps7", rtag], ["t1b"])
                                P.op("dve", (lambda yv=yv: (lambda e: e.scalar_tensor_tensor(
                                    out=yv, in0=t1b, scalar=neglam[:, L:L + 1], in1=t0b, op0=ALU.mult, op1=ALU.add)))(),
                                    reads=["t0b", "t1b"], accum=[("ybuf", yb)])
                        pending.append((idx + 2, fin))

                n = len(tiles)
                pending = []
                for i in range(min(LOOK, n)):
                    emit_scores(i)
                for i in range(n):
                    if i + LOOK < n:
                        emit_scores(i + LOOK)
                    emit_rest(i)
                    while pending and pending[0][0] <= i:
                        pending.pop(0)[1]()
                while pending:
                    pending.pop(0)[1]()
                state["step"] += n
                tk = sq_ * S + qc * 512
                dma("sp", Y[tk:tk + 512, ycol[name]:ycol[name] + 256].rearrange("(q p) c -> p q c", p=128), ybuf[yb],
                    reads=[("ybuf", yb)], accum=["Y"], chan=("b_y", yb))
        P.barrier()
        AR.reset(m)

    def phase_C(L, x_src):
        m = AR.mark()
        w_out_sb = AR.take([128, 8, D], BF16)
        for c in range(8):
            dma("pool", w_out_sb[:, c, :], w_out[L, c * 128:(c + 1) * 128, :], accum=["w_out"], chan="c_w")
        yt = [AR.take([128, D], F32) for _ in range(2)]
        xt = [AR.take([128, D], F32) for _ in range(2)]
        sqc = AR.take([128, D], F32)
        ssc = [AR.take([128, 8], F32) for _ in range(2)]
        ybf = [AR.take([128, D], BF16) for _ in range(2)]
        mT = [AR.take([128, 8, 128], BF16) for _ in range(2)]
        xo = [AR.take([128, D], F32) for _ in range(2)]
        inv_c = AR.take([128, 8], F32)
        P.op("dve", lambda e: e.memset(inv_c[:, 0:3], 1.0 / 256), writes=["inv_c"])
        P.op("dve", lambda e: e.memset(inv_c[:, 3:8], 1.0 / 64), accum=["inv_c"])
        def c_front(t):
            p = t % 2
            R = lambda n_: (n_, p)
            dma("sp", yt[p], Y[t * 128:(t + 1) * 128, :], reads=["Y"], writes=[R("yt")], chan=("c_y", p))
            dma("sp", xt[p], x_src[t * 128:(t + 1) * 128, :], reads=["Xsrc"], writes=[R("xt")], chan=("c_x", p))
            act(sqc, yt[p], AF.Square, [R("yt")], ["sqc"])
            red(ssc[p][:, 0:3], sqc[:, 0:768].rearrange("p (a e) -> p a e", a=3), ALU.add, ["sqc"], [R("ssc")])
            P.op("dve", (lambda p=p: (lambda e: e.tensor_reduce(out=ssc[p][:, 3:7], in_=sqc[:, 768:1024].rearrange("p (a e) -> p a e", a=4),
                                                                 axis=AX.X, op=ALU.add)))(), reads=["sqc"], accum=[R("ssc")])
            rstd_chain(ssc[p][:, 0:7], inv_c[:, 0:7], 7, R("ssc"), None)
            tt("dve", ybf[p][:, 0:768].rearrange("p (a e) -> p a e", a=3), yt[p][:, 0:768].rearrange("p (a e) -> p a e", a=3),
               bcast(ssc[p][:, 0:3], 2, [128, 3, 256]), ALU.mult, [R("yt"), R("ssc")], [R("ybf")])
            P.op("dve", (lambda p=p: (lambda e: e.tensor_tensor(
                out=ybf[p][:, 768:1024].rearrange("p (a e) -> p a e", a=4), in0=yt[p][:, 768:1024].rearrange("p (a e) -> p a e", a=4),
                in1=bcast(ssc[p][:, 3:7], 2, [128, 4, 64]), op=ALU.mult)))(), reads=[R("yt"), R("ssc")], accum=[R("ybf")])
        def c_back(t):
            p = t % 2
            R = lambda n_: (n_, p)
            def trf(e, p=p):
                ins = None
                for c in range(8):
                    ins = e.transpose(out=bank_bf(7)[:, c * 128:(c + 1) * 128], in_=ybf[p][:, c * 128:(c + 1) * 128], identity=ident_b)
                return ins
            P.op("pe", trf, reads=[R("ybf")], writes=["ps7"])
            tt("dve", mT[p], bank_bf(7).rearrange("p (c k) -> p c k", c=8), bcast(gv[:, L, GV_BETA:GV_BETA + 8], 2, [128, 8, 128]),
               ALU.mult, ["ps7"], [R("mT")])
            def mmf(e, p=p):
                ins = None
                for j in range(2):
                    for c in range(8):
                        ins = e.matmul(bank(j), lhsT=mT[p][:, c, :], rhs=w_out_sb[:, c, j * 512:(j + 1) * 512], start=(c == 0),
                                       stop=(c == 7))
                return ins
            P.op("pe", mmf, reads=[R("mT"), "w_out"], writes=["ps0", "ps1"])
            tt("dve", xo[p][:, 0:512], xt[p][:, 0:512], bank(0), ALU.add, [R("xt"), "ps0"], [R("xo0")])
            tt("dve", xo[p][:, 512:1024], xt[p][:, 512:1024], bank(1), ALU.add, [R("xt"), "ps1"], [R("xo1")])
            dma("pool", X1[t * 128:(t + 1) * 128, :], xo[p], reads=[R("xo0"), R("xo1")], accum=["X1"], chan=("c_o", p))
        c_front(0)
        for t in range(NT):
            if t + 1 < NT:
                c_front(t + 1)
            c_back(t)
        P.barrier()
        AR.reset(m)

    def phase_D(L, dst):
        m = AR.mark()
        wr = AR.take([128, 8, 20], F32)
        dma("sp", wr, router_w[L].rearrange("(c p) n -> p c n", p=128), writes=["wr"], chan="d_wr")
        hT = AR.take([128, 8, S], BF16)
        hTf = AR.take([128, 8, 128], F32)
        yacc = AR.take([128, 16, D], F32)
        lg = AR.take([128, 16, 20], F32)
        gate = AR.take([128, 16, 16], F32)
        xs = [AR.take([128, D], F32) for _ in range(2)]
        hf = [AR.take([128, D], F32) for _ in range(2)]
        ss = [AR.take([128, 2], F32) for _ in range(2)]
        wg = [AR.take([128, 8, DFF], BF16) for _ in range(2)]
        wu = [AR.take([128, 8, DFF], BF16) for _ in range(2)]
        wd = [AR.take([128, 4, D], BF16) for _ in range(2)]
        sg = [AR.take([128, 512], BF16) for _ in range(2)]
        hid = [AR.take([128, 4, 512], BF16) for _ in range(2)]
        r1 = AR.take([128, 16, 4], F32)
        r2 = AR.take([128, 16, 4], F32)
        r3 = AR.take([128, 16], F32)
        r4 = AR.take([128, 16], F32)
        e1 = AR.take([128, 16, 16], F32)
        e2 = AR.take([128, 16, 16], F32)
        r5 = AR.take([128, 16], F32)

        def load_expert(e_, par, seqi):
            for c in range(0, 8, 4):
                dma("pool", wg[par][:, c:c + 4, :], w_gate[L, e_, c * 128:(c + 4) * 128, :].rearrange("(c p) f -> p c f", p=128),
                    accum=[("wg", par)], chan=("d_w", par))
                dma("pool", wu[par][:, c:c + 4, :], w_up[L, e_, c * 128:(c + 4) * 128, :].rearrange("(c p) f -> p c f", p=128),
                    accum=[("wu", par)], chan=("d_w", par))
            dma("pool", wd[par], w_down[L, e_].rearrange("(c p) d -> p c d", p=128), accum=[("wd", par)], chan=("d_w", par))

        for sq_ in range(SEQ_PER_CORE):
            load_expert(0, 0, sq_)
            def d_front(t):
                p = t % 2
                R = lambda n_: (n_, p)
                tok = sq_ * S + t * 128
                dma("sp", xs[p], X1[tok:tok + 128, :], reads=["X1"], writes=[R("xs")], chan=("d_x", p))
                act(hf[p], xs[p], AF.Square, [R("xs")], [R("ss"), R("hf")], accum_out=ss[p][:, 0:1])
                rstd_chain(ss[p][:, 0:1], 1.0 / D, 1, R("ss"), None)
                act(hf[p], xs[p], AF.Copy, [R("xs"), R("ss")], [R("hf")], scale=ss[p][:, 0:1])
                P.op("pool", (lambda t=t, p=p: (lambda e: e.tensor_copy(out=yacc[:, t, :], in_=xs[p])))(), reads=[R("xs")],
                     writes=[("yacc", t)])
            def d_back(t):
                p = t % 2
                R = lambda n_: (n_, p)
                for half in range(2):
                    def trf(e, p=p, half=half):
                        ins = None
                        for c in range(4):
                            cc = half * 4 + c
                            ins = e.transpose(out=bank(6 + half)[:, c * 128:(c + 1) * 128], in_=hf[p][:, cc * 128:(cc + 1) * 128],
                                              identity=ident_f)
                        return ins
                    P.op("pe", trf, reads=[R("hf")], writes=["ps%d" % (6 + half)])
                    g2 = gv[:, L, GV_G2 + 4 * half:GV_G2 + 4 * half + 4]
                    src = bank(6 + half).rearrange("p (c k) -> p c k", c=4)
                    P.op("dve", (lambda src=src, g2=g2, half=half: (lambda e: e.tensor_tensor(
                        out=hTf[:, 4 * half:4 * half + 4, :], in0=src, in1=bcast(g2, 2, [128, 4, 128]), op=ALU.mult)))(),
                        reads=["ps%d" % (6 + half)], accum=["hTf"] if half else [], writes=[] if half else ["hTf"])
                    P.op("act", (lambda src=src, half=half, t=t: (lambda e: e.activation(
                        out=hT[:, 4 * half:4 * half + 4, t * 128:(t + 1) * 128], in_=hTf[:, 4 * half:4 * half + 4, :], func=AF.Copy)))(),
                        reads=["hTf"], accum=["hT"])
                def mmr(e):
                    ins = None
                    for c in range(8):
                        ins = e.matmul(bank(5)[:, 0:20], lhsT=hTf[:, c, :], rhs=wr[:, c, :], start=(c == 0), stop=(c == 7))
                    return ins
                P.op("pe", mmr, reads=["hTf", "wr"], writes=["ps5"])
                P.op("dve", (lambda t=t: (lambda e: e.tensor_tensor(out=lg[:, t, :], in0=bank(5)[:, 0:20],
                                                                    in1=gbt[:, L, GB_RB:GB_RB + 20], op=ALU.add)))(),
                     reads=["ps5"], accum=["lg"])
            d_front(0)
            for t in range(16):
                if t + 1 < 16:
                    d_front(t + 1)
                d_back(t)
            gl = lg[:, :, 0:4]
            el = lg[:, :, 4:20].rearrange("p t (g j) -> p t g j", g=4)
            red(r3, gl, ALU.max, ["lg"], ["r3"])
            tt("dve", r1, gl, bcast(r3, 2, [128, 16, 4]), ALU.is_ge, ["lg", "r3"], ["r1"])
            tt("dve", r2, gl, bcast(r3, 2, [128, 16, 4]), ALU.subtract, ["lg", "r3"], ["r2"])
            act(r2, r2, AF.Exp, ["r2"], ["r2"])
            red(r4, r2, ALU.add, ["r2"], ["r4"])
            recip(r4, r4, ["r4"], ["r4"])
            ts("dve", r1, r1, -1.0, 1.0e4, ALU.add, ALU.mult, ["r1"], ["r1"])
            tt("dve", e1.rearrange("p t (g j) -> p t g j", g=4), el, bcast(r1, 3, [128, 16, 4, 4]), ALU.add, ["lg", "r1"], ["e1"])
            red(r3, e1, ALU.max, ["e1"], ["r3"])
            tt("dve", e1, e1, bcast(r3, 2, [128, 16, 16]), ALU.subtract, ["e1", "r3"], ["e1"])
            ts("dve", e2, e1, 0.0, -2.0, ALU.is_ge, ALU.mult, ["e1"], ["e2"])
            act(e1, e1, AF.Exp, ["e1"], ["e1"])
            tt("dve", e2, e2, e1, ALU.add, ["e2", "e1"], ["e2"])
            red(r5, e2, ALU.max, ["e2"], ["r5"])
            tt("dve", e2, e1, bcast(r5, 2, [128, 16, 16]), ALU.is_ge, ["e1", "r5"], ["e2"])
            tt("dve", e2, e2, e1, ALU.mult, ["e2", "e1"], ["e2"])
            red(r5, e2, ALU.add, ["e2"], ["r5"])
            recip(r5, r5, ["r5"], ["r5"])
            tt("dve", r5, r5, r4, ALU.mult, ["r5", "r4"], ["r5"])
            tt("dve", gate, e2, bcast(r5, 2, [128, 16, 16]), ALU.mult, ["e2", "r5"], ["gate"])
            step = 0
            for e_ in range(NE):
                par = e_ % 2
                if e_ + 1 < NE:
                    load_expert(e_ + 1, 1 - par, sq_)
                for blk in range(4):
                    hb_ = (e_ * 4 + blk) % 2
                    for fc in range(4):
                        gp = step % 2
                        step += 1
                        def gu(e, gp=gp, fc=fc, blk=blk, par=par):
                            ins = None
                            for c in range(8):
                                ins = e.matmul(bank(gp), lhsT=wg[par][:, c, fc * 128:(fc + 1) * 128],
                                               rhs=hT[:, c, blk * 512:(blk + 1) * 512], start=(c == 0), stop=(c == 7))
                            for c in range(8):
                                ins = e.matmul(bank(2 + gp), lhsT=wu[par][:, c, fc * 128:(fc + 1) * 128],
                                               rhs=hT[:, c, blk * 512:(blk + 1) * 512], start=(c == 0), stop=(c == 7))
                            return ins
                        P.op("pe", gu, reads=["hT", ("wg", par), ("wu", par)], writes=["ps%d" % gp, "ps%d" % (2 + gp)])
                        act(sg[gp], bank(gp), AF.Silu, ["ps%d" % gp], [("sg", gp)])
                        P.op("dve", (lambda hb_=hb_, fc=fc, gp=gp: (lambda e: e.tensor_tensor(
                            out=hid[hb_][:, fc, :], in0=bank(2 + gp), in1=sg[gp], op=ALU.mult)))(),
                            reads=["ps%d" % (2 + gp), ("sg", gp)], accum=[("hid", hb_)] if fc else [],
                            writes=[] if fc else [("hid", hb_)])
                    for tt_ in range(4):
                        t = blk * 4 + tt_
                        for half in range(2):
                            ob = 4 + (tt_ * 2 + half) % 2
                            def dn(e, hb_=hb_, tt_=tt_, half=half, ob=ob, par=par):
                                ins = None
                                for fc in range(4):
                                    ins = e.matmul(bank(ob), lhsT=hid[hb_][:, fc, tt_ * 128:(tt_ + 1) * 128],
                                                   rhs=wd[par][:, fc, half * 512:(half + 1) * 512], start=(fc == 0), stop=(fc == 3))
                                return ins
                            P.op("pe", dn, reads=[("hid", hb_), ("wd", par)], writes=["ps%d" % ob])
                            ya = yacc[:, t, half * 512:(half + 1) * 512]
                            P.op("dve", (lambda ya=ya, ob=ob, t=t, e_=e_: (lambda e: e.scalar_tensor_tensor(
                                out=ya, in0=bank(ob), scalar=gate[:, t, e_:e_ + 1], in1=ya, op0=ALU.mult, op1=ALU.add)))(),
                                reads=["ps%d" % ob, "gate", ("yacc", t)], writes=[("yacc", t)])
            for t in range(16):
                tok = sq_ * S + t * 128
                dma("sp", dst[tok:tok + 128, :], yacc[:, t, :], reads=[("yacc", t)], accum=["DST"], chan="d_o")
        P.barrier()
        AR.reset(m)

    cur = x_in
    for L in range(n_layers):
        phase_A(L, cur)
        if stop_after == ("A", L):
            break
        phase_B(L)
        if stop_after == ("B", L):
            break
        phase_C(L, cur)
        if stop_after == ("C", L):
            break
        dst = out_d if L == n_layers - 1 else X2
        phase_D(L, dst)
        cur = X2
    if dbg:
        for name, src, shape, dt in (("dbg_Y", Y, [T, D], F32), ("dbg_X1", X1, [T, D], F32), ("dbg_QT", QT, [NSLOT, 128, T], BF16),
                                     ("dbg_VS", VS, [T, NV], BF16)):
            o = nc.dram_tensor(name, shape, dt, kind="ExternalOutput").ap()
            dma("sp", o, src, chan="dbg")
        P.barrier()
    P.emit()
    return nc


def _rel_bucket(rel):
    half = 16
    max_exact = 8
    n = np.abs(rel)
    nf = np.maximum(n, 1).astype(np.float32)
    log_ratio = np.log(nf / max_exact) / math.log(1024 / max_exact)
    large = np.minimum(max_exact + (log_ratio * (half - max_exact)).astype(np.int32), half - 1)
    return np.where(rel > 0, half, 0) + np.where(n < max_exact, n, large)


def _rope_tab():
    def cs(pos, dim):
        inv = 1.0 / (10000.0 ** (np.arange(0, dim, 2, dtype=np.float32) / dim))
        ang = pos.astype(np.float32)[:, None] * inv[None, :]
        ang = np.concatenate([ang, ang], -1)
        c, s = np.cos(ang), np.sin(ang)
        s = np.concatenate([-s[:, :dim // 2], s[:, dim // 2:]], -1)
        return c.astype(np.float32), s.astype(np.float32)
    pos = np.arange(S)
    tabs = []
    for p in (pos, pos // 64, pos % 64):
        c, s = cs(p, 32)
        tabs += [c, s]
    return np.ascontiguousarray(np.stack(tabs, 1).reshape(S, 6 * 32))


def _strip_index():
    p = np.arange(128)[:, None]
    j = np.arange(STRIP_W)[None, :]
    delta = p - j + STRIP_OFF
    return delta


_CACHE = {}


def _prepare_consts():
    if "c" in _CACHE:
        return _CACHE["c"]
    delta = _strip_index()
    bidx = _rel_bucket(delta)
    ad = np.abs(delta)
    mult = ((ad <= 64).astype(np.float32) + ((delta % 4 == 0) & (ad <= 256)).astype(np.float32)
            + ((delta % 16 == 0) & (ad <= 1024)).astype(np.float32))
    _CACHE["c"] = (bidx, np.ascontiguousarray(mult), _rope_tab())
    return _CACHE["c"]


def _in_maps(inputs):
    bidx, mult, rope = _prepare_consts()
    f = lambda a: np.ascontiguousarray(np.asarray(a, dtype=np.float32))
    rel_bias = f(inputs["rel_bias"])
    g = rel_bias[bidx]
    strip_dil = np.ascontiguousarray(np.transpose(g[:, :, 0:4], (2, 0, 1)))
    strip_dif = np.ascontiguousarray(np.transpose(g[:, :, 4:8], (2, 0, 1)))
    common = {
        "norm1_g": f(inputs["norm1_g"]), "w_in": f(inputs["w_in"]), "mla_q_norm_g": f(inputs["mla_q_norm_g"]),
        "mla_kv_norm_g": f(inputs["mla_kv_norm_g"]), "mla_w_uq": f(inputs["mla_w_uq"]), "mla_w_ukv": f(inputs["mla_w_ukv"]),
        "mla_qk_g": f(inputs["mla_qk_g"]).reshape(DEPTH, 192), "dil_qk_g": f(inputs["dil_qk_g"]).reshape(DEPTH, 128),
        "gqa_qk_g": f(inputs["gqa_qk_g"]).reshape(DEPTH, 128), "diff_qk_g": f(inputs["diff_qk_g"]).reshape(DEPTH, 64),
        "diff_lambda": f(inputs["diff_lambda"]).reshape(DEPTH, 128), "diff_subln_g": f(inputs["diff_subln_g"]),
        "mix_beta": f(inputs["mix_beta"]), "w_out": f(inputs["w_out"]), "norm2_g": f(inputs["norm2_g"]),
        "router_w": np.ascontiguousarray(np.concatenate([f(inputs["router_group_w"]), f(inputs["router_expert_w"])], -1)),
        "router_b": np.ascontiguousarray(np.concatenate([f(inputs["router_group_b"]), f(inputs["router_expert_b"])], -1)),
        "expert_w_gate": f(inputs["expert_w_gate"]), "expert_w_up": f(inputs["expert_w_up"]),
        "expert_w_down": f(inputs["expert_w_down"]),
        "rope_tab": rope, "strip_dil": strip_dil, "strip_dif": strip_dif, "strip_mult": mult,
    }
    x = f(inputs["x"])
    maps = []
    for c in range(NCORES):
        mp = dict(common)
        mp["x"] = np.ascontiguousarray(x[c * SEQ_PER_CORE:(c + 1) * SEQ_PER_CORE].reshape(T, D))
        maps.append(mp)
    return maps


def kernel(**inputs):
    if "nc" not in _CACHE:
        _CACHE["nc"] = build_program()
    nc = _CACHE["nc"]
    maps = _in_maps(inputs)
    res = run_bass_kernel_spmd(nc, maps, core_ids=list(range(NCORES)))
    out = np.stack([np.asarray(r["out"]).reshape(SEQ_PER_CORE, S, D) for r in res.results], 0)
    return out.reshape(BATCH, S, D).astype(np.float32)
```
